# Optimizing a Trainium2 kernel written in Bass

```python
import math
import jax, jax.numpy as jnp
from jax import lax
import numpy as np

D_MODEL = 2048
BATCH = 4
SEQ = 4096
DEPTH = 1

HEAD_DIM = 128
A_Q_HEADS = 8
A_KV_HEADS = 2
A_GROUP = A_Q_HEADS // A_KV_HEADS
ROPE_THETA = 10000.0
GRID_W = 64
B_HEADS = 8
B_QK_DIM = 64
B_V_DIM = 2 * B_QK_DIM
REL_BUCKETS = 32
REL_MAX_DIST = 128
N_EXPERTS = 32
TOP_K = 4
D_FF = D_MODEL
SWIGLU_LIMIT = 7.0
SWIGLU_ALPHA = 1.702
MOE_BLOCK = 128
Q_BLOCK = 128
EPS = 1e-6

A_Q_W = A_Q_HEADS * HEAD_DIM
A_KV_W = A_KV_HEADS * HEAD_DIM
B_QK_W = B_HEADS * 2 * B_QK_DIM
B_V_W = B_HEADS * B_V_DIM
IN_W = A_Q_W + 2 * A_KV_W + 2 * B_QK_W + B_V_W
MIX_W = A_Q_W + B_V_W

kernel_name = "hymba_style_gqa_axialrope_diffattn_moe_encoder"


def lambda_init_for(layer_idx):
    return 0.8 - 0.6 * math.exp(-0.3 * layer_idx)


def rms_norm(x, g):
    xf = x.astype(jnp.float32)
    y = xf * lax.rsqrt(jnp.mean(xf * xf, axis=-1, keepdims=True) + EPS)
    return (y * g.astype(jnp.float32)).astype(x.dtype)


def axial_rope_tables(rows):
    row = jnp.repeat(jnp.arange(rows, dtype=jnp.float32), GRID_W)
    col = jnp.tile(jnp.arange(GRID_W, dtype=jnp.float32), rows)
    axis_dim = HEAD_DIM // 2
    inv = ROPE_THETA ** (-jnp.arange(0, axis_dim, 2, dtype=jnp.float32) / axis_dim)
    ang = jnp.concatenate([row[:, None] * inv, col[:, None] * inv], axis=-1)
    return jnp.cos(ang), jnp.sin(ang)


def apply_rope(x, cos, sin):
    xr = x.reshape(x.shape[:-1] + (HEAD_DIM // 2, 2)).astype(jnp.float32)
    c = cos[:, None, :]
    s = sin[:, None, :]
    x0, x1 = xr[..., 0], xr[..., 1]
    out = jnp.stack([x0 * c - x1 * s, x0 * s + x1 * c], axis=-1)
    return out.reshape(x.shape).astype(x.dtype)


def rel_bucket(rp):
    nb = REL_BUCKETS // 2
    max_exact = nb // 2
    ret = jnp.where(rp > 0, nb, 0)
    n = jnp.abs(rp)
    nf = jnp.maximum(n, 1).astype(jnp.float32)
    large = max_exact + (jnp.log(nf / max_exact) / math.log(REL_MAX_DIST / max_exact)
                         * (nb - max_exact)).astype(jnp.int32)
    large = jnp.minimum(large, nb - 1)
    return ret + jnp.where(n < max_exact, n, large)


def gqa_axial_attention(q, k, v):
    B, S = q.shape[:2]
    nb = S // Q_BLOCK
    qg = q.reshape(B, nb, Q_BLOCK, A_KV_HEADS, A_GROUP, HEAD_DIM).transpose(1, 0, 2, 3, 4, 5)
    scale = HEAD_DIM ** -0.5

    def block(qb):
        s = jnp.einsum('bqkgd,bskd->bkgqs', qb, k).astype(jnp.float32) * scale
        p = jax.nn.softmax(s, axis=-1).astype(v.dtype)
        return jnp.einsum('bkgqs,bskd->bqkgd', p, v)

    o = lax.map(block, qg)
    return o.transpose(1, 0, 2, 3, 4, 5).reshape(B, S, A_Q_W)


def diff_attention(q1, q2, k1, k2, v, rel_table, lam, subln_g, lambda_init):
    B, S = q1.shape[:2]
    nb = S // Q_BLOCK
    q1b = q1.reshape(B, nb, Q_BLOCK, B_HEADS, B_QK_DIM).transpose(1, 0, 2, 3, 4)
    q2b = q2.reshape(B, nb, Q_BLOCK, B_HEADS, B_QK_DIM).transpose(1, 0, 2, 3, 4)
    starts = jnp.arange(nb, dtype=jnp.int32) * Q_BLOCK
    k_pos = jnp.arange(S, dtype=jnp.int32)
    scale = B_QK_DIM ** -0.5

    def block(args):
        qa, qb, start = args
        q_pos = start + jnp.arange(Q_BLOCK, dtype=jnp.int32)
        bucket = rel_bucket(k_pos[None, :] - q_pos[:, None])
        bias = rel_table.astype(jnp.float32)[bucket].transpose(2, 0, 1)
        s1 = jnp.einsum('bqhd,bshd->bhqs', qa, k1).astype(jnp.float32) * scale + bias
        s2 = jnp.einsum('bqhd,bshd->bhqs', qb, k2).astype(jnp.float32) * scale + bias
        attn = jax.nn.softmax(s1, axis=-1) - lam * jax.nn.softmax(s2, axis=-1)
        o = jnp.einsum('bhqs,bshd->bqhd', attn.astype(v.dtype), v)
        return rms_norm(o, subln_g) * (1.0 - lambda_init)

    o = lax.map(block, (q1b, q2b, starts))
    return o.transpose(1, 0, 2, 3, 4).reshape(B, S, B_V_W)


def routed_experts(h, w_router, b_router, w_gu, b_gu, w_down, b_down):
    T, D = h.shape
    logits = (h @ w_router).astype(jnp.float32) + b_router.astype(jnp.float32)
    top_val, top_idx = lax.top_k(logits, TOP_K)
    gates = jax.nn.softmax(top_val, axis=-1)
    n_assign = T * TOP_K
    e_flat = top_idx.reshape(-1)
    tok_flat = jnp.arange(n_assign, dtype=jnp.int32) // TOP_K
    g_flat = gates.reshape(-1)
    order = jnp.argsort(e_flat)
    e_sorted = e_flat[order]
    tok_sorted = tok_flat[order]
    g_sorted = g_flat[order]
    counts = jnp.bincount(e_flat, length=N_EXPERTS)
    padded = (counts + MOE_BLOCK - 1) // MOE_BLOCK * MOE_BLOCK
    start = jnp.cumsum(counts) - counts
    pad_end = jnp.cumsum(padded)
    pad_start = pad_end - padded
    dest = pad_start[e_sorted] + (jnp.arange(n_assign, dtype=jnp.int32) - start[e_sorted])
    n_rows = n_assign + N_EXPERTS * MOE_BLOCK
    n_blocks = n_rows // MOE_BLOCK
    row_tok = jnp.full((n_rows,), T, jnp.int32).at[dest].set(tok_sorted)
    row_gate = jnp.zeros((n_rows,), jnp.float32).at[dest].set(g_sorted)
    block_expert = jnp.minimum(
        jnp.searchsorted(pad_end, jnp.arange(n_blocks, dtype=jnp.int32) * MOE_BLOCK, side='right'),
        N_EXPERTS - 1)
    h_pad = jnp.concatenate([h, jnp.zeros((1, D), h.dtype)], axis=0)

    def expert_block(args):
        toks, e = args
        xb = h_pad[toks]
        gu = xb @ w_gu[e] + b_gu[e]
        g, u = jnp.split(gu, 2, axis=-1)
        g = jnp.minimum(g, SWIGLU_LIMIT)
        u = jnp.clip(u, -SWIGLU_LIMIT, SWIGLU_LIMIT)
        glu = g * jax.nn.sigmoid(SWIGLU_ALPHA * g)
        return ((u + 1.0) * glu) @ w_down[e] + b_down[e]

    out = lax.map(expert_block, (row_tok.reshape(n_blocks, MOE_BLOCK), block_expert))
    out = out.reshape(n_rows, D) * row_gate[:, None].astype(out.dtype)
    y = jnp.zeros((T + 1, D), out.dtype).at[row_tok].add(out)
    return y[:T]


def setup_inputs(seed: int = 0) -> dict:
    key = jax.random.key(seed)
    ks = jax.random.split(key, 24)
    f32 = jnp.float32
    nrm = lambda k, shape, s: jax.random.normal(k, shape, f32) * s
    gain = lambda k, shape: 1.0 + 0.01 * jax.random.normal(k, shape, f32)
    return {
        "x": nrm(ks[0], (BATCH, SEQ, D_MODEL), 1.0),
        "c": nrm(ks[1], (BATCH, D_MODEL), 1.0),
        "rel_bias_table": nrm(ks[2], (REL_BUCKETS, B_HEADS), 0.5),
        "w_ada": nrm(ks[3], (DEPTH, D_MODEL, 6 * D_MODEL), 0.01),
        "b_ada": nrm(ks[4], (DEPTH, 6 * D_MODEL), 0.01),
        "norm_attn": gain(ks[5], (DEPTH, D_MODEL)),
        "norm_ffn": gain(ks[6], (DEPTH, D_MODEL)),
        "w_in": nrm(ks[7], (DEPTH, D_MODEL, IN_W), D_MODEL ** -0.5),
        "w_out": nrm(ks[8], (DEPTH, MIX_W, D_MODEL), MIX_W ** -0.5),
        "a_q_norm": gain(ks[9], (DEPTH, HEAD_DIM)),
        "a_k_norm": gain(ks[10], (DEPTH, HEAD_DIM)),
        "b_q_norm": gain(ks[11], (DEPTH, B_QK_DIM)),
        "b_k_norm": gain(ks[12], (DEPTH, B_QK_DIM)),
        "lambda_q1": nrm(ks[13], (DEPTH, B_QK_DIM), 0.1),
        "lambda_k1": nrm(ks[14], (DEPTH, B_QK_DIM), 0.1),
        "lambda_q2": nrm(ks[15], (DEPTH, B_QK_DIM), 0.1),
        "lambda_k2": nrm(ks[16], (DEPTH, B_QK_DIM), 0.1),
        "b_subln": gain(ks[17], (DEPTH, B_V_DIM)),
        "w_router": nrm(ks[18], (DEPTH, D_MODEL, N_EXPERTS), D_MODEL ** -0.5),
        "b_router": nrm(ks[19], (DEPTH, N_EXPERTS), 0.01),
        "w_gu": nrm(ks[20], (DEPTH, N_EXPERTS, D_MODEL, 2 * D_FF), D_MODEL ** -0.5),
        "b_gu": nrm(ks[21], (DEPTH, N_EXPERTS, 2 * D_FF), 0.01),
        "w_down": nrm(ks[22], (DEPTH, N_EXPERTS, D_FF, D_MODEL), D_FF ** -0.5),
        "b_down": nrm(ks[23], (DEPTH, N_EXPERTS, D_MODEL), 0.01),
    }


def reference(x, c, rel_bias_table, w_ada, b_ada, norm_attn, norm_ffn, w_in, w_out,
              a_q_norm, a_k_norm, b_q_norm, b_k_norm, lambda_q1, lambda_k1, lambda_q2,
              lambda_k2, b_subln, w_router, b_router, w_gu, b_gu, w_down, b_down):
    B, S, D = x.shape
    rows = S // GRID_W
    cos, sin = axial_rope_tables(rows)
    c_act = jax.nn.silu(c)
    for l in range(DEPTH):
        lam_init = lambda_init_for(l)
        mod = (c_act @ w_ada[l] + b_ada[l])[:, None, :]
        sh_a, sc_a, g_a, sh_f, sc_f, g_f = jnp.split(mod, 6, axis=-1)

        h = rms_norm(x, norm_attn[l]) * (1.0 + sc_a) + sh_a
        proj = h @ w_in[l]
        o0 = A_Q_W
        o1 = o0 + A_KV_W
        o2 = o1 + A_KV_W
        o3 = o2 + B_QK_W
        o4 = o3 + B_QK_W
        qa = proj[..., :o0].reshape(B, S, A_Q_HEADS, HEAD_DIM)
        ka = proj[..., o0:o1].reshape(B, S, A_KV_HEADS, HEAD_DIM)
        va = proj[..., o1:o2].reshape(B, S, A_KV_HEADS, HEAD_DIM)
        qa = apply_rope(rms_norm(qa, a_q_norm[l]), cos, sin)
        ka = apply_rope(rms_norm(ka, a_k_norm[l]), cos, sin)
        out_a = gqa_axial_attention(qa, ka, va)
        qb = rms_norm(proj[..., o2:o3].reshape(B, S, B_HEADS, 2, B_QK_DIM), b_q_norm[l])
        kb = rms_norm(proj[..., o3:o4].reshape(B, S, B_HEADS, 2, B_QK_DIM), b_k_norm[l])
        vb = proj[..., o4:].reshape(B, S, B_HEADS, B_V_DIM)
        lam = (jnp.exp(jnp.sum(lambda_q1[l].astype(jnp.float32) * lambda_k1[l].astype(jnp.float32)))
               - jnp.exp(jnp.sum(lambda_q2[l].astype(jnp.float32) * lambda_k2[l].astype(jnp.float32)))
               + lam_init)
        out_b = diff_attention(qb[..., 0, :], qb[..., 1, :], kb[..., 0, :], kb[..., 1, :], vb,
                               rel_bias_table, lam, b_subln[l], lam_init)
        mix = jnp.concatenate([out_a, out_b], axis=-1) @ w_out[l]
        x = x + g_a * mix

        h = rms_norm(x, norm_ffn[l]) * (1.0 + sc_f) + sh_f
        y = routed_experts(h.reshape(B * S, D), w_router[l], b_router[l], w_gu[l], b_gu[l],
                           w_down[l], b_down[l]).reshape(B, S, D)
        x = x + g_f * y
    return x
```

```python
import math
from contextlib import ExitStack
import numpy as np
import concourse.bass as bass
import concourse.mybir as mybir
from concourse.bass_utils import run_bass_kernel_spmd

F32 = mybir.dt.float32
BF16 = mybir.dt.bfloat16
I32 = mybir.dt.int32
AF = mybir.ActivationFunctionType
ALU = mybir.AluOpType
AX = mybir.AxisListType

EPS = 1e-6
HEAD = 128
TOPK = 4
LIM = 7.0
ALPHA = 1.702
LAM_INIT = 0.8 - 0.6 * math.exp(0.0)
FEXT = 640
ENG = ("pe", "act", "dve", "pool", "sp")


class Cfg:
    def __init__(self, D=2048, S=4096, E=32, DFF=2048, CAP=768):
        self.D, self.S, self.E, self.DFF, self.CAP = D, S, E, DFF, CAP
        self.DC = D // 128
        self.FC = DFF // 128
        self.NQ = S // 2
        self.NGRP = 6
        self.GW = 768


class Buf:
    __slots__ = ("w", "r")

    def __init__(self):
        self.w = {}
        self.r = {}


class Sem:
    def __init__(self, h):
        self.h = h
        self.n = 0


class _Rec:
    def __getattr__(self, name):
        def f(*a, **k):
            self.call = (name, a, k)
            return self
        return f


class Prog:
    def __init__(self, nc, sems):
        self.nc = nc
        self.free_sems = list(sems)
        self.prog = {e: Sem(self.free_sems.pop()) for e in ENG if e != "sp"}
        self.bar = Sem(self.free_sems.pop())
        self.ops = {e: [] for e in ENG}
        self.waited = {e: {} for e in ENG}
        self.pend_r = {e: [] for e in ENG}
        self.pend_w = {e: [] for e in ENG}
        self.touched = {e: {} for e in ENG}
        self.nphase = 0

    def sem(self):
        return Sem(self.free_sems.pop())

    def _deps(self, eng, reads, writes, extra):
        deps = {}

        def add(tok):
            s, v = tok
            if deps.get(s, (None, 0))[1] < v:
                deps[s] = (s, v)

        for b in reads:
            for t in b.w.values():
                add(t)
        for b in writes:
            for t in b.w.values():
                add(t)
            for t in b.r.values():
                add(t)
        for t in extra:
            if t is not None:
                add(t)
        out = []
        wd = self.waited[eng]
        for s, v in deps.values():
            if wd.get(s.h, 0) < v:
                wd[s.h] = v
                out.append((s.h, v))
        return out

    def op(self, eng, fn, reads=(), writes=(), sig=True, dma=None, extra=()):
        waits = self._deps(eng, reads, writes, extra)
        tok = None
        rec = _Rec()
        fn(rec)
        fn = rec.call
        if dma is not None:
            dma.n += 16
            tok = (dma, dma.n)
            self.ops[eng].append((fn, waits, dma.h, 16))
            self.touched[eng][dma.h] = (dma, dma.n)
            for b in reads:
                b.r[("dma", dma.h)] = tok
            for b in writes:
                b.w = {("dma", dma.h): tok}
                b.r = {}
            return tok
        if sig:
            s = self.prog[eng]
            s.n += 1
            tok = (s, s.n)
            self.ops[eng].append((fn, waits, s.h, 1))
            self.touched[eng][s.h] = (s, s.n)
            for b in list(reads) + self.pend_r[eng]:
                b.r[eng] = tok
            for b in list(writes) + self.pend_w[eng]:
                b.w = {eng: tok}
                b.r = {}
            self.pend_r[eng] = []
            self.pend_w[eng] = []
        else:
            self.ops[eng].append((fn, waits, None, 0))
            self.pend_r[eng] += list(reads)
            self.pend_w[eng] += list(writes)
        return tok

    def flush(self):
        nc = self.nc
        self.nphase += 1
        target = 5 * self.nphase
        ops = self.ops
        touched = self.touched
        bar = self.bar

        def mk(name):
            def body(e):
                regs = {}
                for fn, waits, inc, amt in ops[name]:
                    for s, v in waits:
                        e.wait_ge(s, v)
                    kw = fn[2]
                    bc = kw.get("bounds_check")
                    if isinstance(bc, int):
                        if bc not in regs:
                            regs[bc] = e.to_reg(bc)
                        kw = dict(kw, bounds_check=regs[bc])
                    ins = getattr(e, fn[0])(*fn[1], **kw)
                    if inc is not None:
                        ins.then_inc(inc, amt)
                for s, v in touched[name].values():
                    e.wait_ge(s.h, v)
                e.nop().then_inc(bar.h, 1)
                e.wait_ge(bar.h, target)
            return body

        with nc.Block() as blk:
            blk.tensor(mk("pe"))
            blk.scalar(mk("act"))
            blk.vector(mk("dve"))
            blk.gpsimd(mk("pool"))
            blk.sync(mk("sp"))
        self.ops = {e: [] for e in ENG}
        self.touched = {e: {} for e in ENG}
        assert all(not v for v in self.pend_r.values()) and all(not v for v in self.pend_w.values())


def build(cfg, debug=None, stop_after=None):
    D, S, E, DFF, CAP, DC, FC, NQ = cfg.D, cfg.S, cfg.E, cfg.DFF, cfg.CAP, cfg.DC, cfg.FC, cfg.NQ
    NTC = S // 128
    NQC = NQ // 128
    NTB = S // 512
    NQB = NQ // 512
    NROW = (E + 1) * CAP
    nc = bass.Bass("TRN2", target_bir_lowering=False)
    dt = nc.dram_tensor

    xkv = dt("xkv", [S, D], F32, kind="ExternalInput").ap()
    c_fm = dt("c_fm", [128, DC], F32, kind="ExternalInput").ap()
    w_ada = dt("w_ada", [D, 6 * D], F32, kind="ExternalInput").ap()
    bada_rep = dt("bada_rep", [128, 6 * D], F32, kind="ExternalInput").ap()
    gn_rep = dt("gn_rep", [128, 2 * D], F32, kind="ExternalInput").ap()
    win_g = dt("win_g", [D, 6 * 768], F32, kind="ExternalInput").ap()
    w_out = dt("w_out", [2048, D], F32, kind="ExternalInput").ap()
    gains = dt("gains", [128, 8], F32, kind="ExternalInput").ap()
    lam_rep = dt("lam_rep", [128, 4 * 64], F32, kind="ExternalInput").ap()
    cmat = dt("cmat", [128, 6 * 128], F32, kind="ExternalInput").ap()
    ropec = dt("ropec", [128, S], F32, kind="ExternalInput").ap()
    ropes = dt("ropes", [128, S], F32, kind="ExternalInput").ap()
    relT = dt("relT", [32, 16], F32, kind="ExternalInput").ap()
    ohr = dt("ohr", [32, 2 * FEXT + 2], F32, kind="ExternalInput").ap()
    w_router = dt("w_router", [D, E], F32, kind="ExternalInput").ap()
    brout_rep = dt("brout_rep", [128, E], F32, kind="ExternalInput").ap()
    w_gu = dt("w_gu", [E, D, 2 * DFF], F32, kind="ExternalInput").ap()
    bgu_fm = dt("bgu_fm", [128, E * 2 * FC], F32, kind="ExternalInput").ap()
    w_down = dt("w_down", [E, DFF, D], F32, kind="ExternalInput").ap()
    b_down = dt("b_down", [E, D], F32, kind="ExternalInput").ap()
    iota_e = dt("iota_e", [128, E], F32, kind="ExternalInput").ap()
    padrow = dt("padrow", [128, E], F32, kind="ExternalInput").ap()
    out = dt("out", [NQ, D], F32, kind="ExternalOutput").ap()

    debug = debug or ()
    ds = lambda name, shape, dtype: dt(name, shape, dtype, kind=("ExternalOutput" if name in debug else "Internal")).ap()
    hT_d = ds("hT_d", [DC, 128, S], BF16)
    mod_d = ds("mod_d", [128, 6 * D], F32)
    OT_d = ds("OT_d", [16, 128, NQ], BF16)
    fext_d = ds("fext_d", [8, 2 * FEXT + 2], F32)
    hs_d = ds("hs_d", [NROW, D], BF16)
    ys_d = ds("ys_d", [NROW, D], F32)

    P = Prog(nc, [nc.semaphore(f"s{i}").__enter__() for i in range(90)])
    SBG = lambda name, shape, dtype: nc.sbuf_tensor(name, shape, dtype).__enter__()
    PS = lambda name, shape, dtype=F32: nc.psum_tensor(name, shape, dtype).__enter__()
    es = [ExitStack()]
    SB = lambda name, shape, dtype: es[0].enter_context(nc.sbuf_tensor(name, shape, dtype))

    def end_phase():
        P.flush()
        es[0].close()
        es[0] = ExitStack()

    CM = SBG("CM", [128, 6, 128], BF16)
    bCM = Buf()
    dsem = P.sem()
    P.op("pool", lambda e: e.dma_start(out=CM[:], in_=cmat.rearrange("p (k n) -> p k n", k=6)), writes=[bCM], dma=dsem)
    IDENT, ONES, JM, SWP, TRI, BLK64 = (CM[:, i, :] for i in range(6))

    epsc = SBG("epsc", [128, 2], F32)
    beps = Buf()
    P.op("dve", lambda e: e.memset(epsc[:, 0:1], EPS), writes=[beps])
    P.op("dve", lambda e: e.memset(epsc[:, 1:2], 0.0), writes=[beps])
    banks = [PS(f"bank{i}", [128, 512]) for i in range(8)]
    bbank = [Buf() for _ in range(8)]

    modrep = SB("modrep", [128, 6 * D], F32)
    bmod = [Buf() for _ in range(6)]
    cin = SB("cin", [128, DC], F32)
    cact = SB("cact", [128, DC], F32)
    crep = SB("crep", [128, DC, 128], BF16)
    bc = Buf(); bcr = Buf()
    s_c = P.sem()
    P.op("sp", lambda e: e.dma_start(out=cin[:], in_=c_fm), writes=[bc], dma=s_c)
    P.op("act", lambda e: e.activation(out=cact[:], in_=cin[:], func=AF.Silu), reads=[bc], writes=[bcr])
    P.op("dve", lambda e: e.tensor_copy(out=crep[:], in_=cact[:].unsqueeze(2).to_broadcast([128, DC, 128])),
         reads=[bcr], writes=[bcr])
    badar = [SB(f"badar{i}", [128, 512], F32) for i in range(2)]
    gnr = SB("gnr", [128, 2 * D], F32)
    bbada = [Buf(), Buf()]; bgn = Buf()
    s_b1 = [P.sem(), P.sem()]; s_b2 = P.sem()
    P.op("sp", lambda e: e.dma_start(out=gnr[:], in_=gn_rep), writes=[bgn], dma=s_b2)
    wa = [SB(f"wa{i}", [128, DC, 512], BF16) for i in range(2)]
    bwa = [Buf(), Buf()]
    s_wa = [P.sem(), P.sem()]
    wada_v = w_ada.rearrange("(c p) n -> p c n", p=128)
    NCB = 6 * D // 512
    for cb in range(NCB):
        sl = cb % 2
        P.op("pool", lambda e, sl=sl, cb=cb: e.dma_start(out=wa[sl][:], in_=wada_v[:, :, cb * 512:(cb + 1) * 512]),
             writes=[bwa[sl]], dma=s_wa[sl])
        bk = cb % 2
        P.op("sp", lambda e, sl=sl, cb=cb: e.dma_start(out=badar[sl][:], in_=bada_rep[:, cb * 512:(cb + 1) * 512]),
             writes=[bbada[sl]], dma=s_b1[sl])
        for j in range(DC):
            P.op("pe", lambda e, sl=sl, j=j, bk=bk: e.matmul(banks[bk][:], lhsT=crep[:, j, :], rhs=wa[sl][:, j, :],
                                                             start=(j == 0), stop=(j == DC - 1)),
                 reads=[bcr, bwa[sl]], writes=[bbank[bk]], sig=(j == DC - 1))
        seg = (cb * 512) // D
        P.op("dve", lambda e, bk=bk, cb=cb: e.tensor_tensor(out=modrep[:, cb * 512:(cb + 1) * 512], in0=banks[bk][:],
                                                            in1=badar[bk][:], op=ALU.add),
             reads=[bbank[bk], bbada[bk]], writes=[bmod[seg]])
    for seg, gi in ((1, 0), (4, 1)):
        P.op("dve", lambda e, seg=seg, gi=gi: e.scalar_tensor_tensor(
            out=modrep[:, seg * D:(seg + 1) * D], in0=modrep[:, seg * D:(seg + 1) * D], scalar=1.0,
            in1=gnr[:, gi * D:(gi + 1) * D], op0=ALU.add, op1=ALU.mult), reads=[bmod[seg], bgn], writes=[bmod[seg]])
    bmod_d = Buf()
    s_md = P.sem()
    P.op("sp", lambda e: e.dma_start(out=mod_d, in_=modrep[:]), reads=bmod, writes=[bmod_d], dma=s_md)

    xt = [SB(f"xt{i}", [128, D], F32) for i in range(2)]
    bxt = [Buf(), Buf()]
    s_xt = [P.sem(), P.sem()]
    junk = SB("junk", [128, D], BF16)
    bjunk = Buf()
    ssA = SB("ssA", [128, NTC], F32)
    rsA = SB("rsA", [128, NTC], F32)
    bss = [Buf() for _ in range(NTC)]
    tmpA = SB("tmpA", [128, D], F32)
    btmpA = Buf()
    hb = [SB(f"hb{i}", [128, 4, D], BF16) for i in range(2)]
    bhb = [Buf(), Buf()]
    hTb = [SB(f"hTb{i}", [128, DC, 512], BF16) for i in range(2)]
    bhTb = [Buf(), Buf()]
    s_hst = [P.sem(), P.sem()]
    bhT_d = Buf()
    hT_v = hT_d.rearrange("c p s -> p c s")
    for t in range(NTC):
        sl = t % 2
        g4 = (t // 4) % 2
        P.op("sp", lambda e, sl=sl, t=t: e.dma_start(out=xt[sl][:], in_=xkv[t * 128:(t + 1) * 128, :]),
             writes=[bxt[sl]], dma=s_xt[sl])
        P.op("act", lambda e, sl=sl, t=t: e.activation(out=junk[:], in_=xt[sl][:], func=AF.Square,
                                                       accum_out=ssA[:, t:t + 1]),
             reads=[bxt[sl]], writes=[bjunk, bss[t]])
        P.op("act", lambda e, t=t: e.activation(out=rsA[:, t:t + 1], in_=ssA[:, t:t + 1], func=AF.Sqrt,
                                                bias=epsc[:, 0:1], scale=1.0 / D), reads=[bss[t], beps], writes=[bss[t]])
        P.op("dve", lambda e, t=t: e.reciprocal(out=rsA[:, t:t + 1], in_=rsA[:, t:t + 1]), reads=[bss[t]], writes=[bss[t]])
        P.op("dve", lambda e, sl=sl, t=t: e.scalar_tensor_tensor(out=tmpA[:], in0=xt[sl][:], scalar=rsA[:, t:t + 1],
                                                                 in1=modrep[:, D:2 * D], op0=ALU.mult, op1=ALU.mult),
             reads=[bxt[sl], bss[t], bmod[1]], writes=[btmpA])
        P.op("dve", lambda e, g4=g4, t=t: e.tensor_tensor(out=hb[g4][:, t % 4, :], in0=tmpA[:], in1=modrep[:, 0:D],
                                                          op=ALU.add), reads=[btmpA, bmod[0]], writes=[bhb[g4]])
        if t % 4 == 3:
            tb = t // 4
            hs = tb % 2
            for j in range(DC):
                bk = 2 + (j % 2)
                pv = banks[bk][:].bitcast(BF16)
                for q in range(4):
                    P.op("pe", lambda e, g4=g4, j=j, q=q, pv=pv: e.transpose(
                        out=pv[:, q * 128:(q + 1) * 128], in_=hb[g4][:, q, j * 128:(j + 1) * 128], identity=IDENT),
                        reads=[bhb[g4], bCM], writes=[bbank[bk]], sig=(q == 3))
                P.op("act", lambda e, hs=hs, j=j, pv=pv: e.copy(out=hTb[hs][:, j, :], in_=pv[:, 0:512]),
                     reads=[bbank[bk]], writes=[bhTb[hs]])
            P.op("sp", lambda e, hs=hs, tb=tb: e.dma_start(out=hT_v[:, :, tb * 512:(tb + 1) * 512], in_=hTb[hs][:]),
                 reads=[bhTb[hs]], writes=[bhT_d], dma=s_hst[hs])
    end_phase()
    if stop_after == "A":
        return nc, P
    NW = 2 * FEXT + 2
    gsb = SB("gsb", [128, 8], F32); bg_ = Buf()
    lamr = SB("lamr", [128, 256], F32); blam = Buf()
    lamt = SB("lamt", [128, 128], F32)
    lams = SB("lams", [128, 4], F32)
    s_g = P.sem(); s_l = P.sem()
    P.op("sp", lambda e: e.dma_start(out=gsb[:], in_=gains), writes=[bg_], dma=s_g)
    P.op("sp", lambda e: e.dma_start(out=lamr[:], in_=lam_rep), writes=[blam], dma=s_l)
    P.op("dve", lambda e: e.tensor_tensor(out=lamt[:, 0:64], in0=lamr[:, 0:64], in1=lamr[:, 64:128], op=ALU.mult),
         reads=[blam], writes=[blam])
    P.op("dve", lambda e: e.tensor_tensor(out=lamt[:, 64:128], in0=lamr[:, 128:192], in1=lamr[:, 192:256], op=ALU.mult),
         reads=[blam], writes=[blam])
    P.op("dve", lambda e: e.reduce_sum(out=lams[:, 0:1], in_=lamt[:, 0:64], axis=AX.X), reads=[blam], writes=[blam])
    P.op("dve", lambda e: e.reduce_sum(out=lams[:, 1:2], in_=lamt[:, 64:128], axis=AX.X), reads=[blam], writes=[blam])
    P.op("act", lambda e: e.activation(out=lams[:, 0:2], in_=lams[:, 0:2], func=AF.Exp), reads=[blam], writes=[blam])
    P.op("dve", lambda e: e.tensor_tensor(out=lams[:, 3:4], in0=lams[:, 1:2], in1=lams[:, 0:1], op=ALU.subtract),
         reads=[blam], writes=[blam])
    P.op("dve", lambda e: e.tensor_scalar_add(out=lams[:, 2:3], in0=lams[:, 3:4], scalar1=-LAM_INIT), reads=[blam], writes=[blam])
    NEGLAM = lams[:, 2:3]
    P.op("dve", lambda e: e.tensor_scalar_mul(out=gsb[:, 5:6], in0=gsb[:, 4:5], scalar1=1.0 - LAM_INIT), reads=[bg_], writes=[bg_])
    SUBG = gsb[:, 5:6]

    tab32 = SB("tab32", [32, 16], F32); btab = Buf()
    tabh = SB("tabh", [32, 16], BF16)
    ohb = SB("ohb", [32, NW], BF16); boh = Buf()
    fx = SB("fx", [8, NW], F32); bfx = Buf()
    s_t = P.sem(); s_o = P.sem(); s_f = P.sem()
    P.op("sp", lambda e: e.dma_start(out=tab32[:], in_=relT), writes=[btab], dma=s_t)
    P.op("pool", lambda e: e.dma_start(out=ohb[:], in_=ohr), writes=[boh], dma=s_o)
    P.op("dve", lambda e: e.tensor_copy(out=tabh[:, 0:8], in_=tab32[:, 0:8]), reads=[btab], writes=[btab])
    P.op("dve", lambda e: e.tensor_tensor(out=tab32[:, 8:16], in0=tab32[:, 8:16], in1=tabh[:, 0:8], op=ALU.subtract),
         reads=[btab], writes=[btab])
    P.op("dve", lambda e: e.tensor_copy(out=tabh[:, 8:16], in_=tab32[:, 8:16]), reads=[btab], writes=[btab])
    for pc in range(0, NW, 512):
        n = min(512, NW - pc)
        P.op("pe", lambda e, pc=pc, n=n: e.matmul(banks[7][0:8, 0:n], lhsT=tabh[:, 0:8], rhs=ohb[:, pc:pc + n], start=True, stop=False),
             reads=[btab, boh], writes=[bbank[7]], sig=False)
        P.op("pe", lambda e, pc=pc, n=n: e.matmul(banks[7][0:8, 0:n], lhsT=tabh[:, 8:16], rhs=ohb[:, pc:pc + n], start=False, stop=True),
             reads=[btab, boh], writes=[bbank[7]])
        P.op("dve", lambda e, pc=pc, n=n: e.tensor_copy(out=fx[:, pc:pc + n], in_=banks[7][0:8, 0:n]), reads=[bbank[7]], writes=[bfx])
    bfext_d = Buf()
    P.op("sp", lambda e: e.dma_start(out=fext_d, in_=fx[:]), reads=[bfx], writes=[bfext_d], dma=s_f)
    cfar = SB("cfar", [128, 2, 8], F32); bcfar = Buf()
    s_cf = P.sem()
    for fs in range(2):
        cf_src = bass.AP(tensor=fext_d.tensor, offset=fs * 2 * FEXT, ap=[[0, 128], [NW, 8]])
        P.op("sp", lambda e, fs=fs, cf_src=cf_src: e.dma_start(out=cfar[:, fs, :], in_=cf_src, allow_slow_non_contiguous=True),
             reads=[bfext_d], writes=[bcfar], dma=s_cf)

    wg = SB("wg", [128, DC, 768], BF16); bwg = Buf(); s_wg = P.sem()
    hTb2 = [SB(f"hTc{i}", [128, DC, 512], BF16) for i in range(2)]
    bhTb2 = [Buf(), Buf()]; s_hl = [P.sem(), P.sem()]
    QT = [SB(f"QT{i}", [128, NQ], BF16) for i in range(4)]; bQT = [Buf() for _ in range(4)]
    KT = [SB(f"KT{i}", [128, S], BF16) for i in range(2)]; bKT = [Buf() for _ in range(2)]
    VV = SB("VV", [128, NTC, 256], BF16); bVV = Buf()
    sqb = [SB(f"sqb{i}", [128, 512], BF16) for i in range(2)]; bsqb = [Buf(), Buf()]
    rsb = [SB(f"rsb{i}", [128, 512], F32) for i in range(2)]; brsb = [Buf(), Buf()]
    qnb = [SB(f"qnb{i}", [128, 512], BF16) for i in range(2)]; bqnb = [Buf(), Buf()]
    t1b = [SB(f"t1b{i}", [128, 512], F32) for i in range(2)]; bt1b = [Buf(), Buf()]
    t2b = [SB(f"t2b{i}", [128, 512], F32) for i in range(2)]; bt2b = [Buf(), Buf()]
    cosb = [SB(f"cosb{i}", [128, 512], F32) for i in range(4)]; bcos = [Buf() for _ in range(4)]; s_cos = [P.sem() for _ in range(4)]
    sinb = [SB(f"sinb{i}", [128, 512], F32) for i in range(4)]; bsin = [Buf() for _ in range(4)]; s_sin = [P.sem() for _ in range(4)]
    PT = [SB(f"PT{i}", [128, 512], BF16) for i in range(8)]; bPT = [Buf() for _ in range(8)]
    ep = [SB(f"ep{i}", [128, 512], F32) for i in range(6)]; bep = [Buf() for _ in range(6)]
    epq = SB("epq", [128, 512], BF16); bepq = Buf()
    OTs = [SB(f"OTs{i}", [128, 512], BF16) for i in range(2)]; bOTs = [Buf(), Buf()]; s_ot = [P.sem(), P.sem()]
    HKf = SB("HKf", [128, 5, 128], F32); bHKf = Buf(); s_hk = P.sem()
    HKh = [SB(f"HKh{i}", [128, 5, 128], BF16) for i in range(2)]
    HKl = [SB(f"HKl{i}", [128, 5, 128], BF16) for i in range(2)]
    bHK = [Buf(), Buf()]
    hTd_v = hT_d.rearrange("c p s -> p c s")
    win_v = win_g.rearrange("(c p) n -> p c n", p=128)
    ctr = {"s": 0, "pt": 0, "ot": 0, "job": 0}
    deferred = []

    for g in getattr(cfg, 'groups', range(6)):
        isA = g < 2
        nq = 4 if isA else 2
        nk = 1 if isA else 2
        vw = 128 if isA else 256
        koff = 512 if isA else 256
        voff = 640 if isA else 512
        P.op("pool", lambda e, g=g: e.dma_start(out=wg[:], in_=win_v[:, :, g * 768:(g + 1) * 768]), writes=[bwg], dma=s_wg)
        if not isA:
            for hh in range(2):
                h = 2 * (g - 2) + hh
                src = bass.AP(tensor=fext_d.tensor, offset=h * NW + FEXT - 256 - 127, ap=[[1, 128], [128, 5], [1, 128]])
                P.op("sp", lambda e, src=src: e.dma_start(out=HKf[:], in_=src), reads=[bfext_d], writes=[bHKf], dma=s_hk)
                P.op("dve", lambda e: e.tensor_scalar_mul(out=HKf[:], in0=HKf[:], scalar1=8.0), reads=[bHKf], writes=[bHKf])
                P.op("dve", lambda e, hh=hh: e.tensor_copy(out=HKh[hh][:], in_=HKf[:]), reads=[bHKf], writes=[bHK[hh]])
                P.op("dve", lambda e, hh=hh: e.tensor_tensor(out=HKl[hh][:], in0=HKf[:], in1=HKh[hh][:], op=ALU.subtract),
                     reads=[bHKf, bHK[hh]], writes=[bHK[hh]])
        jobs = []
        for tb in range(NTB):
            for ki in range(nk):
                jobs.append((tb, "k", ki))
            if tb < NQB:
                for qi in range(nq):
                    jobs.append((tb, "q", qi))
        n = len(jobs)
        st = {}

        def stage1(i):
            tb, kind, idx = jobs[i]
            hs = tb % 2
            first = (i == 0) or (jobs[i - 1][0] != tb)
            if first:
                P.op("sp", lambda e, hs=hs, tb=tb: e.dma_start(out=hTb2[hs][:], in_=hTd_v[:, :, tb * 512:(tb + 1) * 512]),
                     writes=[bhTb2[hs]], dma=s_hl[hs])
                if isA:
                    h4 = tb % 4
                    P.op("act", lambda e, h4=h4, tb=tb: e.dma_start(out=cosb[h4][:], in_=ropec[:, tb * 512:(tb + 1) * 512]),
                         writes=[bcos[h4]], dma=s_cos[h4])
                    P.op("act", lambda e, h4=h4, tb=tb: e.dma_start(out=sinb[h4][:], in_=ropes[:, tb * 512:(tb + 1) * 512]),
                         writes=[bsin[h4]], dma=s_sin[h4])
            jn = ctr["job"]; ctr["job"] += 1
            pb = jn % 3
            c0 = (koff + idx * 128) if kind == "k" else idx * 128
            for j in range(DC):
                P.op("pe", lambda e, pb=pb, j=j, c0=c0, hs=hs: e.matmul(banks[pb][:], lhsT=wg[:, j, c0:c0 + 128], rhs=hTb2[hs][:, j, :],
                                                                      start=(j == 0), stop=(j == DC - 1)),
                     reads=[bwg, bhTb2[hs]], writes=[bbank[pb]], sig=(j == DC - 1))
            r2 = jn % 2
            P.op("act", lambda e, pb=pb, r2=r2: e.activation(out=sqb[r2][:], in_=banks[pb][:], func=AF.Square),
                 reads=[bbank[pb]], writes=[bsqb[r2]])
            st[i] = (jn, pb, r2)
            last = (i == n - 1) or (jobs[i + 1][0] != tb)
            if last:
                for tc in range(4):
                    for j in range(DC):
                        P.op("pe", lambda e, j=j, tc=tc, hs=hs: e.matmul(banks[7][:, 0:vw], lhsT=hTb2[hs][:, j, tc * 128:(tc + 1) * 128],
                                                                       rhs=wg[:, j, voff:voff + vw], start=(j == 0), stop=(j == DC - 1)),
                             reads=[bwg, bhTb2[hs]], writes=[bbank[7]], sig=(j == DC - 1))
                    P.op("act", lambda e, tb=tb, tc=tc: e.copy(out=VV[:, tb * 4 + tc, 0:vw], in_=banks[7][:, 0:vw]),
                         reads=[bbank[7]], writes=[bVV])

        def stage2(i):
            tb, kind, idx = jobs[i]
            jn, pb, r2 = st[i]
            sb_ = 3 + r2
            P.op("pe", lambda e, sb_=sb_, r2=r2: e.matmul(banks[sb_][:], lhsT=(ONES if isA else BLK64), rhs=sqb[r2][:], start=True, stop=True),
                 reads=[bCM, bsqb[r2]], writes=[bbank[sb_]])
            P.op("act", lambda e, sb_=sb_, r2=r2: e.activation(out=rsb[r2][:], in_=banks[sb_][:], func=AF.Sqrt, bias=epsc[:, 0:1],
                                                               scale=(1.0 / 128 if isA else 1.0 / 64)),
                 reads=[bbank[sb_], beps], writes=[brsb[r2]])
            P.op("dve", lambda e, r2=r2: e.reciprocal(out=rsb[r2][:], in_=rsb[r2][:]), reads=[brsb[r2]], writes=[brsb[r2]])
            gcol = (1 if kind == "k" else 0) + (0 if isA else 2)
            if isA:
                dst, dbuf = qnb[r2][:], bqnb[r2]
            elif kind == "k":
                dst, dbuf = KT[idx][:, tb * 512:(tb + 1) * 512], bKT[idx]
            else:
                dst, dbuf = QT[idx][:, tb * 512:(tb + 1) * 512], bQT[idx]
            P.op("dve", lambda e, pb=pb, r2=r2, gcol=gcol, dst=dst: e.scalar_tensor_tensor(
                out=dst, in0=banks[pb][:], scalar=gsb[:, gcol:gcol + 1], in1=rsb[r2][:], op0=ALU.mult, op1=ALU.mult),
                reads=[bbank[pb], bg_, brsb[r2]], writes=[dbuf])

        def stage3(i):
            if not isA:
                return
            tb, kind, idx = jobs[i]
            jn, pb, r2 = st[i]
            hs = tb % 4
            wb = 5 + r2
            P.op("pe", lambda e, wb=wb, r2=r2: e.matmul(banks[wb][:], lhsT=SWP, rhs=qnb[r2][:], start=True, stop=True),
                 reads=[bCM, bqnb[r2]], writes=[bbank[wb]])
            P.op("dve", lambda e, r2=r2, hs=hs: e.tensor_tensor(out=t1b[r2][:], in0=qnb[r2][:], in1=cosb[hs][:], op=ALU.mult),
                 reads=[bqnb[r2], bcos[hs]], writes=[bt1b[r2]])
            P.op("dve", lambda e, r2=r2, hs=hs, wb=wb: e.tensor_tensor(out=t2b[r2][:], in0=banks[wb][:], in1=sinb[hs][:], op=ALU.mult),
                 reads=[bbank[wb], bsin[hs]], writes=[bt2b[r2]])
            if kind == "k":
                dst, dbuf = KT[idx][:, tb * 512:(tb + 1) * 512], bKT[idx]
            else:
                dst, dbuf = QT[idx][:, tb * 512:(tb + 1) * 512], bQT[idx]
            P.op("dve", lambda e, r2=r2, dst=dst: e.tensor_tensor(out=dst, in0=t1b[r2][:], in1=t2b[r2][:], op=ALU.add),
                 reads=[bt1b[r2], bt2b[r2]], writes=[dbuf])

        for i in range(n + 2):
            if i < n:
                stage1(i)
            if 0 <= i - 1 < n:
                stage2(i - 1)
            if 0 <= i - 2 < n:
                stage3(i - 2)

        scale = (128.0 ** -0.5) if isA else 0.125
        nblk = 0
        for qi in range(nq):
            otile = (4 * g + qi) if isA else (8 + 2 * (g - 2) + qi)
            hglob = None if isA else 2 * (g - 2) + qi
            maps = [(0, 128)] if isA else [(0, 64), (64, 128)]
            ktile = 0 if isA else qi
            vc0 = 0 if isA else qi * 128
            for Qb in range(NQB):
                if isA:
                    accb = [3 + nblk % 2]; sumb = [5 + nblk % 2]
                else:
                    accb = [3, 4]; sumb = [5, 6]
                nblk += 1
                sinfo = {}

                def emitS(kc):
                    for mi, (p0, p1) in enumerate(maps):
                        sbk = ctr["s"] % 3; ctr["s"] += 1
                        ptk = ctr["pt"] % 8; ctr["pt"] += 1
                        jrel = kc - 4 * Qb
                        mixed = (not isA) and (-1 <= jrel <= 4) and not getattr(cfg, 'nobias', False)
                        near = [i for i in range(4) if abs(jrel - i) <= 1] if mixed else []
                        P.op("pe", lambda e, sbk=sbk, p0=p0, p1=p1, kc=kc: e.matmul(
                            banks[sbk][:], lhsT=KT[ktile][p0:p1, kc * 128:(kc + 1) * 128], rhs=QT[qi][p0:p1, Qb * 512:(Qb + 1) * 512],
                            start=True, stop=(len(near) == 0)), reads=[bKT[ktile], bQT[qi]], writes=[bbank[sbk]], sig=(len(near) == 0))
                        for ni, i in enumerate(near):
                            di = jrel - i + 2
                            lastn = ni == len(near) - 1
                            P.op("pe", lambda e, sbk=sbk, i=i, di=di: e.matmul(banks[sbk][:, i * 128:(i + 1) * 128], lhsT=HKh[qi][:, di, :], rhs=JM,
                                                                             start=False, stop=False),
                                 reads=[bHK[qi], bCM], writes=[bbank[sbk]], sig=False)
                            P.op("pe", lambda e, sbk=sbk, i=i, di=di, lastn=lastn: e.matmul(banks[sbk][:, i * 128:(i + 1) * 128], lhsT=HKl[qi][:, di, :], rhs=JM,
                                                                                         start=False, stop=lastn),
                                 reads=[bHK[qi], bCM], writes=[bbank[sbk]], sig=lastn)
                        if isA:
                            P.op("act", lambda e, sbk=sbk, ptk=ptk: e.activation(out=PT[ptk][:], in_=banks[sbk][:], func=AF.Exp, scale=scale),
                                 reads=[bbank[sbk]], writes=[bPT[ptk]])
                        elif getattr(cfg, 'nobias', False):
                            P.op("act", lambda e, sbk=sbk, ptk=ptk: e.activation(out=PT[ptk][:], in_=banks[sbk][:], func=AF.Exp, scale=scale),
                                 reads=[bbank[sbk]], writes=[bPT[ptk]])
                        elif not mixed:
                            fs = 0 if jrel < 0 else 1
                            P.op("act", lambda e, sbk=sbk, ptk=ptk, fs=fs: e.activation(out=PT[ptk][:], in_=banks[sbk][:], func=AF.Exp, scale=scale,
                                                                                    bias=cfar[:, fs, hglob:hglob + 1]),
                                 reads=[bbank[sbk], bcfar], writes=[bPT[ptk]])
                        else:
                            for i in range(4):
                                dd = jrel - i
                                bias_ap = epsc[:, 1:2] if abs(dd) <= 1 else cfar[:, (0 if dd < 0 else 1), hglob:hglob + 1]
                                P.op("act", lambda e, sbk=sbk, ptk=ptk, i=i, bias_ap=bias_ap: e.activation(
                                    out=PT[ptk][:, i * 128:(i + 1) * 128], in_=banks[sbk][:, i * 128:(i + 1) * 128], func=AF.Exp, scale=scale, bias=bias_ap),
                                    reads=[bbank[sbk], bcfar, beps], writes=[bPT[ptk]])
                        sinfo[(kc, mi)] = ptk

                def emitPV(kc):
                    for mi in range(len(maps)):
                        ptk = sinfo[(kc, mi)]
                        P.op("pe", lambda e, mi=mi, ptk=ptk, kc=kc: e.matmul(banks[accb[mi]][:], lhsT=VV[:, kc, vc0:vc0 + 128], rhs=PT[ptk][:],
                                                                           start=(kc == 0), stop=(kc == NTC - 1)),
                             reads=[bVV, bPT[ptk]], writes=[bbank[accb[mi]]], sig=False)
                        P.op("pe", lambda e, mi=mi, ptk=ptk, kc=kc: e.matmul(banks[sumb[mi]][:], lhsT=ONES, rhs=PT[ptk][:],
                                                                           start=(kc == 0), stop=(kc == NTC - 1)),
                             reads=[bCM, bPT[ptk]], writes=[bbank[sumb[mi]]], sig=True)

                for step in range(NTC + 2):
                    if step < NTC:
                        emitS(step)
                    if step == 3 and deferred:
                        for fdef in deferred:
                            fdef()
                        deferred.clear()
                    if step >= 2:
                        emitPV(step - 2)
                osl = ctr["ot"] % 2; ctr["ot"] += 1
                if isA:
                    P.op("dve", lambda e, sb_=sumb[0]: e.reciprocal(out=ep[0][:], in_=banks[sb_][:]), reads=[bbank[sumb[0]]], writes=[bep[0]])
                    P.op("dve", lambda e, ab=accb[0], osl=osl: e.tensor_tensor(out=OTs[osl][:], in0=banks[ab][:], in1=ep[0][:], op=ALU.mult),
                         reads=[bbank[accb[0]], bep[0]], writes=[bOTs[osl]])
                    P.op("sp", lambda e, osl=osl, otile=otile, Qb=Qb: e.dma_start(out=OT_d[otile, :, Qb * 512:(Qb + 1) * 512], in_=OTs[osl][:]),
                         reads=[bOTs[osl]], dma=s_ot[osl])
                else:
                    P.op("dve", lambda e: e.reciprocal(out=ep[0][:], in_=banks[5][:]), reads=[bbank[5]], writes=[bep[0]])
                    P.op("dve", lambda e: e.tensor_tensor(out=ep[1][:], in0=banks[3][:], in1=ep[0][:], op=ALU.mult),
                         reads=[bbank[3], bep[0]], writes=[bep[1]])
                    P.op("dve", lambda e: e.reciprocal(out=ep[2][:], in_=banks[6][:]), reads=[bbank[6]], writes=[bep[2]])
                    P.op("dve", lambda e: e.tensor_tensor(out=ep[3][:], in0=banks[4][:], in1=ep[2][:], op=ALU.mult),
                         reads=[bbank[4], bep[2]], writes=[bep[3]])
                    P.op("dve", lambda e: e.scalar_tensor_tensor(out=ep[4][:], in0=ep[3][:], scalar=NEGLAM, in1=ep[1][:], op0=ALU.mult, op1=ALU.add),
                         reads=[bep[3], bep[1], blam], writes=[bep[4]])
                    P.op("act", lambda e: e.activation(out=epq[:], in_=ep[4][:], func=AF.Square), reads=[bep[4]], writes=[bepq])

                    def tail(osl=osl, otile=otile, Qb=Qb):
                        P.op("pe", lambda e: e.matmul(banks[7][:], lhsT=ONES, rhs=epq[:], start=True, stop=True), reads=[bCM, bepq], writes=[bbank[7]])
                        P.op("act", lambda e: e.activation(out=ep[5][:], in_=banks[7][:], func=AF.Sqrt, bias=epsc[:, 0:1], scale=1.0 / 128),
                             reads=[bbank[7], beps], writes=[bep[5]])
                        P.op("dve", lambda e: e.reciprocal(out=ep[5][:], in_=ep[5][:]), reads=[bep[5]], writes=[bep[5]])
                        P.op("dve", lambda e, osl=osl: e.scalar_tensor_tensor(out=OTs[osl][:], in0=ep[4][:], scalar=SUBG, in1=ep[5][:], op0=ALU.mult, op1=ALU.mult),
                             reads=[bep[4], bep[5], bg_], writes=[bOTs[osl]])
                        P.op("sp", lambda e, osl=osl, otile=otile, Qb=Qb: e.dma_start(out=OT_d[otile, :, Qb * 512:(Qb + 1) * 512], in_=OTs[osl][:]),
                             reads=[bOTs[osl]], dma=s_ot[osl])
                    deferred.append(tail)
        for fdef in deferred:
            fdef()
        deferred.clear()
    if "dbgQ" in debug:
        dq = dt("dbgQ", [128, NQ], BF16, kind="ExternalOutput").ap()
        dk = dt("dbgK", [128, S], BF16, kind="ExternalOutput").ap()
        dv = dt("dbgV", [128, NTC * 256], BF16, kind="ExternalOutput").ap()
        sdbg = P.sem()
        de = dt("dbgE", [6, 128, 512], F32, kind="ExternalOutput").ap()
        for i6 in range(6):
            P.op("sp", lambda e, i6=i6: e.dma_start(out=de[i6], in_=ep[i6][:]), reads=[bep[i6]], dma=sdbg)
        dp = dt("dbgP", [4, 128, 512], BF16, kind="ExternalOutput").ap()
        for i4 in range(4):
            P.op("sp", lambda e, i4=i4: e.dma_start(out=dp[i4], in_=PT[i4][:]), reads=[bPT[i4]], dma=sdbg)
        P.op("sp", lambda e: e.dma_start(out=dq, in_=QT[1][:]), reads=[bQT[1]], dma=sdbg)
        P.op("sp", lambda e: e.dma_start(out=dk, in_=KT[1][:]), reads=[bKT[1]], dma=sdbg)
        P.op("sp", lambda e: e.dma_start(out=dv, in_=VV[:].rearrange("p a b -> p (a b)")), reads=[bVV], dma=sdbg)
    end_phase()
    if stop_after == "B":
        return nc, P
    IOA = bass.IndirectOffsetOnAxis
    dest_i = SBG("dest_i", [128, NQC, 4], I32); bdest = Buf()
    dsc_i = SBG("dsc_i", [128, NQC, 4], I32)
    gk = SBG("gk", [128, NQC, 4], F32); bgk = Buf()
    wo = SB("wo", [128, 16, D], BF16); bwo = Buf(); s_wo = P.sem()
    P.op("pool", lambda e: e.dma_start(out=wo[:], in_=w_out.rearrange("(k p) n -> p k n", p=128)), writes=[bwo], dma=s_wo)
    mrep = SB("mrep", [128, 3, D], F32); bmr = Buf(); s_mr = P.sem()
    for i3, seg in enumerate((2, 3, 4)):
        P.op("sp", lambda e, i3=i3, seg=seg: e.dma_start(out=mrep[:, i3, :], in_=mod_d[:, seg * D:(seg + 1) * D]), writes=[bmr], dma=s_mr)
    wr = SB("wr", [128, DC, E], BF16); bwr = Buf(); s_wr = P.sem()
    P.op("pool", lambda e: e.dma_start(out=wr[:], in_=w_router.rearrange("(c p) n -> p c n", p=128)), writes=[bwr], dma=s_wr)
    brr = SB("brr", [128, E], F32); iot = SB("iot", [128, E], F32); ecap = SB("ecap", [128, E], F32); bconst = Buf(); s_cn = P.sem()
    P.op("sp", lambda e: e.dma_start(out=brr[:], in_=brout_rep), writes=[bconst], dma=s_cn)
    P.op("sp", lambda e: e.dma_start(out=iot[:], in_=iota_e), writes=[bconst], dma=s_cn)
    prw = SB("prw", [128, E], F32)
    P.op("sp", lambda e: e.dma_start(out=prw[:], in_=padrow), writes=[bconst], dma=s_cn)
    P.op("dve", lambda e: e.tensor_scalar_mul(out=ecap[:], in0=iot[:], scalar1=float(CAP)), reads=[bconst], writes=[bconst])
    base = SB("base", [128, E], F32); bbase = Buf()
    P.op("dve", lambda e: e.memset(base[:], 0.0), writes=[bbase])
    zt = SB("zt", [128, D], BF16); bzt = Buf(); s_z = P.sem()
    P.op("dve", lambda e: e.memset(zt[:], 0.0), writes=[bzt])
    zf = SB("zf", [128, D], F32); bzf = Buf()
    P.op("dve", lambda e: e.memset(zf[:], 0.0), writes=[bzf])
    for r0 in range(E * CAP, NROW, 128):
        P.op("sp", lambda e, r0=r0: e.dma_start(out=ys_d[r0:r0 + 128, :], in_=zf[:]), reads=[bzf], dma=s_z)
    bhs_d = Buf()
    for r0 in range(0, NROW, 128):
        nr = min(128, NROW - r0)
        P.op("sp", lambda e, r0=r0, nr=nr: e.dma_start(out=hs_d[r0:r0 + nr, :], in_=zt[0:nr, :]), reads=[bzt], dma=s_z)
    OTc = SB("OTc", [128, 16, 128], BF16); bOTc = Buf(); s_oc = P.sem()
    xq = SB("xq", [128, D], F32); bxq = Buf(); s_xq = P.sem()
    x1 = SB("x1", [128, D], F32); bx1 = Buf(); s_x1 = P.sem()
    tmpc = SB("tmpc", [128, D], F32); btmpc = Buf()
    junkc = SB("junkc", [128, D], BF16); bjc = Buf()
    ssc = SB("ssc", [128, 2], F32); bssc = Buf()
    h2 = SB("h2", [128, D], BF16); bh2 = Buf(); s_sc = P.sem()
    h2T = SB("h2T", [128, DC, 128], BF16); bh2T = Buf()
    lg = SB("lg", [128, E], F32); blg = Buf()
    mx8 = SB("mx8", [128, 8], F32); negm = SB("negm", [128, 1], F32)
    msk = SB("msk", [128, E], F32); mskb = SB("mskb", [128, E], BF16)
    ex = SB("ex", [128, E], F32); gat = SB("gat", [128, E], F32); rsum = SB("rsum", [128, 2], F32)
    dst = SB("dst", [128, E], F32); oh = SB("oh", [128, E], F32); prod = SB("prod", [128, E], F32)
    dk = SB("dk", [128, 4], F32)
    dks = SB("dks", [128, 4], F32)
    dsts = SB("dsts", [128, E], F32)
    ovf = SB("ovf", [128, E], F32)
    brt = Buf()
    OTv = OT_d.rearrange("k p s -> p k s")
    for t in range(NQC):
        P.op("sp", lambda e, t=t: e.dma_start(out=OTc[:], in_=OTv[:, :, t * 128:(t + 1) * 128]), writes=[bOTc], dma=s_oc)
        P.op("sp", lambda e, t=t: e.dma_start(out=xq[:], in_=xkv[t * 128:(t + 1) * 128, :]), writes=[bxq], dma=s_xq)
        for cb in range(D // 512):
            bk = cb % 2
            for k in range(16):
                P.op("pe", lambda e, bk=bk, k=k, cb=cb: e.matmul(banks[bk][:], lhsT=OTc[:, k, :], rhs=wo[:, k, cb * 512:(cb + 1) * 512],
                                                               start=(k == 0), stop=(k == 15)),
                     reads=[bOTc, bwo], writes=[bbank[bk]], sig=(k == 15))
            P.op("dve", lambda e, bk=bk, cb=cb: e.tensor_tensor(out=tmpc[:, cb * 512:(cb + 1) * 512], in0=banks[bk][:],
                                                                in1=mrep[:, 0, cb * 512:(cb + 1) * 512], op=ALU.mult),
                 reads=[bbank[bk], bmr], writes=[btmpc])
            P.op("dve", lambda e, cb=cb: e.tensor_tensor(out=x1[:, cb * 512:(cb + 1) * 512], in0=tmpc[:, cb * 512:(cb + 1) * 512],
                                                         in1=xq[:, cb * 512:(cb + 1) * 512], op=ALU.add),
                 reads=[btmpc, bxq], writes=[bx1])
        P.op("sp", lambda e, t=t: e.dma_start(out=out[t * 128:(t + 1) * 128, :], in_=x1[:]), reads=[bx1], dma=s_x1)
        P.op("act", lambda e: e.activation(out=junkc[:], in_=x1[:], func=AF.Square, accum_out=ssc[:, 0:1]), reads=[bx1], writes=[bjc, bssc])
        P.op("act", lambda e: e.activation(out=ssc[:, 1:2], in_=ssc[:, 0:1], func=AF.Sqrt, bias=epsc[:, 0:1], scale=1.0 / D),
             reads=[bssc, beps], writes=[bssc])
        P.op("dve", lambda e: e.reciprocal(out=ssc[:, 1:2], in_=ssc[:, 1:2]), reads=[bssc], writes=[bssc])
        P.op("dve", lambda e: e.scalar_tensor_tensor(out=tmpc[:], in0=x1[:], scalar=ssc[:, 1:2], in1=mrep[:, 2, :], op0=ALU.mult, op1=ALU.mult),
             reads=[bx1, bssc, bmr], writes=[btmpc])
        P.op("dve", lambda e: e.tensor_tensor(out=h2[:], in0=tmpc[:], in1=mrep[:, 1, :], op=ALU.add), reads=[btmpc, bmr], writes=[bh2])
        for j in range(DC):
            bk = 2 + j % 2
            pv = banks[bk][:].bitcast(BF16)
            P.op("pe", lambda e, j=j, pv=pv: e.transpose(out=pv[:, 0:128], in_=h2[:, j * 128:(j + 1) * 128], identity=IDENT),
                 reads=[bh2, bCM], writes=[bbank[bk]])
            P.op("act", lambda e, j=j, pv=pv: e.copy(out=h2T[:, j, :], in_=pv[:, 0:128]), reads=[bbank[bk]], writes=[bh2T])
        for j in range(DC):
            P.op("pe", lambda e, j=j: e.matmul(banks[4][:, 0:E], lhsT=h2T[:, j, :], rhs=wr[:, j, :], start=(j == 0), stop=(j == DC - 1)),
                 reads=[bh2T, bwr], writes=[bbank[4]], sig=(j == DC - 1))
        R_ = dict(reads=[brt], writes=[brt])
        P.op("dve", lambda e: e.tensor_tensor(out=lg[:], in0=banks[4][:, 0:E], in1=brr[:], op=ALU.add), reads=[bbank[4], bconst, brt], writes=[brt])
        P.op("dve", lambda e: e.max(out=mx8[:], in_=lg[:]), **R_)
        P.op("dve", lambda e: e.tensor_scalar(out=msk[:], in0=lg[:], scalar1=mx8[:, 3:4], scalar2=None, op0=ALU.is_ge), **R_)
        P.op("dve", lambda e: e.tensor_copy(out=mskb[:], in_=msk[:]), **R_)
        P.op("dve", lambda e: e.tensor_scalar_mul(out=negm[:], in0=mx8[:, 0:1], scalar1=-1.0), **R_)
        P.op("act", lambda e: e.activation(out=ex[:], in_=lg[:], func=AF.Exp, bias=negm[:, 0:1], scale=1.0), **R_)
        P.op("dve", lambda e: e.tensor_tensor(out=ex[:], in0=ex[:], in1=msk[:], op=ALU.mult), **R_)
        P.op("dve", lambda e: e.reduce_sum(out=rsum[:, 0:1], in_=ex[:], axis=AX.X), **R_)
        P.op("dve", lambda e: e.reciprocal(out=rsum[:, 1:2], in_=rsum[:, 0:1]), **R_)
        P.op("dve", lambda e: e.tensor_scalar(out=gat[:], in0=ex[:], scalar1=rsum[:, 1:2], scalar2=None, op0=ALU.mult), **R_)
        P.op("pe", lambda e: e.matmul(banks[5][:, 0:E], lhsT=TRI, rhs=mskb[:], start=True, stop=True), reads=[bCM, brt], writes=[bbank[5]])
        P.op("pe", lambda e: e.matmul(banks[5][:, E:2 * E], lhsT=ONES, rhs=mskb[:], start=True, stop=True), reads=[bCM, brt], writes=[bbank[5]])
        P.op("dve", lambda e: e.tensor_tensor(out=dst[:], in0=banks[5][:, 0:E], in1=base[:], op=ALU.add), reads=[bbank[5], bbase, brt], writes=[brt])
        P.op("dve", lambda e: e.tensor_scalar(out=ovf[:], in0=dst[:], scalar1=float(CAP), scalar2=None, op0=ALU.is_ge), **R_)
        P.op("dve", lambda e: e.tensor_tensor(out=dst[:], in0=dst[:], in1=ecap[:], op=ALU.add), reads=[bconst, brt], writes=[brt])
        P.op("dve", lambda e: e.tensor_tensor(out=prod[:], in0=prw[:], in1=dst[:], op=ALU.subtract), reads=[bconst, brt], writes=[brt])
        P.op("dve", lambda e: e.tensor_tensor(out=prod[:], in0=prod[:], in1=ovf[:], op=ALU.mult), **R_)
        P.op("dve", lambda e: e.tensor_tensor(out=dst[:], in0=dst[:], in1=prod[:], op=ALU.add), **R_)
        P.op("dve", lambda e: e.scalar_tensor_tensor(out=dsts[:], in0=ovf[:], scalar=1.0e6, in1=dst[:], op0=ALU.mult, op1=ALU.add), **R_)
        P.op("dve", lambda e: e.tensor_tensor(out=base[:], in0=banks[5][:, E:2 * E], in1=base[:], op=ALU.add), reads=[bbank[5], bbase], writes=[bbase])
        for k in range(4):
            P.op("dve", lambda e, k=k: e.tensor_scalar(out=oh[:], in0=lg[:], scalar1=mx8[:, k:k + 1], scalar2=None, op0=ALU.is_equal), **R_)
            P.op("dve", lambda e: e.tensor_tensor(out=prod[:], in0=oh[:], in1=dst[:], op=ALU.mult), **R_)
            P.op("dve", lambda e, k=k: e.reduce_sum(out=dk[:, k:k + 1], in_=prod[:], axis=AX.X), **R_)
            P.op("dve", lambda e: e.tensor_tensor(out=prod[:], in0=oh[:], in1=dsts[:], op=ALU.mult), **R_)
            P.op("dve", lambda e, k=k: e.reduce_sum(out=dks[:, k:k + 1], in_=prod[:], axis=AX.X), **R_)
            P.op("dve", lambda e: e.tensor_tensor(out=prod[:], in0=oh[:], in1=gat[:], op=ALU.mult), **R_)
            P.op("dve", lambda e, k=k, t=t: e.reduce_sum(out=gk[:, t, k:k + 1], in_=prod[:], axis=AX.X), reads=[brt], writes=[brt, bgk])
        P.op("dve", lambda e, t=t: e.tensor_copy(out=dest_i[:, t, :], in_=dk[:]), reads=[brt], writes=[bdest])
        P.op("dve", lambda e, t=t: e.tensor_copy(out=dsc_i[:, t, :], in_=dks[:]), reads=[brt], writes=[bdest])
        for k in range(4):
            P.op("pool", lambda e, t=t, k=k: e.indirect_dma_start(out=hs_d[:, :], out_offset=IOA(ap=dsc_i[:, t, k:k + 1], axis=0),
                                                                  in_=h2[:, :], in_offset=None, bounds_check=NROW - 1, oob_is_err=False),
                 reads=[bh2, bdest], extra=[(s_z, s_z.n)], dma=s_sc)
    end_phase()
    if stop_after == "C":
        return nc, P

    bgu = SB("bgu", [128, E * 2 * FC], F32); bbgu = Buf(); s_bg = P.sem()
    P.op("sp", lambda e: e.dma_start(out=bgu[:], in_=bgu_fm), writes=[bbgu], dma=s_bg)
    rows = [SB(f"rows{i}", [128, D], BF16) for i in range(2)]; brow = [Buf(), Buf()]; s_row = [P.sem(), P.sem()]
    XT = SB("XT", [128, DC, CAP], BF16); bXT = Buf()
    wgb = [SB(f"wgb{i}", [128, DC, 512], BF16) for i in range(2)]; bwgb = [Buf(), Buf()]; s_wgb = [P.sem(), P.sem()]
    wub = [SB(f"wub{i}", [128, DC, 512], BF16) for i in range(2)]; bwub = [Buf(), Buf()]; s_wub = [P.sem(), P.sem()]
    wdb = [SB(f"wdb{i}", [128, FC, 512], BF16) for i in range(2)]; bwdb = [Buf(), Buf()]; s_wdb = [P.sem(), P.sem()]
    actT = SB("actT", [128, FC, CAP], BF16); bact = Buf()
    NH = 1 if CAP <= 512 else 2
    HW_ = CAP // NH
    gt = SB("gt", [128, HW_], F32); sg = SB("sg", [128, HW_], F32); u1 = SB("u1", [128, HW_], F32); glu = SB("glu", [128, HW_], F32)
    bgt = Buf(); bsg = Buf(); bu1 = Buf(); bglu = Buf()
    bdn = SB("bdn", [128, D], F32); bbdn = Buf(); s_bdn = P.sem()
    chunks = [(s0, min(128, CAP - s0)) for s0 in range(0, CAP, 128)]
    ybuf = [SB(f"ybuf{i}", [128, 512], F32) for i in range(4)]; bybuf = [Buf() for _ in range(4)]; s_y = [P.sem() for _ in range(4)]
    ri = 0; wi = 0; di = 0; yi = 0; gi_ = 0
    NFB = DFF // 512 if DFF >= 512 else 1
    FBW = min(512, DFF)
    for ex_ in range(E):
        for ci, (s0, n) in enumerate(chunks):
            rs = ri % 2; ri += 1
            P.op("sp", lambda e, rs=rs, s0=s0, n=n, ex_=ex_: e.dma_start(out=rows[rs][0:n, :], in_=hs_d[ex_ * CAP + s0:ex_ * CAP + s0 + n, :]),
                 writes=[brow[rs]], dma=s_row[rs])
            for j in range(DC):
                bk = 6 + j % 2
                pv = banks[bk][:].bitcast(BF16)
                P.op("pe", lambda e, rs=rs, j=j, n=n, pv=pv: e.transpose(out=pv[:, 0:n], in_=rows[rs][0:n, j * 128:(j + 1) * 128], identity=IDENT[0:n, 0:n]),
                     reads=[brow[rs], bCM], writes=[bbank[bk]])
                P.op("act", lambda e, j=j, s0=s0, n=n, pv=pv: e.copy(out=XT[:, j, s0:s0 + n], in_=pv[:, 0:n]), reads=[bbank[bk]], writes=[bXT])
        P.op("sp", lambda e, ex_=ex_: e.dma_start(out=bdn[:], in_=bass.AP(tensor=b_down.tensor, offset=ex_ * D, ap=[[0, 128], [1, D]])),
             writes=[bbdn], dma=s_bdn)
        for fb in range(NFB):
            ws = wi % 2; wi += 1
            P.op("pool", lambda e, ws=ws, fb=fb, ex_=ex_: e.dma_start(out=wgb[ws][:, :, 0:FBW], in_=w_gu[ex_].rearrange("(c p) n -> p c n", p=128)[:, :, fb * FBW:(fb + 1) * FBW]),
                 writes=[bwgb[ws]], dma=s_wgb[ws])
            P.op("pool", lambda e, ws=ws, fb=fb, ex_=ex_: e.dma_start(out=wub[ws][:, :, 0:FBW], in_=w_gu[ex_].rearrange("(c p) n -> p c n", p=128)[:, :, DFF + fb * FBW:DFF + (fb + 1) * FBW]),
                 writes=[bwub[ws]], dma=s_wub[ws])
            for f4 in range(FBW // 128):
                fc = fb * (FBW // 128) + f4
                cg = ex_ * 2 * FC + fc
                cu = ex_ * 2 * FC + FC + fc
                for hh in range(NH):
                    h0 = hh * HW_
                    gb = (gi_ % 2) * 2; gi_ += 1
                    for j in range(DC):
                        P.op("pe", lambda e, ws=ws, j=j, f4=f4, gb=gb, h0=h0: e.matmul(banks[gb][:, 0:HW_], lhsT=wgb[ws][:, j, f4 * 128:(f4 + 1) * 128],
                                                                                      rhs=XT[:, j, h0:h0 + HW_], start=(j == 0), stop=(j == DC - 1)),
                             reads=[bwgb[ws], bXT], writes=[bbank[gb]], sig=(j == DC - 1))
                    for j in range(DC):
                        P.op("pe", lambda e, ws=ws, j=j, f4=f4, gb=gb, h0=h0: e.matmul(banks[gb + 1][:, 0:HW_], lhsT=wub[ws][:, j, f4 * 128:(f4 + 1) * 128],
                                                                                      rhs=XT[:, j, h0:h0 + HW_], start=(j == 0), stop=(j == DC - 1)),
                             reads=[bwub[ws], bXT], writes=[bbank[gb + 1]], sig=(j == DC - 1))
                    P.op("dve", lambda e, cg=cg, gb=gb: e.tensor_scalar(out=gt[:], in0=banks[gb][:, 0:HW_], scalar1=bgu[:, cg:cg + 1], scalar2=LIM, op0=ALU.add, op1=ALU.min),
                         reads=[bbank[gb], bbgu], writes=[bgt])
                    P.op("act", lambda e: e.activation(out=sg[:], in_=gt[:], func=AF.Sigmoid, scale=ALPHA), reads=[bgt], writes=[bsg])
                    P.op("dve", lambda e, cu=cu, gb=gb: e.tensor_scalar(out=u1[:], in0=banks[gb + 1][:, 0:HW_], scalar1=bgu[:, cu:cu + 1], scalar2=LIM, op0=ALU.add, op1=ALU.min),
                         reads=[bbank[gb + 1], bbgu], writes=[bu1])
                    P.op("dve", lambda e: e.tensor_scalar(out=u1[:], in0=u1[:], scalar1=-LIM, scalar2=1.0, op0=ALU.max, op1=ALU.add), reads=[bu1], writes=[bu1])
                    P.op("dve", lambda e: e.tensor_tensor(out=glu[:], in0=gt[:], in1=sg[:], op=ALU.mult), reads=[bgt, bsg], writes=[bglu])
                    P.op("dve", lambda e, fc=fc, h0=h0: e.tensor_tensor(out=actT[:, fc, h0:h0 + HW_], in0=u1[:], in1=glu[:], op=ALU.mult), reads=[bu1, bglu], writes=[bact])
        for cb in range(D // 512):
            ds_ = di % 2; di += 1
            P.op("pool", lambda e, ds_=ds_, cb=cb, ex_=ex_: e.dma_start(out=wdb[ds_][:], in_=w_down[ex_].rearrange("(c p) n -> p c n", p=128)[:, :, cb * 512:(cb + 1) * 512]),
                 writes=[bwdb[ds_]], dma=s_wdb[ds_])
            for ci, (s0, n) in enumerate(chunks):
                bk = 4 + (ci % 2)
                ys = yi % 4; yi += 1
                for fc in range(FC):
                    P.op("pe", lambda e, bk=bk, fc=fc, s0=s0, n=n, ds_=ds_: e.matmul(banks[bk][0:n, :], lhsT=actT[:, fc, s0:s0 + n], rhs=wdb[ds_][:, fc, :],
                                                                                  start=(fc == 0), stop=(fc == FC - 1)),
                         reads=[bact, bwdb[ds_]], writes=[bbank[bk]], sig=(fc == FC - 1))
                P.op("dve", lambda e, bk=bk, ys=ys, n=n, cb=cb: e.tensor_tensor(out=ybuf[ys][0:n, :], in0=banks[bk][0:n, :],
                                                                            in1=bdn[0:n, cb * 512:(cb + 1) * 512], op=ALU.add),
                     reads=[bbank[bk], bbdn], writes=[bybuf[ys]])
                P.op("sp", lambda e, ys=ys, s0=s0, n=n, ex_=ex_, cb=cb: e.dma_start(out=ys_d[ex_ * CAP + s0:ex_ * CAP + s0 + n, cb * 512:(cb + 1) * 512], in_=ybuf[ys][0:n, :]),
                     reads=[bybuf[ys]], dma=s_y[ys])
    end_phase()
    if stop_after == "D":
        return nc, P

    gfr = SB("gfr", [128, D], F32); bgfr = Buf(); s_gf = P.sem()
    P.op("sp", lambda e: e.dma_start(out=gfr[:], in_=mod_d[:, 5 * D:6 * D]), writes=[bgfr], dma=s_gf)
    Yk = [SB(f"Yk{i}", [128, D], F32) for i in range(4)]; bYk = [Buf() for _ in range(4)]; s_yk = [P.sem() for _ in range(4)]
    x1e = SB("x1e", [128, D], F32); bx1e = Buf(); s_x1e = P.sem()
    acc = SB("acc", [128, D], F32); bacc = Buf()
    fin = SB("fin", [128, D], F32); bfin = Buf(); s_fin = P.sem()
    for t in range(NQC):
        P.op("sp", lambda e, t=t: e.dma_start(out=x1e[:], in_=out[t * 128:(t + 1) * 128, :]), writes=[bx1e], dma=s_x1e)
        for k in range(4):
            P.op("pool", lambda e, t=t, k=k: e.indirect_dma_start(out=Yk[k][:, :], out_offset=None, in_=ys_d[:, :],
                                                                  in_offset=IOA(ap=dest_i[:, t, k:k + 1], axis=0)),
                 reads=[bdest], writes=[bYk[k]], dma=s_yk[k])
        P.op("dve", lambda e, t=t: e.tensor_scalar(out=acc[:], in0=Yk[0][:], scalar1=gk[:, t, 0:1], scalar2=None, op0=ALU.mult),
             reads=[bYk[0], bgk], writes=[bacc])
        for k in range(1, 4):
            P.op("dve", lambda e, t=t, k=k: e.scalar_tensor_tensor(out=acc[:], in0=Yk[k][:], scalar=gk[:, t, k:k + 1], in1=acc[:], op0=ALU.mult, op1=ALU.add),
                 reads=[bYk[k], bgk, bacc], writes=[bacc])
        P.op("dve", lambda e: e.tensor_tensor(out=fin[:], in0=acc[:], in1=gfr[:], op=ALU.mult), reads=[bacc, bgfr], writes=[bfin])
        P.op("dve", lambda e: e.tensor_tensor(out=fin[:], in0=fin[:], in1=x1e[:], op=ALU.add), reads=[bfin, bx1e], writes=[bfin])
        P.op("sp", lambda e, t=t: e.dma_start(out=out[t * 128:(t + 1) * 128, :], in_=fin[:]), reads=[bfin, bx1e], dma=s_fin)
    end_phase()
    return nc, P


def _rel_bucket_np(rp):
    nb = 16
    max_exact = 8
    ret = np.where(rp > 0, nb, 0)
    n = np.abs(rp)
    nf = np.maximum(n, 1).astype(np.float32)
    large = max_exact + (np.log(nf / np.float32(max_exact)) / np.float32(math.log(128 / max_exact))
                         * np.float32(nb - max_exact)).astype(np.int32)
    large = np.minimum(large, nb - 1)
    return ret + np.where(n < max_exact, n, large)


def _static_tables(cfg, hf):
    S = cfg.S
    order = np.arange(S) if hf == 0 else np.arange(S)[::-1].copy()
    row = (order // 64).astype(np.float32)
    col = (order % 64).astype(np.float32)
    inv = (np.float32(10000.0) ** (-np.arange(0, 64, 2, dtype=np.float32) / np.float32(64))).astype(np.float32)
    ang = np.concatenate([row[:, None] * inv, col[:, None] * inv], axis=-1).astype(np.float32)
    c = np.cos(ang).astype(np.float32).T
    s = np.sin(ang).astype(np.float32).T
    ropec = np.ascontiguousarray(np.concatenate([c, c], 0))
    ropes = np.ascontiguousarray(np.concatenate([-s, s], 0))
    r = np.arange(-FEXT, FEXT + 2)
    sgn = 1 if hf == 0 else -1
    bk = _rel_bucket_np(sgn * r)
    ohr = (bk[None, :] == np.arange(32)[:, None]).astype(np.float32)
    cm = np.zeros((6, 128, 128), np.float32)
    cm[0] = np.eye(128)
    cm[1] = 1.0
    cm[2] = np.eye(128)[::-1]
    cm[3] = np.roll(np.eye(128), 64, axis=0)
    cm[4] = np.triu(np.ones((128, 128)), 1)
    cm[5, :64, :64] = 1.0
    cm[5, 64:, 64:] = 1.0
    cmat = np.ascontiguousarray(cm.transpose(1, 0, 2).reshape(128, 6 * 128))
    return order, ropec, ropes, ohr, cmat


def _win_cols():
    perm = np.concatenate([np.arange(0, 128, 2), np.arange(1, 128, 2)])
    cols = []
    for a in range(2):
        for h in range(4 * a, 4 * a + 4):
            cols.append(h * 128 + perm)
        cols.append(1024 + a * 128 + perm)
        cols.append(1280 + a * 128 + np.arange(128))
    for bg in range(4):
        for base in (1536, 2560, 3584):
            for h in (2 * bg, 2 * bg + 1):
                cols.append(base + h * 128 + np.arange(128))
    return np.concatenate(cols), perm


def prep_inputs(cfg, inp):
    D, S, E, DFF, DC, FC, NQ = cfg.D, cfg.S, cfg.E, cfg.DFF, cfg.DC, cfg.FC, cfg.NQ
    f = lambda a: np.ascontiguousarray(np.asarray(a, dtype=np.float32))
    rep = lambda v: np.ascontiguousarray(np.broadcast_to(np.asarray(v, np.float32).reshape(1, -1), (128, np.asarray(v).size)))
    cols, perm = _win_cols()
    shared = {
        "w_ada": f(inp["w_ada"][0]),
        "bada_rep": rep(inp["b_ada"][0]),
        "gn_rep": rep(np.concatenate([inp["norm_attn"][0], inp["norm_ffn"][0]])),
        "win_g": f(np.asarray(inp["w_in"][0])[:, cols]),
        "w_out": f(inp["w_out"][0]),
        "gains": f(np.stack([np.asarray(inp["a_q_norm"][0])[perm], np.asarray(inp["a_k_norm"][0])[perm],
                             np.tile(np.asarray(inp["b_q_norm"][0]), 2), np.tile(np.asarray(inp["b_k_norm"][0]), 2),
                             np.asarray(inp["b_subln"][0])] + [np.zeros(128, np.float32)] * 3, axis=1)),
        "lam_rep": rep(np.concatenate([inp["lambda_q1"][0], inp["lambda_k1"][0], inp["lambda_q2"][0], inp["lambda_k2"][0]])),
        "relT": f(np.concatenate([inp["rel_bias_table"], inp["rel_bias_table"]], axis=1)),
        "w_router": f(inp["w_router"][0]),
        "brout_rep": rep(inp["b_router"][0]),
        "w_gu": f(inp["w_gu"][0]),
        "bgu_fm": f(np.asarray(inp["b_gu"][0]).reshape(E, 2 * FC, 128).transpose(2, 0, 1).reshape(128, E * 2 * FC)),
        "w_down": f(inp["w_down"][0]),
        "b_down": f(inp["b_down"][0]),
        "iota_e": rep(np.arange(E)),
        "padrow": np.ascontiguousarray(np.broadcast_to((E * cfg.CAP + np.arange(128, dtype=np.float32))[:, None], (128, E))),
    }
    maps, orders = [], []
    tabs = [_static_tables(cfg, hf) for hf in range(2)]
    x = np.asarray(inp["x"])
    c = np.asarray(inp["c"])
    for core in range(8):
        b, hf = core // 2, core % 2
        order, ropec, ropes, ohr, cmat = tabs[hf]
        m = dict(shared)
        m["xkv"] = f(x[b][order])
        m["c_fm"] = f(c[b].reshape(DC, 128).T)
        m["ropec"], m["ropes"], m["ohr"], m["cmat"] = ropec, ropes, ohr, cmat
        maps.append(m)
        orders.append(order)
    return maps, orders


_CACHE = {}


def kernel(**inputs):
    cfg = Cfg()
    if "nc" not in _CACHE:
        _CACHE["nc"] = build(cfg)[0]
    nc = _CACHE["nc"]
    maps, orders = prep_inputs(cfg, inputs)
    res = run_bass_kernel_spmd(nc, maps, core_ids=list(range(8)))
    out = np.zeros((4, cfg.S, cfg.D), np.float32)
    for core in range(8):
        b = core // 2
        out[b, orders[core][:cfg.NQ]] = res.results[core]["out"]
    return out
```

```python
import math
from contextlib import ExitStack
import numpy as np
import concourse.bass as bass
import concourse.mybir as mybir
from concourse.bass_utils import run_bass_kernel_spmd

F32 = mybir.dt.float32
BF16 = mybir.dt.bfloat16
I32 = mybir.dt.int32
AF = mybir.ActivationFunctionType
ALU = mybir.AluOpType
AX = mybir.AxisListType

EPS = 1e-6
HEAD = 128
TOPK = 4
LIM = 7.0
ALPHA = 1.702
LAM_INIT = 0.8 - 0.6 * math.exp(0.0)
FEXT = 640
ENG = ("pe", "act", "dve", "pool", "sp")


class Cfg:
    def __init__(self, D=2048, S=4096, E=32, DFF=2048, CAP=768):
        self.D, self.S, self.E, self.DFF, self.CAP = D, S, E, DFF, CAP
        self.DC = D // 128
        self.FC = DFF // 128
        self.NQ = S // 2
        self.NGRP = 6
        self.GW = 768


class Buf:
    __slots__ = ("w", "r")

    def __init__(self):
        self.w = {}
        self.r = {}


class Sem:
    def __init__(self, h):
        self.h = h
        self.n = 0


class _Rec:
    def __getattr__(self, name):
        def f(*a, **k):
            self.call = (name, a, k)
            return self
        return f


class Prog:
    def __init__(self, nc, sems):
        self.nc = nc
        self.free_sems = list(sems)
        self.prog = {e: Sem(self.free_sems.pop()) for e in ENG if e != "sp"}
        self.bar = Sem(self.free_sems.pop())
        self.ops = {e: [] for e in ENG}
        self.waited = {e: {} for e in ENG}
        self.pend_r = {e: [] for e in ENG}
        self.pend_w = {e: [] for e in ENG}
        self.touched = {e: {} for e in ENG}
        self.nphase = 0

    def sem(self):
        return Sem(self.free_sems.pop())

    def _deps(self, eng, reads, writes, extra):
        deps = {}

        def add(tok):
            s, v = tok
            if deps.get(s, (None, 0))[1] < v:
                deps[s] = (s, v)

        for b in reads:
            for t in b.w.values():
                add(t)
        for b in writes:
            for t in b.w.values():
                add(t)
            for t in b.r.values():
                add(t)
        for t in extra:
            if t is not None:
                add(t)
        out = []
        wd = self.waited[eng]
        for s, v in deps.values():
            if wd.get(s.h, 0) < v:
                wd[s.h] = v
                out.append((s.h, v))
        return out

    def op(self, eng, fn, reads=(), writes=(), sig=True, dma=None, extra=()):
        waits = self._deps(eng, reads, writes, extra)
        tok = None
        rec = _Rec()
        fn(rec)
        fn = rec.call
        if dma is not None:
            dma.n += 16
            tok = (dma, dma.n)
            self.ops[eng].append((fn, waits, dma.h, 16, dma.n))
            self.touched[eng][dma.h] = (dma, dma.n)
            for b in reads:
                b.r[("dma", dma.h)] = tok
            for b in writes:
                b.w = {("dma", dma.h): tok}
                b.r = {}
            return tok
        if sig:
            s = self.prog[eng]
            s.n += 1
            tok = (s, s.n)
            self.ops[eng].append((fn, waits, s.h, 1, s.n))
            self.touched[eng][s.h] = (s, s.n)
            for b in list(reads) + self.pend_r[eng]:
                b.r[eng] = tok
            for b in list(writes) + self.pend_w[eng]:
                b.w = {eng: tok}
                b.r = {}
            self.pend_r[eng] = []
            self.pend_w[eng] = []
        else:
            self.ops[eng].append((fn, waits, None, 0, 0))
            self.pend_r[eng] += list(reads)
            self.pend_w[eng] += list(writes)
        return tok

    def load_count(self, ap):
        for e in ENG:
            self.ops[e].append(("load", ap))

    def begin_cond(self, thr):
        self._snap = {e: dict(self.waited[e]) for e in ENG}
        for e in ENG:
            self.ops[e].append(("begin", thr))

    def end_cond(self):
        assert all(not v for v in self.pend_r.values()) and all(not v for v in self.pend_w.values())
        for e in ENG:
            self.ops[e].append(("end",))
        self.waited = self._snap

    def flush(self):
        nc = self.nc
        self.nphase += 1
        target = 5 * self.nphase
        ops = self.ops
        touched = self.touched
        bar = self.bar

        prog_h = {e: (self.prog[e].h if e in self.prog else None) for e in ENG}

        def mk(name):
            def body(e):
                regs = {}
                creg = [None]

                def run(item):
                    fn, waits, inc, amt, _after = item
                    for s, v in waits:
                        e.wait_ge(s, v)
                    kw = fn[2]
                    bc = kw.get("bounds_check")
                    if isinstance(bc, int):
                        if bc not in regs:
                            regs[bc] = e.to_reg(bc)
                        kw = dict(kw, bounds_check=regs[bc])
                    ins = getattr(e, fn[0])(*fn[1], **kw)
                    if inc is not None:
                        ins.then_inc(inc, amt)

                lst = ops[name]
                i = 0
                while i < len(lst):
                    item = lst[i]
                    if item[0] == "load":
                        lm = getattr(self, "load_engs", ENG)
                        if name not in lm:
                            i += 1
                            continue
                        if creg[0] is None:
                            creg[0] = e.alloc_register(name=f"cnt_{name}_{self.nphase}")
                        e.reg_load(creg[0], item[1])
                        i += 1
                    elif item[0] == "begin":
                        thr = item[1]
                        j = i + 1
                        while lst[j][0] != "end":
                            j += 1
                        region = lst[i + 1:j]
                        i = j + 1
                        if not region:
                            continue
                        acc = {}
                        order = []
                        for fn, waits, inc, amt, after in region:
                            if inc is None:
                                continue
                            if inc not in acc:
                                acc[inc] = [after - amt, 0]
                                order.append(inc)
                            acc[inc][1] += amt
                        mode = getattr(self, "cond_mode", "full")
                        if mode == "loadonly" or (isinstance(mode, tuple) and name not in mode):
                            for it in region:
                                run(it)
                            continue
                        with e.If_lt(creg[0], thr + 1):
                            for inc in order:
                                bef, tot = acc[inc]
                                if bef > 0:
                                    e.wait_ge(inc, bef)
                                e.nop().then_inc(inc, tot)
                        with e.Else():
                            for it in region:
                                run(it)
                    else:
                        run(item)
                        i += 1
                for s, v in touched[name].values():
                    e.wait_ge(s.h, v)
                e.nop().then_inc(bar.h, 1)
                e.wait_ge(bar.h, target)
            return body

        with nc.Block() as blk:
            blk.tensor(mk("pe"))
            blk.scalar(mk("act"))
            blk.vector(mk("dve"))
            blk.gpsimd(mk("pool"))
            blk.sync(mk("sp"))
        self.ops = {e: [] for e in ENG}
        self.touched = {e: {} for e in ENG}
        assert all(not v for v in self.pend_r.values()) and all(not v for v in self.pend_w.values())


def build(cfg, debug=None, stop_after=None):
    D, S, E, DFF, CAP, DC, FC, NQ = cfg.D, cfg.S, cfg.E, cfg.DFF, cfg.CAP, cfg.DC, cfg.FC, cfg.NQ
    NTC = S // 128
    NQC = NQ // 128
    NTB = S // 512
    NQB = NQ // 512
    NROW = (E + 1) * CAP
    nc = bass.Bass("TRN2", target_bir_lowering=False)
    dt = nc.dram_tensor

    xkv = dt("xkv", [S, D], F32, kind="ExternalInput").ap()
    c_fm = dt("c_fm", [128, DC], F32, kind="ExternalInput").ap()
    w_ada = dt("w_ada", [D, 6 * D], F32, kind="ExternalInput").ap()
    bada_rep = dt("bada_rep", [128, 6 * D], F32, kind="ExternalInput").ap()
    gn_rep = dt("gn_rep", [128, 2 * D], F32, kind="ExternalInput").ap()
    win_g = dt("win_g", [D, 6 * 768], F32, kind="ExternalInput").ap()
    w_out = dt("w_out", [2048, D], F32, kind="ExternalInput").ap()
    gains = dt("gains", [128, 8], F32, kind="ExternalInput").ap()
    lam_rep = dt("lam_rep", [128, 4 * 64], F32, kind="ExternalInput").ap()
    cmat = dt("cmat", [128, 6 * 128], F32, kind="ExternalInput").ap()
    ropec = dt("ropec", [128, S], F32, kind="ExternalInput").ap()
    ropes = dt("ropes", [128, S], F32, kind="ExternalInput").ap()
    relT = dt("relT", [32, 16], F32, kind="ExternalInput").ap()
    ohr = dt("ohr", [32, 2 * FEXT + 2], F32, kind="ExternalInput").ap()
    w_router = dt("w_router", [D, E], F32, kind="ExternalInput").ap()
    brout_rep = dt("brout_rep", [128, E], F32, kind="ExternalInput").ap()
    w_gu = dt("w_gu", [E, D, 2 * DFF], F32, kind="ExternalInput").ap()
    bgu_fm = dt("bgu_fm", [128, E * 2 * FC], F32, kind="ExternalInput").ap()
    w_down = dt("w_down", [E, DFF, D], F32, kind="ExternalInput").ap()
    b_down = dt("b_down", [E, D], F32, kind="ExternalInput").ap()
    iota_e = dt("iota_e", [128, E], F32, kind="ExternalInput").ap()
    padrow = dt("padrow", [128, E], F32, kind="ExternalInput").ap()
    out = dt("out", [NQ, D], F32, kind="ExternalOutput").ap()

    debug = debug or ()
    ds = lambda name, shape, dtype: dt(name, shape, dtype, kind=("ExternalOutput" if name in debug else "Internal")).ap()
    hT_d = ds("hT_d", [DC, 128, S], BF16)
    mod_d = ds("mod_d", [128, 6 * D], F32)
    OT_d = ds("OT_d", [16, 128, NQ], BF16)
    fext_d = ds("fext_d", [8, 2 * FEXT + 2], F32)
    hs_d = ds("hs_d", [NROW, D], BF16)
    ys_d = ds("ys_d", [NROW, D], F32)
    cnt_d = ds("cnt_d", [1, E], I32)

    P = Prog(nc, [nc.semaphore(f"s{i}").__enter__() for i in range(90)])
    SBG = lambda name, shape, dtype: nc.sbuf_tensor(name, shape, dtype).__enter__()
    PS = lambda name, shape, dtype=F32: nc.psum_tensor(name, shape, dtype).__enter__()
    es = [ExitStack()]
    SB = lambda name, shape, dtype: es[0].enter_context(nc.sbuf_tensor(name, shape, dtype))

    def end_phase():
        P.flush()
        es[0].close()
        es[0] = ExitStack()

    CM = SBG("CM", [128, 6, 128], BF16)
    bCM = Buf()
    dsem = P.sem()
    P.op("pool", lambda e: e.dma_start(out=CM[:], in_=cmat.rearrange("p (k n) -> p k n", k=6)), writes=[bCM], dma=dsem)
    IDENT, ONES, JM, SWP, TRI, BLK64 = (CM[:, i, :] for i in range(6))

    epsc = SBG("epsc", [128, 2], F32)
    beps = Buf()
    P.op("dve", lambda e: e.memset(epsc[:, 0:1], EPS), writes=[beps])
    P.op("dve", lambda e: e.memset(epsc[:, 1:2], 0.0), writes=[beps])
    banks = [PS(f"bank{i}", [128, 512]) for i in range(8)]
    bbank = [Buf() for _ in range(8)]

    modrep = SB("modrep", [128, 6 * D], F32)
    bmod = [Buf() for _ in range(6)]
    cin = SB("cin", [128, DC], F32)
    cact = SB("cact", [128, DC], F32)
    crep = SB("crep", [128, DC, 128], BF16)
    bc = Buf(); bcr = Buf()
    s_c = P.sem()
    P.op("sp", lambda e: e.dma_start(out=cin[:], in_=c_fm), writes=[bc], dma=s_c)
    P.op("act", lambda e: e.activation(out=cact[:], in_=cin[:], func=AF.Silu), reads=[bc], writes=[bcr])
    P.op("dve", lambda e: e.tensor_copy(out=crep[:], in_=cact[:].unsqueeze(2).to_broadcast([128, DC, 128])),
         reads=[bcr], writes=[bcr])
    badar = [SB(f"badar{i}", [128, 512], F32) for i in range(2)]
    gnr = SB("gnr", [128, 2 * D], F32)
    bbada = [Buf(), Buf()]; bgn = Buf()
    s_b1 = [P.sem(), P.sem()]; s_b2 = P.sem()
    P.op("sp", lambda e: e.dma_start(out=gnr[:], in_=gn_rep), writes=[bgn], dma=s_b2)
    wa = [SB(f"wa{i}", [128, DC, 512], BF16) for i in range(2)]
    bwa = [Buf(), Buf()]
    s_wa = [P.sem(), P.sem()]
    wada_v = w_ada.rearrange("(c p) n -> p c n", p=128)
    NCB = 6 * D // 512
    for cb in range(NCB):
        sl = cb % 2
        P.op("pool", lambda e, sl=sl, cb=cb: e.dma_start(out=wa[sl][:], in_=wada_v[:, :, cb * 512:(cb + 1) * 512]),
             writes=[bwa[sl]], dma=s_wa[sl])
        bk = cb % 2
        P.op("sp", lambda e, sl=sl, cb=cb: e.dma_start(out=badar[sl][:], in_=bada_rep[:, cb * 512:(cb + 1) * 512]),
             writes=[bbada[sl]], dma=s_b1[sl])
        for j in range(DC):
            P.op("pe", lambda e, sl=sl, j=j, bk=bk: e.matmul(banks[bk][:], lhsT=crep[:, j, :], rhs=wa[sl][:, j, :],
                                                             start=(j == 0), stop=(j == DC - 1)),
                 reads=[bcr, bwa[sl]], writes=[bbank[bk]], sig=(j == DC - 1))
        seg = (cb * 512) // D
        P.op("dve", lambda e, bk=bk, cb=cb: e.tensor_tensor(out=modrep[:, cb * 512:(cb + 1) * 512], in0=banks[bk][:],
                                                            in1=badar[bk][:], op=ALU.add),
             reads=[bbank[bk], bbada[bk]], writes=[bmod[seg]])
    for seg, gi in ((1, 0), (4, 1)):
        P.op("dve", lambda e, seg=seg, gi=gi: e.scalar_tensor_tensor(
            out=modrep[:, seg * D:(seg + 1) * D], in0=modrep[:, seg * D:(seg + 1) * D], scalar=1.0,
            in1=gnr[:, gi * D:(gi + 1) * D], op0=ALU.add, op1=ALU.mult), reads=[bmod[seg], bgn], writes=[bmod[seg]])
    bmod_d = Buf()
    s_md = P.sem()
    P.op("sp", lambda e: e.dma_start(out=mod_d, in_=modrep[:]), reads=bmod, writes=[bmod_d], dma=s_md)

    xt = [SB(f"xt{i}", [128, D], F32) for i in range(2)]
    bxt = [Buf(), Buf()]
    s_xt = [P.sem(), P.sem()]
    junk = SB("junk", [128, D], BF16)
    bjunk = Buf()
    ssA = SB("ssA", [128, NTC], F32)
    rsA = SB("rsA", [128, NTC], F32)
    bss = [Buf() for _ in range(NTC)]
    tmpA = SB("tmpA", [128, D], F32)
    btmpA = Buf()
    hb = [SB(f"hb{i}", [128, 4, D], BF16) for i in range(2)]
    bhb = [Buf(), Buf()]
    hTb = [SB(f"hTb{i}", [128, DC, 512], BF16) for i in range(2)]
    bhTb = [Buf(), Buf()]
    s_hst = [P.sem(), P.sem()]
    bhT_d = Buf()
    hT_v = hT_d.rearrange("c p s -> p c s")
    for t in range(NTC):
        sl = t % 2
        g4 = (t // 4) % 2
        P.op("sp", lambda e, sl=sl, t=t: e.dma_start(out=xt[sl][:], in_=xkv[t * 128:(t + 1) * 128, :]),
             writes=[bxt[sl]], dma=s_xt[sl])
        P.op("act", lambda e, sl=sl, t=t: e.activation(out=junk[:], in_=xt[sl][:], func=AF.Square,
                                                       accum_out=ssA[:, t:t + 1]),
             reads=[bxt[sl]], writes=[bjunk, bss[t]])
        P.op("act", lambda e, t=t: e.activation(out=rsA[:, t:t + 1], in_=ssA[:, t:t + 1], func=AF.Sqrt,
                                                bias=epsc[:, 0:1], scale=1.0 / D), reads=[bss[t], beps], writes=[bss[t]])
        P.op("dve", lambda e, t=t: e.reciprocal(out=rsA[:, t:t + 1], in_=rsA[:, t:t + 1]), reads=[bss[t]], writes=[bss[t]])
        P.op("dve", lambda e, sl=sl, t=t: e.scalar_tensor_tensor(out=tmpA[:], in0=xt[sl][:], scalar=rsA[:, t:t + 1],
                                                                 in1=modrep[:, D:2 * D], op0=ALU.mult, op1=ALU.mult),
             reads=[bxt[sl], bss[t], bmod[1]], writes=[btmpA])
        P.op("dve", lambda e, g4=g4, t=t: e.tensor_tensor(out=hb[g4][:, t % 4, :], in0=tmpA[:], in1=modrep[:, 0:D],
                                                          op=ALU.add), reads=[btmpA, bmod[0]], writes=[bhb[g4]])
        if t % 4 == 3:
            tb = t // 4
            hs = tb % 2
            for j in range(DC):
                bk = 2 + (j % 2)
                pv = banks[bk][:].bitcast(BF16)
                for q in range(4):
                    P.op("pe", lambda e, g4=g4, j=j, q=q, pv=pv: e.transpose(
                        out=pv[:, q * 128:(q + 1) * 128], in_=hb[g4][:, q, j * 128:(j + 1) * 128], identity=IDENT),
                        reads=[bhb[g4], bCM], writes=[bbank[bk]], sig=(q == 3))
                P.op("act", lambda e, hs=hs, j=j, pv=pv: e.copy(out=hTb[hs][:, j, :], in_=pv[:, 0:512]),
                     reads=[bbank[bk]], writes=[bhTb[hs]])
            P.op("sp", lambda e, hs=hs, tb=tb: e.dma_start(out=hT_v[:, :, tb * 512:(tb + 1) * 512], in_=hTb[hs][:]),
                 reads=[bhTb[hs]], writes=[bhT_d], dma=s_hst[hs])
    end_phase()
    if stop_after == "A":
        return nc, P
    NW = 2 * FEXT + 2
    gsb = SB("gsb", [128, 8], F32); bg_ = Buf()
    lamr = SB("lamr", [128, 256], F32); blam = Buf()
    lamt = SB("lamt", [128, 128], F32)
    lams = SB("lams", [128, 4], F32)
    s_g = P.sem(); s_l = P.sem()
    P.op("sp", lambda e: e.dma_start(out=gsb[:], in_=gains), writes=[bg_], dma=s_g)
    P.op("sp", lambda e: e.dma_start(out=lamr[:], in_=lam_rep), writes=[blam], dma=s_l)
    P.op("dve", lambda e: e.tensor_tensor(out=lamt[:, 0:64], in0=lamr[:, 0:64], in1=lamr[:, 64:128], op=ALU.mult),
         reads=[blam], writes=[blam])
    P.op("dve", lambda e: e.tensor_tensor(out=lamt[:, 64:128], in0=lamr[:, 128:192], in1=lamr[:, 192:256], op=ALU.mult),
         reads=[blam], writes=[blam])
    P.op("dve", lambda e: e.reduce_sum(out=lams[:, 0:1], in_=lamt[:, 0:64], axis=AX.X), reads=[blam], writes=[blam])
    P.op("dve", lambda e: e.reduce_sum(out=lams[:, 1:2], in_=lamt[:, 64:128], axis=AX.X), reads=[blam], writes=[blam])
    P.op("act", lambda e: e.activation(out=lams[:, 0:2], in_=lams[:, 0:2], func=AF.Exp), reads=[blam], writes=[blam])
    P.op("dve", lambda e: e.tensor_tensor(out=lams[:, 3:4], in0=lams[:, 1:2], in1=lams[:, 0:1], op=ALU.subtract),
         reads=[blam], writes=[blam])
    P.op("dve", lambda e: e.tensor_scalar_add(out=lams[:, 2:3], in0=lams[:, 3:4], scalar1=-LAM_INIT), reads=[blam], writes=[blam])
    NEGLAM = lams[:, 2:3]
    P.op("dve", lambda e: e.tensor_scalar_mul(out=gsb[:, 5:6], in0=gsb[:, 4:5], scalar1=1.0 - LAM_INIT), reads=[bg_], writes=[bg_])
    SUBG = gsb[:, 5:6]

    tab32 = SB("tab32", [32, 16], F32); btab = Buf()
    tabh = SB("tabh", [32, 16], BF16)
    ohb = SB("ohb", [32, NW], BF16); boh = Buf()
    fx = SB("fx", [8, NW], F32); bfx = Buf()
    s_t = P.sem(); s_o = P.sem(); s_f = P.sem()
    P.op("sp", lambda e: e.dma_start(out=tab32[:], in_=relT), writes=[btab], dma=s_t)
    P.op("pool", lambda e: e.dma_start(out=ohb[:], in_=ohr), writes=[boh], dma=s_o)
    P.op("dve", lambda e: e.tensor_copy(out=tabh[:, 0:8], in_=tab32[:, 0:8]), reads=[btab], writes=[btab])
    P.op("dve", lambda e: e.tensor_tensor(out=tab32[:, 8:16], in0=tab32[:, 8:16], in1=tabh[:, 0:8], op=ALU.subtract),
         reads=[btab], writes=[btab])
    P.op("dve", lambda e: e.tensor_copy(out=tabh[:, 8:16], in_=tab32[:, 8:16]), reads=[btab], writes=[btab])
    for pc in range(0, NW, 512):
        n = min(512, NW - pc)
        P.op("pe", lambda e, pc=pc, n=n: e.matmul(banks[7][0:8, 0:n], lhsT=tabh[:, 0:8], rhs=ohb[:, pc:pc + n], start=True, stop=False),
             reads=[btab, boh], writes=[bbank[7]], sig=False)
        P.op("pe", lambda e, pc=pc, n=n: e.matmul(banks[7][0:8, 0:n], lhsT=tabh[:, 8:16], rhs=ohb[:, pc:pc + n], start=False, stop=True),
             reads=[btab, boh], writes=[bbank[7]])
        P.op("dve", lambda e, pc=pc, n=n: e.tensor_copy(out=fx[:, pc:pc + n], in_=banks[7][0:8, 0:n]), reads=[bbank[7]], writes=[bfx])
    bfext_d = Buf()
    P.op("sp", lambda e: e.dma_start(out=fext_d, in_=fx[:]), reads=[bfx], writes=[bfext_d], dma=s_f)
    cfar = SB("cfar", [128, 2, 8], F32); bcfar = Buf()
    s_cf = P.sem()
    for fs in range(2):
        cf_src = bass.AP(tensor=fext_d.tensor, offset=fs * 2 * FEXT, ap=[[0, 128], [NW, 8]])
        P.op("sp", lambda e, fs=fs, cf_src=cf_src: e.dma_start(out=cfar[:, fs, :], in_=cf_src, allow_slow_non_contiguous=True),
             reads=[bfext_d], writes=[bcfar], dma=s_cf)

    wg = SB("wg", [128, DC, 768], BF16); bwg = Buf(); s_wg = P.sem()
    hTb2 = [SB(f"hTc{i}", [128, DC, 512], BF16) for i in range(2)]
    bhTb2 = [Buf(), Buf()]; s_hl = [P.sem(), P.sem()]
    QT = [SB(f"QT{i}", [128, NQ], BF16) for i in range(4)]; bQT = [Buf() for _ in range(4)]
    KT = [SB(f"KT{i}", [128, S], BF16) for i in range(2)]; bKT = [Buf() for _ in range(2)]
    VV = SB("VV", [128, NTC, 256], BF16); bVV = Buf()
    sqb = [SB(f"sqb{i}", [128, 512], BF16) for i in range(2)]; bsqb = [Buf(), Buf()]
    rsb = [SB(f"rsb{i}", [128, 512], F32) for i in range(2)]; brsb = [Buf(), Buf()]
    qnb = [SB(f"qnb{i}", [128, 512], BF16) for i in range(2)]; bqnb = [Buf(), Buf()]
    t1b = [SB(f"t1b{i}", [128, 512], F32) for i in range(2)]; bt1b = [Buf(), Buf()]
    t2b = [SB(f"t2b{i}", [128, 512], F32) for i in range(2)]; bt2b = [Buf(), Buf()]
    cosb = [SB(f"cosb{i}", [128, 512], F32) for i in range(4)]; bcos = [Buf() for _ in range(4)]; s_cos = [P.sem() for _ in range(4)]
    sinb = [SB(f"sinb{i}", [128, 512], F32) for i in range(4)]; bsin = [Buf() for _ in range(4)]; s_sin = [P.sem() for _ in range(4)]
    PT = [SB(f"PT{i}", [128, 512], BF16) for i in range(8)]; bPT = [Buf() for _ in range(8)]
    ep = [SB(f"ep{i}", [128, 512], F32) for i in range(6)]; bep = [Buf() for _ in range(6)]
    epq = SB("epq", [128, 512], BF16); bepq = Buf()
    OTs = [SB(f"OTs{i}", [128, 512], BF16) for i in range(2)]; bOTs = [Buf(), Buf()]; s_ot = [P.sem(), P.sem()]
    HKf = SB("HKf", [128, 5, 128], F32); bHKf = Buf(); s_hk = P.sem()
    HKh = [SB(f"HKh{i}", [128, 5, 128], BF16) for i in range(2)]
    HKl = [SB(f"HKl{i}", [128, 5, 128], BF16) for i in range(2)]
    bHK = [Buf(), Buf()]
    hTd_v = hT_d.rearrange("c p s -> p c s")
    win_v = win_g.rearrange("(c p) n -> p c n", p=128)
    ctr = {"s": 0, "pt": 0, "ot": 0, "job": 0}
    deferred = []

    for g in getattr(cfg, 'groups', range(6)):
        isA = g < 2
        nq = 4 if isA else 2
        nk = 1 if isA else 2
        vw = 128 if isA else 256
        koff = 512 if isA else 256
        voff = 640 if isA else 512
        P.op("pool", lambda e, g=g: e.dma_start(out=wg[:], in_=win_v[:, :, g * 768:(g + 1) * 768]), writes=[bwg], dma=s_wg)
        if not isA:
            for hh in range(2):
                h = 2 * (g - 2) + hh
                src = bass.AP(tensor=fext_d.tensor, offset=h * NW + FEXT - 256 - 127, ap=[[1, 128], [128, 5], [1, 128]])
                P.op("sp", lambda e, src=src: e.dma_start(out=HKf[:], in_=src), reads=[bfext_d], writes=[bHKf], dma=s_hk)
                P.op("dve", lambda e: e.tensor_scalar_mul(out=HKf[:], in0=HKf[:], scalar1=8.0), reads=[bHKf], writes=[bHKf])
                P.op("dve", lambda e, hh=hh: e.tensor_copy(out=HKh[hh][:], in_=HKf[:]), reads=[bHKf], writes=[bHK[hh]])
                P.op("dve", lambda e, hh=hh: e.tensor_tensor(out=HKl[hh][:], in0=HKf[:], in1=HKh[hh][:], op=ALU.subtract),
                     reads=[bHKf, bHK[hh]], writes=[bHK[hh]])
        jobs = []
        for tb in range(NTB):
            for ki in range(nk):
                jobs.append((tb, "k", ki))
            if tb < NQB:
                for qi in range(nq):
                    jobs.append((tb, "q", qi))
        n = len(jobs)
        st = {}

        def stage1(i):
            tb, kind, idx = jobs[i]
            hs = tb % 2
            first = (i == 0) or (jobs[i - 1][0] != tb)
            if first:
                P.op("sp", lambda e, hs=hs, tb=tb: e.dma_start(out=hTb2[hs][:], in_=hTd_v[:, :, tb * 512:(tb + 1) * 512]),
                     writes=[bhTb2[hs]], dma=s_hl[hs])
                if isA:
                    h4 = tb % 4
                    P.op("act", lambda e, h4=h4, tb=tb: e.dma_start(out=cosb[h4][:], in_=ropec[:, tb * 512:(tb + 1) * 512]),
                         writes=[bcos[h4]], dma=s_cos[h4])
                    P.op("act", lambda e, h4=h4, tb=tb: e.dma_start(out=sinb[h4][:], in_=ropes[:, tb * 512:(tb + 1) * 512]),
                         writes=[bsin[h4]], dma=s_sin[h4])
            jn = ctr["job"]; ctr["job"] += 1
            pb = jn % 3
            c0 = (koff + idx * 128) if kind == "k" else idx * 128
            for j in range(DC):
                P.op("pe", lambda e, pb=pb, j=j, c0=c0, hs=hs: e.matmul(banks[pb][:], lhsT=wg[:, j, c0:c0 + 128], rhs=hTb2[hs][:, j, :],
                                                                      start=(j == 0), stop=(j == DC - 1)),
                     reads=[bwg, bhTb2[hs]], writes=[bbank[pb]], sig=(j == DC - 1))
            r2 = jn % 2
            P.op("act", lambda e, pb=pb, r2=r2: e.activation(out=sqb[r2][:], in_=banks[pb][:], func=AF.Square),
                 reads=[bbank[pb]], writes=[bsqb[r2]])
            st[i] = (jn, pb, r2)
            last = (i == n - 1) or (jobs[i + 1][0] != tb)
            if last:
                for tc in range(4):
                    for j in range(DC):
                        P.op("pe", lambda e, j=j, tc=tc, hs=hs: e.matmul(banks[7][:, 0:vw], lhsT=hTb2[hs][:, j, tc * 128:(tc + 1) * 128],
                                                                       rhs=wg[:, j, voff:voff + vw], start=(j == 0), stop=(j == DC - 1)),
                             reads=[bwg, bhTb2[hs]], writes=[bbank[7]], sig=(j == DC - 1))
                    P.op("act", lambda e, tb=tb, tc=tc: e.copy(out=VV[:, tb * 4 + tc, 0:vw], in_=banks[7][:, 0:vw]),
                         reads=[bbank[7]], writes=[bVV])

        def stage2(i):
            tb, kind, idx = jobs[i]
            jn, pb, r2 = st[i]
            sb_ = 3 + r2
            P.op("pe", lambda e, sb_=sb_, r2=r2: e.matmul(banks[sb_][:], lhsT=(ONES if isA else BLK64), rhs=sqb[r2][:], start=True, stop=True),
                 reads=[bCM, bsqb[r2]], writes=[bbank[sb_]])
            P.op("act", lambda e, sb_=sb_, r2=r2: e.activation(out=rsb[r2][:], in_=banks[sb_][:], func=AF.Sqrt, bias=epsc[:, 0:1],
                                                               scale=(1.0 / 128 if isA else 1.0 / 64)),
                 reads=[bbank[sb_], beps], writes=[brsb[r2]])
            P.op("dve", lambda e, r2=r2: e.reciprocal(out=rsb[r2][:], in_=rsb[r2][:]), reads=[brsb[r2]], writes=[brsb[r2]])
            gcol = (1 if kind == "k" else 0) + (0 if isA else 2)
            if isA:
                dst, dbuf = qnb[r2][:], bqnb[r2]
            elif kind == "k":
                dst, dbuf = KT[idx][:, tb * 512:(tb + 1) * 512], bKT[idx]
            else:
                dst, dbuf = QT[idx][:, tb * 512:(tb + 1) * 512], bQT[idx]
            P.op("dve", lambda e, pb=pb, r2=r2, gcol=gcol, dst=dst: e.scalar_tensor_tensor(
                out=dst, in0=banks[pb][:], scalar=gsb[:, gcol:gcol + 1], in1=rsb[r2][:], op0=ALU.mult, op1=ALU.mult),
                reads=[bbank[pb], bg_, brsb[r2]], writes=[dbuf])

        def stage3(i):
            if not isA:
                return
            tb, kind, idx = jobs[i]
            jn, pb, r2 = st[i]
            hs = tb % 4
            wb = 5 + r2
            P.op("pe", lambda e, wb=wb, r2=r2: e.matmul(banks[wb][:], lhsT=SWP, rhs=qnb[r2][:], start=True, stop=True),
                 reads=[bCM, bqnb[r2]], writes=[bbank[wb]])
            P.op("dve", lambda e, r2=r2, hs=hs: e.tensor_tensor(out=t1b[r2][:], in0=qnb[r2][:], in1=cosb[hs][:], op=ALU.mult),
                 reads=[bqnb[r2], bcos[hs]], writes=[bt1b[r2]])
            P.op("dve", lambda e, r2=r2, hs=hs, wb=wb: e.tensor_tensor(out=t2b[r2][:], in0=banks[wb][:], in1=sinb[hs][:], op=ALU.mult),
                 reads=[bbank[wb], bsin[hs]], writes=[bt2b[r2]])
            if kind == "k":
                dst, dbuf = KT[idx][:, tb * 512:(tb + 1) * 512], bKT[idx]
            else:
                dst, dbuf = QT[idx][:, tb * 512:(tb + 1) * 512], bQT[idx]
            P.op("dve", lambda e, r2=r2, dst=dst: e.tensor_tensor(out=dst, in0=t1b[r2][:], in1=t2b[r2][:], op=ALU.add),
                 reads=[bt1b[r2], bt2b[r2]], writes=[dbuf])

        for i in range(n + 2):
            if i < n:
                stage1(i)
            if 0 <= i - 1 < n:
                stage2(i - 1)
            if 0 <= i - 2 < n:
                stage3(i - 2)

        scale = (128.0 ** -0.5) if isA else 0.125
        nblk = 0
        for qi in range(nq):
            otile = (4 * g + qi) if isA else (8 + 2 * (g - 2) + qi)
            hglob = None if isA else 2 * (g - 2) + qi
            maps = [(0, 128)] if isA else [(0, 64), (64, 128)]
            ktile = 0 if isA else qi
            vc0 = 0 if isA else qi * 128
            for Qb in range(NQB):
                if isA:
                    accb = [3 + nblk % 2]; sumb = [5 + nblk % 2]
                else:
                    accb = [3, 4]; sumb = [5, 6]
                nblk += 1
                sinfo = {}

                def emitS(kc):
                    for mi, (p0, p1) in enumerate(maps):
                        sbk = ctr["s"] % 3; ctr["s"] += 1
                        ptk = ctr["pt"] % 8; ctr["pt"] += 1
                        jrel = kc - 4 * Qb
                        mixed = (not isA) and (-1 <= jrel <= 4) and not getattr(cfg, 'nobias', False)
                        near = [i for i in range(4) if abs(jrel - i) <= 1] if mixed else []
                        P.op("pe", lambda e, sbk=sbk, p0=p0, p1=p1, kc=kc: e.matmul(
                            banks[sbk][:], lhsT=KT[ktile][p0:p1, kc * 128:(kc + 1) * 128], rhs=QT[qi][p0:p1, Qb * 512:(Qb + 1) * 512],
                            start=True, stop=(len(near) == 0)), reads=[bKT[ktile], bQT[qi]], writes=[bbank[sbk]], sig=(len(near) == 0))
                        for ni, i in enumerate(near):
                            di = jrel - i + 2
                            lastn = ni == len(near) - 1
                            P.op("pe", lambda e, sbk=sbk, i=i, di=di: e.matmul(banks[sbk][:, i * 128:(i + 1) * 128], lhsT=HKh[qi][:, di, :], rhs=JM,
                                                                             start=False, stop=False),
                                 reads=[bHK[qi], bCM], writes=[bbank[sbk]], sig=False)
                            P.op("pe", lambda e, sbk=sbk, i=i, di=di, lastn=lastn: e.matmul(banks[sbk][:, i * 128:(i + 1) * 128], lhsT=HKl[qi][:, di, :], rhs=JM,
                                                                                         start=False, stop=lastn),
                                 reads=[bHK[qi], bCM], writes=[bbank[sbk]], sig=lastn)
                        if isA:
                            P.op("act", lambda e, sbk=sbk, ptk=ptk: e.activation(out=PT[ptk][:], in_=banks[sbk][:], func=AF.Exp, scale=scale),
                                 reads=[bbank[sbk]], writes=[bPT[ptk]])
                        elif getattr(cfg, 'nobias', False):
                            P.op("act", lambda e, sbk=sbk, ptk=ptk: e.activation(out=PT[ptk][:], in_=banks[sbk][:], func=AF.Exp, scale=scale),
                                 reads=[bbank[sbk]], writes=[bPT[ptk]])
                        elif not mixed:
                            fs = 0 if jrel < 0 else 1
                            P.op("act", lambda e, sbk=sbk, ptk=ptk, fs=fs: e.activation(out=PT[ptk][:], in_=banks[sbk][:], func=AF.Exp, scale=scale,
                                                                                    bias=cfar[:, fs, hglob:hglob + 1]),
                                 reads=[bbank[sbk], bcfar], writes=[bPT[ptk]])
                        else:
                            for i in range(4):
                                dd = jrel - i
                                bias_ap = epsc[:, 1:2] if abs(dd) <= 1 else cfar[:, (0 if dd < 0 else 1), hglob:hglob + 1]
                                P.op("act", lambda e, sbk=sbk, ptk=ptk, i=i, bias_ap=bias_ap: e.activation(
                                    out=PT[ptk][:, i * 128:(i + 1) * 128], in_=banks[sbk][:, i * 128:(i + 1) * 128], func=AF.Exp, scale=scale, bias=bias_ap),
                                    reads=[bbank[sbk], bcfar, beps], writes=[bPT[ptk]])
                        sinfo[(kc, mi)] = ptk

                def emitPV(kc):
                    for mi in range(len(maps)):
                        ptk = sinfo[(kc, mi)]
                        P.op("pe", lambda e, mi=mi, ptk=ptk, kc=kc: e.matmul(banks[accb[mi]][:], lhsT=VV[:, kc, vc0:vc0 + 128], rhs=PT[ptk][:],
                                                                           start=(kc == 0), stop=(kc == NTC - 1)),
                             reads=[bVV, bPT[ptk]], writes=[bbank[accb[mi]]], sig=False)
                        P.op("pe", lambda e, mi=mi, ptk=ptk, kc=kc: e.matmul(banks[sumb[mi]][:], lhsT=ONES, rhs=PT[ptk][:],
                                                                           start=(kc == 0), stop=(kc == NTC - 1)),
                             reads=[bCM, bPT[ptk]], writes=[bbank[sumb[mi]]], sig=True)

                for step in range(NTC + 2):
                    if step < NTC:
                        emitS(step)
                    if step == 3 and deferred:
                        for fdef in deferred:
                            fdef()
                        deferred.clear()
                    if step >= 2:
                        emitPV(step - 2)
                osl = ctr["ot"] % 2; ctr["ot"] += 1
                if isA:
                    P.op("dve", lambda e, sb_=sumb[0]: e.reciprocal(out=ep[0][:], in_=banks[sb_][:]), reads=[bbank[sumb[0]]], writes=[bep[0]])
                    P.op("dve", lambda e, ab=accb[0], osl=osl: e.tensor_tensor(out=OTs[osl][:], in0=banks[ab][:], in1=ep[0][:], op=ALU.mult),
                         reads=[bbank[accb[0]], bep[0]], writes=[bOTs[osl]])
                    P.op("sp", lambda e, osl=osl, otile=otile, Qb=Qb: e.dma_start(out=OT_d[otile, :, Qb * 512:(Qb + 1) * 512], in_=OTs[osl][:]),
                         reads=[bOTs[osl]], dma=s_ot[osl])
                else:
                    P.op("dve", lambda e: e.reciprocal(out=ep[0][:], in_=banks[5][:]), reads=[bbank[5]], writes=[bep[0]])
                    P.op("dve", lambda e: e.tensor_tensor(out=ep[1][:], in0=banks[3][:], in1=ep[0][:], op=ALU.mult),
                         reads=[bbank[3], bep[0]], writes=[bep[1]])
                    P.op("dve", lambda e: e.reciprocal(out=ep[2][:], in_=banks[6][:]), reads=[bbank[6]], writes=[bep[2]])
                    P.op("dve", lambda e: e.tensor_tensor(out=ep[3][:], in0=banks[4][:], in1=ep[2][:], op=ALU.mult),
                         reads=[bbank[4], bep[2]], writes=[bep[3]])
                    P.op("dve", lambda e: e.scalar_tensor_tensor(out=ep[4][:], in0=ep[3][:], scalar=NEGLAM, in1=ep[1][:], op0=ALU.mult, op1=ALU.add),
                         reads=[bep[3], bep[1], blam], writes=[bep[4]])
                    P.op("act", lambda e: e.activation(out=epq[:], in_=ep[4][:], func=AF.Square), reads=[bep[4]], writes=[bepq])

                    def tail(osl=osl, otile=otile, Qb=Qb):
                        P.op("pe", lambda e: e.matmul(banks[7][:], lhsT=ONES, rhs=epq[:], start=True, stop=True), reads=[bCM, bepq], writes=[bbank[7]])
                        P.op("act", lambda e: e.activation(out=ep[5][:], in_=banks[7][:], func=AF.Sqrt, bias=epsc[:, 0:1], scale=1.0 / 128),
                             reads=[bbank[7], beps], writes=[bep[5]])
                        P.op("dve", lambda e: e.reciprocal(out=ep[5][:], in_=ep[5][:]), reads=[bep[5]], writes=[bep[5]])
                        P.op("dve", lambda e, osl=osl: e.scalar_tensor_tensor(out=OTs[osl][:], in0=ep[4][:], scalar=SUBG, in1=ep[5][:], op0=ALU.mult, op1=ALU.mult),
                             reads=[bep[4], bep[5], bg_], writes=[bOTs[osl]])
                        P.op("sp", lambda e, osl=osl, otile=otile, Qb=Qb: e.dma_start(out=OT_d[otile, :, Qb * 512:(Qb + 1) * 512], in_=OTs[osl][:]),
                             reads=[bOTs[osl]], dma=s_ot[osl])
                    deferred.append(tail)
        for fdef in deferred:
            fdef()
        deferred.clear()
    if "dbgQ" in debug:
        dq = dt("dbgQ", [128, NQ], BF16, kind="ExternalOutput").ap()
        dk = dt("dbgK", [128, S], BF16, kind="ExternalOutput").ap()
        dv = dt("dbgV", [128, NTC * 256], BF16, kind="ExternalOutput").ap()
        sdbg = P.sem()
        de = dt("dbgE", [6, 128, 512], F32, kind="ExternalOutput").ap()
        for i6 in range(6):
            P.op("sp", lambda e, i6=i6: e.dma_start(out=de[i6], in_=ep[i6][:]), reads=[bep[i6]], dma=sdbg)
        dp = dt("dbgP", [4, 128, 512], BF16, kind="ExternalOutput").ap()
        for i4 in range(4):
            P.op("sp", lambda e, i4=i4: e.dma_start(out=dp[i4], in_=PT[i4][:]), reads=[bPT[i4]], dma=sdbg)
        P.op("sp", lambda e: e.dma_start(out=dq, in_=QT[1][:]), reads=[bQT[1]], dma=sdbg)
        P.op("sp", lambda e: e.dma_start(out=dk, in_=KT[1][:]), reads=[bKT[1]], dma=sdbg)
        P.op("sp", lambda e: e.dma_start(out=dv, in_=VV[:].rearrange("p a b -> p (a b)")), reads=[bVV], dma=sdbg)
    end_phase()
    if stop_after == "B":
        return nc, P
    IOA = bass.IndirectOffsetOnAxis
    dest_i = SBG("dest_i", [128, NQC, 4], I32); bdest = Buf()
    dsc_i = SBG("dsc_i", [128, NQC, 4], I32)
    gk = SBG("gk", [128, NQC, 4], F32); bgk = Buf()
    wo = SB("wo", [128, 16, D], BF16); bwo = Buf(); s_wo = P.sem()
    P.op("pool", lambda e: e.dma_start(out=wo[:], in_=w_out.rearrange("(k p) n -> p k n", p=128)), writes=[bwo], dma=s_wo)
    mrep = SB("mrep", [128, 3, D], F32); bmr = Buf(); s_mr = P.sem()
    for i3, seg in enumerate((2, 3, 4)):
        P.op("sp", lambda e, i3=i3, seg=seg: e.dma_start(out=mrep[:, i3, :], in_=mod_d[:, seg * D:(seg + 1) * D]), writes=[bmr], dma=s_mr)
    wr = SB("wr", [128, DC, E], BF16); bwr = Buf(); s_wr = P.sem()
    P.op("pool", lambda e: e.dma_start(out=wr[:], in_=w_router.rearrange("(c p) n -> p c n", p=128)), writes=[bwr], dma=s_wr)
    brr = SB("brr", [128, E], F32); iot = SB("iot", [128, E], F32); ecap = SB("ecap", [128, E], F32); bconst = Buf(); s_cn = P.sem()
    P.op("sp", lambda e: e.dma_start(out=brr[:], in_=brout_rep), writes=[bconst], dma=s_cn)
    P.op("sp", lambda e: e.dma_start(out=iot[:], in_=iota_e), writes=[bconst], dma=s_cn)
    prw = SB("prw", [128, E], F32)
    P.op("sp", lambda e: e.dma_start(out=prw[:], in_=padrow), writes=[bconst], dma=s_cn)
    P.op("dve", lambda e: e.tensor_scalar_mul(out=ecap[:], in0=iot[:], scalar1=float(CAP)), reads=[bconst], writes=[bconst])
    base = SB("base", [128, E], F32); bbase = Buf()
    P.op("dve", lambda e: e.memset(base[:], 0.0), writes=[bbase])
    zt = SB("zt", [128, D], BF16); bzt = Buf(); s_z = P.sem()
    P.op("dve", lambda e: e.memset(zt[:], 0.0), writes=[bzt])
    zf = SB("zf", [128, D], F32); bzf = Buf()
    P.op("dve", lambda e: e.memset(zf[:], 0.0), writes=[bzf])
    s_zy = P.sem()
    for r0 in range(0, NROW, 128):
        P.op("act", lambda e, r0=r0: e.dma_start(out=ys_d[r0:r0 + 128, :], in_=zf[:]), reads=[bzf], dma=s_zy)
    bhs_d = Buf()
    for r0 in range(0, NROW, 128):
        nr = min(128, NROW - r0)
        P.op("sp", lambda e, r0=r0, nr=nr: e.dma_start(out=hs_d[r0:r0 + nr, :], in_=zt[0:nr, :]), reads=[bzt], dma=s_z)
    OTc = SB("OTc", [128, 16, 128], BF16); bOTc = Buf(); s_oc = P.sem()
    xq = SB("xq", [128, D], F32); bxq = Buf(); s_xq = P.sem()
    x1 = SB("x1", [128, D], F32); bx1 = Buf(); s_x1 = P.sem()
    tmpc = SB("tmpc", [128, D], F32); btmpc = Buf()
    junkc = SB("junkc", [128, D], BF16); bjc = Buf()
    ssc = SB("ssc", [128, 2], F32); bssc = Buf()
    h2 = SB("h2", [128, D], BF16); bh2 = Buf(); s_sc = P.sem()
    h2T = SB("h2T", [128, DC, 128], BF16); bh2T = Buf()
    lg = SB("lg", [128, E], F32); blg = Buf()
    mx8 = SB("mx8", [128, 8], F32); negm = SB("negm", [128, 1], F32)
    msk = SB("msk", [128, E], F32); mskb = SB("mskb", [128, E], BF16)
    ex = SB("ex", [128, E], F32); gat = SB("gat", [128, E], F32); rsum = SB("rsum", [128, 2], F32)
    dst = SB("dst", [128, E], F32); oh = SB("oh", [128, E], F32); prod = SB("prod", [128, E], F32)
    dk = SB("dk", [128, 4], F32)
    dks = SB("dks", [128, 4], F32)
    dsts = SB("dsts", [128, E], F32)
    ovf = SB("ovf", [128, E], F32)
    brt = Buf()
    OTv = OT_d.rearrange("k p s -> p k s")
    for t in range(NQC):
        P.op("sp", lambda e, t=t: e.dma_start(out=OTc[:], in_=OTv[:, :, t * 128:(t + 1) * 128]), writes=[bOTc], dma=s_oc)
        P.op("sp", lambda e, t=t: e.dma_start(out=xq[:], in_=xkv[t * 128:(t + 1) * 128, :]), writes=[bxq], dma=s_xq)
        for cb in range(D // 512):
            bk = cb % 2
            for k in range(16):
                P.op("pe", lambda e, bk=bk, k=k, cb=cb: e.matmul(banks[bk][:], lhsT=OTc[:, k, :], rhs=wo[:, k, cb * 512:(cb + 1) * 512],
                                                               start=(k == 0), stop=(k == 15)),
                     reads=[bOTc, bwo], writes=[bbank[bk]], sig=(k == 15))
            P.op("dve", lambda e, bk=bk, cb=cb: e.tensor_tensor(out=tmpc[:, cb * 512:(cb + 1) * 512], in0=banks[bk][:],
                                                                in1=mrep[:, 0, cb * 512:(cb + 1) * 512], op=ALU.mult),
                 reads=[bbank[bk], bmr], writes=[btmpc])
            P.op("dve", lambda e, cb=cb: e.tensor_tensor(out=x1[:, cb * 512:(cb + 1) * 512], in0=tmpc[:, cb * 512:(cb + 1) * 512],
                                                         in1=xq[:, cb * 512:(cb + 1) * 512], op=ALU.add),
                 reads=[btmpc, bxq], writes=[bx1])
        P.op("sp", lambda e, t=t: e.dma_start(out=out[t * 128:(t + 1) * 128, :], in_=x1[:]), reads=[bx1], dma=s_x1)
        P.op("act", lambda e: e.activation(out=junkc[:], in_=x1[:], func=AF.Square, accum_out=ssc[:, 0:1]), reads=[bx1], writes=[bjc, bssc])
        P.op("act", lambda e: e.activation(out=ssc[:, 1:2], in_=ssc[:, 0:1], func=AF.Sqrt, bias=epsc[:, 0:1], scale=1.0 / D),
             reads=[bssc, beps], writes=[bssc])
        P.op("dve", lambda e: e.reciprocal(out=ssc[:, 1:2], in_=ssc[:, 1:2]), reads=[bssc], writes=[bssc])
        P.op("dve", lambda e: e.scalar_tensor_tensor(out=tmpc[:], in0=x1[:], scalar=ssc[:, 1:2], in1=mrep[:, 2, :], op0=ALU.mult, op1=ALU.mult),
             reads=[bx1, bssc, bmr], writes=[btmpc])
        P.op("dve", lambda e: e.tensor_tensor(out=h2[:], in0=tmpc[:], in1=mrep[:, 1, :], op=ALU.add), reads=[btmpc, bmr], writes=[bh2])
        for j in range(DC):
            bk = 2 + j % 2
            pv = banks[bk][:].bitcast(BF16)
            P.op("pe", lambda e, j=j, pv=pv: e.transpose(out=pv[:, 0:128], in_=h2[:, j * 128:(j + 1) * 128], identity=IDENT),
                 reads=[bh2, bCM], writes=[bbank[bk]])
            P.op("act", lambda e, j=j, pv=pv: e.copy(out=h2T[:, j, :], in_=pv[:, 0:128]), reads=[bbank[bk]], writes=[bh2T])
        for j in range(DC):
            P.op("pe", lambda e, j=j: e.matmul(banks[4][:, 0:E], lhsT=h2T[:, j, :], rhs=wr[:, j, :], start=(j == 0), stop=(j == DC - 1)),
                 reads=[bh2T, bwr], writes=[bbank[4]], sig=(j == DC - 1))
        R_ = dict(reads=[brt], writes=[brt])
        P.op("dve", lambda e: e.tensor_tensor(out=lg[:], in0=banks[4][:, 0:E], in1=brr[:], op=ALU.add), reads=[bbank[4], bconst, brt], writes=[brt])
        P.op("dve", lambda e: e.max(out=mx8[:], in_=lg[:]), **R_)
        P.op("dve", lambda e: e.tensor_scalar(out=msk[:], in0=lg[:], scalar1=mx8[:, 3:4], scalar2=None, op0=ALU.is_ge), **R_)
        P.op("dve", lambda e: e.tensor_copy(out=mskb[:], in_=msk[:]), **R_)
        P.op("dve", lambda e: e.tensor_scalar_mul(out=negm[:], in0=mx8[:, 0:1], scalar1=-1.0), **R_)
        P.op("act", lambda e: e.activation(out=ex[:], in_=lg[:], func=AF.Exp, bias=negm[:, 0:1], scale=1.0), **R_)
        P.op("dve", lambda e: e.tensor_tensor(out=ex[:], in0=ex[:], in1=msk[:], op=ALU.mult), **R_)
        P.op("dve", lambda e: e.reduce_sum(out=rsum[:, 0:1], in_=ex[:], axis=AX.X), **R_)
        P.op("dve", lambda e: e.reciprocal(out=rsum[:, 1:2], in_=rsum[:, 0:1]), **R_)
        P.op("dve", lambda e: e.tensor_scalar(out=gat[:], in0=ex[:], scalar1=rsum[:, 1:2], scalar2=None, op0=ALU.mult), **R_)
        P.op("pe", lambda e: e.matmul(banks[5][:, 0:E], lhsT=TRI, rhs=mskb[:], start=True, stop=True), reads=[bCM, brt], writes=[bbank[5]])
        P.op("pe", lambda e: e.matmul(banks[5][:, E:2 * E], lhsT=ONES, rhs=mskb[:], start=True, stop=True), reads=[bCM, brt], writes=[bbank[5]])
        P.op("dve", lambda e: e.tensor_tensor(out=dst[:], in0=banks[5][:, 0:E], in1=base[:], op=ALU.add), reads=[bbank[5], bbase, brt], writes=[brt])
        P.op("dve", lambda e: e.tensor_scalar(out=ovf[:], in0=dst[:], scalar1=float(CAP), scalar2=None, op0=ALU.is_ge), **R_)
        P.op("dve", lambda e: e.tensor_tensor(out=dst[:], in0=dst[:], in1=ecap[:], op=ALU.add), reads=[bconst, brt], writes=[brt])
        P.op("dve", lambda e: e.tensor_tensor(out=prod[:], in0=prw[:], in1=dst[:], op=ALU.subtract), reads=[bconst, brt], writes=[brt])
        P.op("dve", lambda e: e.tensor_tensor(out=prod[:], in0=prod[:], in1=ovf[:], op=ALU.mult), **R_)
        P.op("dve", lambda e: e.tensor_tensor(out=dst[:], in0=dst[:], in1=prod[:], op=ALU.add), **R_)
        P.op("dve", lambda e: e.scalar_tensor_tensor(out=dsts[:], in0=ovf[:], scalar=1.0e6, in1=dst[:], op0=ALU.mult, op1=ALU.add), **R_)
        P.op("dve", lambda e: e.tensor_tensor(out=base[:], in0=banks[5][:, E:2 * E], in1=base[:], op=ALU.add), reads=[bbank[5], bbase], writes=[bbase])
        for k in range(4):
            P.op("dve", lambda e, k=k: e.tensor_scalar(out=oh[:], in0=lg[:], scalar1=mx8[:, k:k + 1], scalar2=None, op0=ALU.is_equal), **R_)
            P.op("dve", lambda e: e.tensor_tensor(out=prod[:], in0=oh[:], in1=dst[:], op=ALU.mult), **R_)
            P.op("dve", lambda e, k=k: e.reduce_sum(out=dk[:, k:k + 1], in_=prod[:], axis=AX.X), **R_)
            P.op("dve", lambda e: e.tensor_tensor(out=prod[:], in0=oh[:], in1=dsts[:], op=ALU.mult), **R_)
            P.op("dve", lambda e, k=k: e.reduce_sum(out=dks[:, k:k + 1], in_=prod[:], axis=AX.X), **R_)
            P.op("dve", lambda e: e.tensor_tensor(out=prod[:], in0=oh[:], in1=gat[:], op=ALU.mult), **R_)
            P.op("dve", lambda e, k=k, t=t: e.reduce_sum(out=gk[:, t, k:k + 1], in_=prod[:], axis=AX.X), reads=[brt], writes=[brt, bgk])
        P.op("dve", lambda e, t=t: e.tensor_copy(out=dest_i[:, t, :], in_=dk[:]), reads=[brt], writes=[bdest])
        P.op("dve", lambda e, t=t: e.tensor_copy(out=dsc_i[:, t, :], in_=dks[:]), reads=[brt], writes=[bdest])
        for k in range(4):
            P.op("pool", lambda e, t=t, k=k: e.indirect_dma_start(out=hs_d[:, :], out_offset=IOA(ap=dsc_i[:, t, k:k + 1], axis=0),
                                                                  in_=h2[:, :], in_offset=None, bounds_check=NROW - 1, oob_is_err=False),
                 reads=[bh2, bdest], extra=[(s_z, s_z.n)], dma=s_sc)
    cnt_i = SB("cnt_i", [128, E], I32); bcnt = Buf(); s_cnt = P.sem()
    P.op("dve", lambda e: e.tensor_copy(out=cnt_i[:], in_=base[:]), reads=[bbase], writes=[bcnt])
    P.op("sp", lambda e: e.dma_start(out=cnt_d, in_=cnt_i[0:1, :]), reads=[bcnt], dma=s_cnt)
    end_phase()
    if stop_after == "C":
        return nc, P

    bgu = SB("bgu", [128, E * 2 * FC], F32); bbgu = Buf(); s_bg = P.sem()
    P.op("sp", lambda e: e.dma_start(out=bgu[:], in_=bgu_fm), writes=[bbgu], dma=s_bg)
    rows = [SB(f"rows{i}", [128, D], BF16) for i in range(2)]; brow = [Buf(), Buf()]; s_row = [P.sem(), P.sem()]
    XT = SB("XT", [128, DC, CAP], BF16); bXT = Buf()
    wgb = [SB(f"wgb{i}", [128, DC, 512], BF16) for i in range(2)]; bwgb = [Buf(), Buf()]; s_wgb = [P.sem(), P.sem()]
    wub = [SB(f"wub{i}", [128, DC, 512], BF16) for i in range(2)]; bwub = [Buf(), Buf()]; s_wub = [P.sem(), P.sem()]
    wdb = [SB(f"wdb{i}", [128, FC, 512], BF16) for i in range(2)]; bwdb = [Buf(), Buf()]; s_wdb = [P.sem(), P.sem()]
    actT = SB("actT", [128, FC, CAP], BF16); bact = Buf()
    NH = 1 if CAP <= 512 else 2
    HW_ = CAP // NH
    gt = SB("gt", [128, HW_], F32); sg = SB("sg", [128, HW_], F32); u1 = SB("u1", [128, HW_], F32); glu = SB("glu", [128, HW_], F32)
    bgt = Buf(); bsg = Buf(); bu1 = Buf(); bglu = Buf()
    bdn = SB("bdn", [128, D], F32); bbdn = Buf(); s_bdn = P.sem()
    chunks = [(s0, min(128, CAP - s0)) for s0 in range(0, CAP, 128)]
    ybuf = [SB(f"ybuf{i}", [128, 512], F32) for i in range(4)]; bybuf = [Buf() for _ in range(4)]; s_y = [P.sem() for _ in range(4)]
    ri = 0; wi = 0; di = 0; yi = 0; gi_ = 0
    NFB = DFF // 512 if DFF >= 512 else 1
    FBW = min(512, DFF)
    GW_ = 256
    NG = CAP // GW_
    assert CAP % GW_ == 0
    dyn = getattr(cfg, "dynamic", True)

    def cond_begin(thr):
        if dyn:
            P.begin_cond(thr)

    def cond_end():
        if dyn:
            P.end_cond()

    for ex_ in range(E):
        if dyn:
            P.load_count(cnt_d[0:1, ex_:ex_ + 1])
        for g in range(NG):
            cond_begin(GW_ * g)
            for ci in range(2 * g, 2 * g + 2):
                s0, n = chunks[ci]
                rs = ri % 2; ri += 1
                P.op("sp", lambda e, rs=rs, s0=s0, n=n, ex_=ex_: e.dma_start(out=rows[rs][0:n, :], in_=hs_d[ex_ * CAP + s0:ex_ * CAP + s0 + n, :]),
                     writes=[brow[rs]], dma=s_row[rs])
                for j in range(DC):
                    bk = 6 + j % 2
                    pv = banks[bk][:].bitcast(BF16)
                    P.op("pe", lambda e, rs=rs, j=j, n=n, pv=pv: e.transpose(out=pv[:, 0:n], in_=rows[rs][0:n, j * 128:(j + 1) * 128], identity=IDENT[0:n, 0:n]),
                         reads=[brow[rs], bCM], writes=[bbank[bk]])
                    P.op("act", lambda e, j=j, s0=s0, n=n, pv=pv: e.copy(out=XT[:, j, s0:s0 + n], in_=pv[:, 0:n]), reads=[bbank[bk]], writes=[bXT])
            cond_end()
        P.op("sp", lambda e, ex_=ex_: e.dma_start(out=bdn[:], in_=bass.AP(tensor=b_down.tensor, offset=ex_ * D, ap=[[0, 128], [1, D]])),
             writes=[bbdn], dma=s_bdn)
        for fb in range(NFB):
            ws = wi % 2; wi += 1
            P.op("pool", lambda e, ws=ws, fb=fb, ex_=ex_: e.dma_start(out=wgb[ws][:, :, 0:FBW], in_=w_gu[ex_].rearrange("(c p) n -> p c n", p=128)[:, :, fb * FBW:(fb + 1) * FBW]),
                 writes=[bwgb[ws]], dma=s_wgb[ws])
            P.op("pool", lambda e, ws=ws, fb=fb, ex_=ex_: e.dma_start(out=wub[ws][:, :, 0:FBW], in_=w_gu[ex_].rearrange("(c p) n -> p c n", p=128)[:, :, DFF + fb * FBW:DFF + (fb + 1) * FBW]),
                 writes=[bwub[ws]], dma=s_wub[ws])
            for g in range(NG):
                h0 = g * GW_
                cond_begin(GW_ * g)
                for f4 in range(FBW // 128):
                    fc = fb * (FBW // 128) + f4
                    cg = ex_ * 2 * FC + fc
                    cu = ex_ * 2 * FC + FC + fc
                    gb = (gi_ % 2) * 2; gi_ += 1
                    for j in range(DC):
                        P.op("pe", lambda e, ws=ws, j=j, f4=f4, gb=gb, h0=h0: e.matmul(banks[gb][:, 0:GW_], lhsT=wgb[ws][:, j, f4 * 128:(f4 + 1) * 128],
                                                                                      rhs=XT[:, j, h0:h0 + GW_], start=(j == 0), stop=(j == DC - 1)),
                             reads=[bwgb[ws], bXT], writes=[bbank[gb]], sig=(j == DC - 1))
                    for j in range(DC):
                        P.op("pe", lambda e, ws=ws, j=j, f4=f4, gb=gb, h0=h0: e.matmul(banks[gb + 1][:, 0:GW_], lhsT=wub[ws][:, j, f4 * 128:(f4 + 1) * 128],
                                                                                      rhs=XT[:, j, h0:h0 + GW_], start=(j == 0), stop=(j == DC - 1)),
                             reads=[bwub[ws], bXT], writes=[bbank[gb + 1]], sig=(j == DC - 1))
                    P.op("dve", lambda e, cg=cg, gb=gb: e.tensor_scalar(out=gt[:, 0:GW_], in0=banks[gb][:, 0:GW_], scalar1=bgu[:, cg:cg + 1], scalar2=LIM, op0=ALU.add, op1=ALU.min),
                         reads=[bbank[gb], bbgu], writes=[bgt])
                    P.op("act", lambda e: e.activation(out=sg[:, 0:GW_], in_=gt[:, 0:GW_], func=AF.Sigmoid, scale=ALPHA), reads=[bgt], writes=[bsg])
                    P.op("dve", lambda e, cu=cu, gb=gb: e.tensor_scalar(out=u1[:, 0:GW_], in0=banks[gb + 1][:, 0:GW_], scalar1=bgu[:, cu:cu + 1], scalar2=LIM, op0=ALU.add, op1=ALU.min),
                         reads=[bbank[gb + 1], bbgu], writes=[bu1])
                    P.op("dve", lambda e: e.tensor_scalar(out=u1[:, 0:GW_], in0=u1[:, 0:GW_], scalar1=-LIM, scalar2=1.0, op0=ALU.max, op1=ALU.add), reads=[bu1], writes=[bu1])
                    P.op("dve", lambda e: e.tensor_tensor(out=glu[:, 0:GW_], in0=gt[:, 0:GW_], in1=sg[:, 0:GW_], op=ALU.mult), reads=[bgt, bsg], writes=[bglu])
                    P.op("dve", lambda e, fc=fc, h0=h0: e.tensor_tensor(out=actT[:, fc, h0:h0 + GW_], in0=u1[:, 0:GW_], in1=glu[:, 0:GW_], op=ALU.mult), reads=[bu1, bglu], writes=[bact])
                cond_end()
        for cb in range(D // 512):
            ds_ = di % 2; di += 1
            P.op("pool", lambda e, ds_=ds_, cb=cb, ex_=ex_: e.dma_start(out=wdb[ds_][:], in_=w_down[ex_].rearrange("(c p) n -> p c n", p=128)[:, :, cb * 512:(cb + 1) * 512]),
                 writes=[bwdb[ds_]], dma=s_wdb[ds_])
            for g in range(NG):
                cond_begin(GW_ * g)
                for ci in range(2 * g, 2 * g + 2):
                    s0, n = chunks[ci]
                    bk = 4 + (ci % 2)
                    ys = yi % 4; yi += 1
                    for fc in range(FC):
                        P.op("pe", lambda e, bk=bk, fc=fc, s0=s0, n=n, ds_=ds_: e.matmul(banks[bk][0:n, :], lhsT=actT[:, fc, s0:s0 + n], rhs=wdb[ds_][:, fc, :],
                                                                                      start=(fc == 0), stop=(fc == FC - 1)),
                             reads=[bact, bwdb[ds_]], writes=[bbank[bk]], sig=(fc == FC - 1))
                    P.op("dve", lambda e, bk=bk, ys=ys, n=n, cb=cb: e.tensor_tensor(out=ybuf[ys][0:n, :], in0=banks[bk][0:n, :],
                                                                                in1=bdn[0:n, cb * 512:(cb + 1) * 512], op=ALU.add),
                         reads=[bbank[bk], bbdn], writes=[bybuf[ys]])
                    P.op("sp", lambda e, ys=ys, s0=s0, n=n, ex_=ex_, cb=cb: e.dma_start(out=ys_d[ex_ * CAP + s0:ex_ * CAP + s0 + n, cb * 512:(cb + 1) * 512], in_=ybuf[ys][0:n, :]),
                         reads=[bybuf[ys]], dma=s_y[ys])
                cond_end()
    end_phase()
    if stop_after == "D":
        return nc, P

    gfr = SB("gfr", [128, D], F32); bgfr = Buf(); s_gf = P.sem()
    P.op("sp", lambda e: e.dma_start(out=gfr[:], in_=mod_d[:, 5 * D:6 * D]), writes=[bgfr], dma=s_gf)
    Yk = [SB(f"Yk{i}", [128, D], F32) for i in range(4)]; bYk = [Buf() for _ in range(4)]; s_yk = [P.sem() for _ in range(4)]
    x1e = SB("x1e", [128, D], F32); bx1e = Buf(); s_x1e = P.sem()
    acc = SB("acc", [128, D], F32); bacc = Buf()
    fin = SB("fin", [128, D], F32); bfin = Buf(); s_fin = P.sem()
    for t in range(NQC):
        P.op("sp", lambda e, t=t: e.dma_start(out=x1e[:], in_=out[t * 128:(t + 1) * 128, :]), writes=[bx1e], dma=s_x1e)
        for k in range(4):
            P.op("pool", lambda e, t=t, k=k: e.indirect_dma_start(out=Yk[k][:, :], out_offset=None, in_=ys_d[:, :],
                                                                  in_offset=IOA(ap=dest_i[:, t, k:k + 1], axis=0)),
                 reads=[bdest], writes=[bYk[k]], dma=s_yk[k])
        P.op("dve", lambda e, t=t: e.tensor_scalar(out=acc[:], in0=Yk[0][:], scalar1=gk[:, t, 0:1], scalar2=None, op0=ALU.mult),
             reads=[bYk[0], bgk], writes=[bacc])
        for k in range(1, 4):
            P.op("dve", lambda e, t=t, k=k: e.scalar_tensor_tensor(out=acc[:], in0=Yk[k][:], scalar=gk[:, t, k:k + 1], in1=acc[:], op0=ALU.mult, op1=ALU.add),
                 reads=[bYk[k], bgk, bacc], writes=[bacc])
        P.op("dve", lambda e: e.tensor_tensor(out=fin[:], in0=acc[:], in1=gfr[:], op=ALU.mult), reads=[bacc, bgfr], writes=[bfin])
        P.op("dve", lambda e: e.tensor_tensor(out=fin[:], in0=fin[:], in1=x1e[:], op=ALU.add), reads=[bfin, bx1e], writes=[bfin])
        P.op("sp", lambda e, t=t: e.dma_start(out=out[t * 128:(t + 1) * 128, :], in_=fin[:]), reads=[bfin, bx1e], dma=s_fin)
    end_phase()
    return nc, P


def _rel_bucket_np(rp):
    nb = 16
    max_exact = 8
    ret = np.where(rp > 0, nb, 0)
    n = np.abs(rp)
    nf = np.maximum(n, 1).astype(np.float32)
    large = max_exact + (np.log(nf / np.float32(max_exact)) / np.float32(math.log(128 / max_exact))
                         * np.float32(nb - max_exact)).astype(np.int32)
    large = np.minimum(large, nb - 1)
    return ret + np.where(n < max_exact, n, large)


def _static_tables(cfg, hf):
    S = cfg.S
    order = np.arange(S) if hf == 0 else np.arange(S)[::-1].copy()
    row = (order // 64).astype(np.float32)
    col = (order % 64).astype(np.float32)
    inv = (np.float32(10000.0) ** (-np.arange(0, 64, 2, dtype=np.float32) / np.float32(64))).astype(np.float32)
    ang = np.concatenate([row[:, None] * inv, col[:, None] * inv], axis=-1).astype(np.float32)
    c = np.cos(ang).astype(np.float32).T
    s = np.sin(ang).astype(np.float32).T
    ropec = np.ascontiguousarray(np.concatenate([c, c], 0))
    ropes = np.ascontiguousarray(np.concatenate([-s, s], 0))
    r = np.arange(-FEXT, FEXT + 2)
    sgn = 1 if hf == 0 else -1
    bk = _rel_bucket_np(sgn * r)
    ohr = (bk[None, :] == np.arange(32)[:, None]).astype(np.float32)
    cm = np.zeros((6, 128, 128), np.float32)
    cm[0] = np.eye(128)
    cm[1] = 1.0
    cm[2] = np.eye(128)[::-1]
    cm[3] = np.roll(np.eye(128), 64, axis=0)
    cm[4] = np.triu(np.ones((128, 128)), 1)
    cm[5, :64, :64] = 1.0
    cm[5, 64:, 64:] = 1.0
    cmat = np.ascontiguousarray(cm.transpose(1, 0, 2).reshape(128, 6 * 128))
    return order, ropec, ropes, ohr, cmat


def _win_cols():
    perm = np.concatenate([np.arange(0, 128, 2), np.arange(1, 128, 2)])
    cols = []
    for a in range(2):
        for h in range(4 * a, 4 * a + 4):
            cols.append(h * 128 + perm)
        cols.append(1024 + a * 128 + perm)
        cols.append(1280 + a * 128 + np.arange(128))
    for bg in range(4):
        for base in (1536, 2560, 3584):
            for h in (2 * bg, 2 * bg + 1):
                cols.append(base + h * 128 + np.arange(128))
    return np.concatenate(cols), perm


def prep_inputs(cfg, inp):
    D, S, E, DFF, DC, FC, NQ = cfg.D, cfg.S, cfg.E, cfg.DFF, cfg.DC, cfg.FC, cfg.NQ
    f = lambda a: np.ascontiguousarray(np.asarray(a, dtype=np.float32))
    rep = lambda v: np.ascontiguousarray(np.broadcast_to(np.asarray(v, np.float32).reshape(1, -1), (128, np.asarray(v).size)))
    cols, perm = _win_cols()
    shared = {
        "w_ada": f(inp["w_ada"][0]),
        "bada_rep": rep(inp["b_ada"][0]),
        "gn_rep": rep(np.concatenate([inp["norm_attn"][0], inp["norm_ffn"][0]])),
        "win_g": f(np.asarray(inp["w_in"][0])[:, cols]),
        "w_out": f(inp["w_out"][0]),
        "gains": f(np.stack([np.asarray(inp["a_q_norm"][0])[perm], np.asarray(inp["a_k_norm"][0])[perm],
                             np.tile(np.asarray(inp["b_q_norm"][0]), 2), np.tile(np.asarray(inp["b_k_norm"][0]), 2),
                             np.asarray(inp["b_subln"][0])] + [np.zeros(128, np.float32)] * 3, axis=1)),
        "lam_rep": rep(np.concatenate([inp["lambda_q1"][0], inp["lambda_k1"][0], inp["lambda_q2"][0], inp["lambda_k2"][0]])),
        "relT": f(np.concatenate([inp["rel_bias_table"], inp["rel_bias_table"]], axis=1)),
        "w_router": f(inp["w_router"][0]),
        "brout_rep": rep(inp["b_router"][0]),
        "w_gu": f(inp["w_gu"][0]),
        "bgu_fm": f(np.asarray(inp["b_gu"][0]).reshape(E, 2 * FC, 128).transpose(2, 0, 1).reshape(128, E * 2 * FC)),
        "w_down": f(inp["w_down"][0]),
        "b_down": f(inp["b_down"][0]),
        "iota_e": rep(np.arange(E)),
        "padrow": np.ascontiguousarray(np.broadcast_to((E * cfg.CAP + np.arange(128, dtype=np.float32))[:, None], (128, E))),
    }
    maps, orders = [], []
    tabs = [_static_tables(cfg, hf) for hf in range(2)]
    x = np.asarray(inp["x"])
    c = np.asarray(inp["c"])
    for core in range(8):
        b, hf = core // 2, core % 2
        order, ropec, ropes, ohr, cmat = tabs[hf]
        m = dict(shared)
        m["xkv"] = f(x[b][order])
        m["c_fm"] = f(c[b].reshape(DC, 128).T)
        m["ropec"], m["ropes"], m["ohr"], m["cmat"] = ropec, ropes, ohr, cmat
        maps.append(m)
        orders.append(order)
    return maps, orders


_CACHE = {}


def kernel(**inputs):
    cfg = Cfg()
    if "nc" not in _CACHE:
        _CACHE["nc"] = build(cfg)[0]
    nc = _CACHE["nc"]
    maps, orders = prep_inputs(cfg, inputs)
    res = run_bass_kernel_spmd(nc, maps, core_ids=list(range(8)))
    out = np.zeros((4, cfg.S, cfg.D), np.float32)
    for core in range(8):
        b = core // 2
        out[b, orders[core][:cfg.NQ]] = res.results[core]["out"]
    return out
```

```python
import math
from contextlib import ExitStack
import numpy as np
import concourse.bass as bass
import concourse.mybir as mybir
from concourse.bass_utils import run_bass_kernel_spmd

F32 = mybir.dt.float32
BF16 = mybir.dt.bfloat16
I32 = mybir.dt.int32
AF = mybir.ActivationFunctionType
ALU = mybir.AluOpType
AX = mybir.AxisListType

EPS = 1e-6
HEAD = 128
TOPK = 4
LIM = 7.0
ALPHA = 1.702
LAM_INIT = 0.8 - 0.6 * math.exp(0.0)
FEXT = 640
ENG = ("pe", "act", "dve", "pool", "sp")


class Cfg:
    def __init__(self, D=2048, S=4096, E=32, DFF=2048, CAP=768):
        self.D, self.S, self.E, self.DFF, self.CAP = D, S, E, DFF, CAP
        self.DC = D // 128
        self.FC = DFF // 128
        self.NQ = S // 2
        self.NGRP = 6
        self.GW = 768


class Buf:
    __slots__ = ("w", "r")

    def __init__(self):
        self.w = {}
        self.r = {}


class Sem:
    def __init__(self, h):
        self.h = h
        self.n = 0


class _Rec:
    def __getattr__(self, name):
        def f(*a, **k):
            self.call = (name, a, k)
            return self
        return f


class Prog:
    def __init__(self, nc, sems):
        self.nc = nc
        self.free_sems = list(sems)
        self.prog = {e: Sem(self.free_sems.pop()) for e in ENG if e != "sp"}
        self.bar = Sem(self.free_sems.pop())
        self.ops = {e: [] for e in ENG}
        self.waited = {e: {} for e in ENG}
        self.pend_r = {e: [] for e in ENG}
        self.pend_w = {e: [] for e in ENG}
        self.touched = {e: {} for e in ENG}
        self.nphase = 0

    def sem(self):
        return Sem(self.free_sems.pop())

    def _deps(self, eng, reads, writes, extra):
        deps = {}

        def add(tok):
            s, v = tok
            if deps.get(s, (None, 0))[1] < v:
                deps[s] = (s, v)

        for b in reads:
            for t in b.w.values():
                add(t)
        for b in writes:
            for t in b.w.values():
                add(t)
            for t in b.r.values():
                add(t)
        for t in extra:
            if t is not None:
                add(t)
        out = []
        wd = self.waited[eng]
        for s, v in deps.values():
            if wd.get(s.h, 0) < v:
                wd[s.h] = v
                out.append((s.h, v))
        return out

    def op(self, eng, fn, reads=(), writes=(), sig=True, dma=None, extra=()):
        waits = self._deps(eng, reads, writes, extra)
        tok = None
        rec = _Rec()
        fn(rec)
        fn = rec.call
        if dma is not None:
            dma.n += 16
            tok = (dma, dma.n)
            self.ops[eng].append((fn, waits, dma.h, 16, dma.n))
            self.touched[eng][dma.h] = (dma, dma.n)
            for b in reads:
                b.r[("dma", dma.h)] = tok
            for b in writes:
                b.w = {("dma", dma.h): tok}
                b.r = {}
            return tok
        if sig:
            s = self.prog[eng]
            s.n += 1
            tok = (s, s.n)
            self.ops[eng].append((fn, waits, s.h, 1, s.n))
            self.touched[eng][s.h] = (s, s.n)
            for b in list(reads) + self.pend_r[eng]:
                b.r[eng] = tok
            for b in list(writes) + self.pend_w[eng]:
                b.w = {eng: tok}
                b.r = {}
            self.pend_r[eng] = []
            self.pend_w[eng] = []
        else:
            self.ops[eng].append((fn, waits, None, 0, 0))
            self.pend_r[eng] += list(reads)
            self.pend_w[eng] += list(writes)
        return tok

    def load_count(self, ap):
        for e in ENG:
            self.ops[e].append(("load", ap))

    def begin_cond(self, thr):
        self._snap = {e: dict(self.waited[e]) for e in ENG}
        for e in ENG:
            self.ops[e].append(("begin", thr))

    def end_cond(self):
        assert all(not v for v in self.pend_r.values()) and all(not v for v in self.pend_w.values())
        for e in ENG:
            self.ops[e].append(("end",))
        self.waited = self._snap

    def flush(self):
        nc = self.nc
        self.nphase += 1
        target = 5 * self.nphase
        ops = self.ops
        touched = self.touched
        bar = self.bar

        prog_h = {e: (self.prog[e].h if e in self.prog else None) for e in ENG}

        def mk(name):
            def body(e):
                regs = {}
                creg = [None]

                def run(item):
                    fn, waits, inc, amt, _after = item
                    for s, v in waits:
                        e.wait_ge(s, v)
                    kw = fn[2]
                    bc = kw.get("bounds_check")
                    if isinstance(bc, int):
                        if bc not in regs:
                            regs[bc] = e.to_reg(bc)
                        kw = dict(kw, bounds_check=regs[bc])
                    ins = getattr(e, fn[0])(*fn[1], **kw)
                    if inc is not None:
                        ins.then_inc(inc, amt)

                lst = ops[name]
                i = 0
                while i < len(lst):
                    item = lst[i]
                    if item[0] == "load":
                        lm = getattr(self, "load_engs", ENG)
                        if name not in lm:
                            i += 1
                            continue
                        if creg[0] is None:
                            creg[0] = e.alloc_register(name=f"cnt_{name}_{self.nphase}")
                        e.reg_load(creg[0], item[1])
                        i += 1
                    elif item[0] == "begin":
                        thr = item[1]
                        j = i + 1
                        while lst[j][0] != "end":
                            j += 1
                        region = lst[i + 1:j]
                        i = j + 1
                        if not region:
                            continue
                        acc = {}
                        order = []
                        for fn, waits, inc, amt, after in region:
                            if inc is None:
                                continue
                            if inc not in acc:
                                acc[inc] = [after - amt, 0]
                                order.append(inc)
                            acc[inc][1] += amt
                        mode = getattr(self, "cond_mode", "full")
                        if mode == "loadonly" or (isinstance(mode, tuple) and name not in mode):
                            for it in region:
                                run(it)
                            continue
                        with e.If_lt(creg[0], thr + 1):
                            for inc in order:
                                bef, tot = acc[inc]
                                if bef > 0:
                                    e.wait_ge(inc, bef)
                                e.nop().then_inc(inc, tot)
                        with e.Else():
                            for it in region:
                                run(it)
                    else:
                        run(item)
                        i += 1
                for s, v in touched[name].values():
                    e.wait_ge(s.h, v)
                e.nop().then_inc(bar.h, 1)
                e.wait_ge(bar.h, target)
            return body

        with nc.Block() as blk:
            blk.tensor(mk("pe"))
            blk.scalar(mk("act"))
            blk.vector(mk("dve"))
            blk.gpsimd(mk("pool"))
            blk.sync(mk("sp"))
        self.ops = {e: [] for e in ENG}
        self.touched = {e: {} for e in ENG}
        assert all(not v for v in self.pend_r.values()) and all(not v for v in self.pend_w.values())


def build(cfg, debug=None, stop_after=None):
    D, S, E, DFF, CAP, DC, FC, NQ = cfg.D, cfg.S, cfg.E, cfg.DFF, cfg.CAP, cfg.DC, cfg.FC, cfg.NQ
    NTC = S // 128
    NQC = NQ // 128
    NTB = S // 512
    NQB = NQ // 512
    NROW = (E + 1) * CAP
    nc = bass.Bass("TRN2", target_bir_lowering=False)
    dt = nc.dram_tensor

    xkv = dt("xkv", [S, D], F32, kind="ExternalInput").ap()
    c_fm = dt("c_fm", [128, DC], F32, kind="ExternalInput").ap()
    w_ada = dt("w_ada", [D, 6 * D], F32, kind="ExternalInput").ap()
    bada_rep = dt("bada_rep", [128, 6 * D], F32, kind="ExternalInput").ap()
    gn_rep = dt("gn_rep", [128, 2 * D], F32, kind="ExternalInput").ap()
    win_g = dt("win_g", [D, 6 * 768], F32, kind="ExternalInput").ap()
    w_out = dt("w_out", [2048, D], F32, kind="ExternalInput").ap()
    gains = dt("gains", [128, 8], F32, kind="ExternalInput").ap()
    lam_rep = dt("lam_rep", [128, 4 * 64], F32, kind="ExternalInput").ap()
    cmat = dt("cmat", [128, 6 * 128], F32, kind="ExternalInput").ap()
    ropec = dt("ropec", [128, S], F32, kind="ExternalInput").ap()
    ropes = dt("ropes", [128, S], F32, kind="ExternalInput").ap()
    relT = dt("relT", [32, 16], F32, kind="ExternalInput").ap()
    ohr = dt("ohr", [32, 2 * FEXT + 2], F32, kind="ExternalInput").ap()
    w_router = dt("w_router", [D, E], F32, kind="ExternalInput").ap()
    brout_rep = dt("brout_rep", [128, E], F32, kind="ExternalInput").ap()
    w_gu = dt("w_gu", [E, D, 2 * DFF], F32, kind="ExternalInput").ap()
    bgu_fm = dt("bgu_fm", [128, E * 2 * FC], F32, kind="ExternalInput").ap()
    w_down = dt("w_down", [E, DFF, D], F32, kind="ExternalInput").ap()
    b_down = dt("b_down", [E, D], F32, kind="ExternalInput").ap()
    iota_e = dt("iota_e", [128, E], F32, kind="ExternalInput").ap()
    padrow = dt("padrow", [128, E], F32, kind="ExternalInput").ap()
    out = dt("out", [NQ, D], F32, kind="ExternalOutput").ap()

    debug = debug or ()
    ds = lambda name, shape, dtype: dt(name, shape, dtype, kind=("ExternalOutput" if name in debug else "Internal")).ap()
    hT_d = ds("hT_d", [DC, 128, S], BF16)
    mod_d = ds("mod_d", [128, 6 * D], F32)
    OT_d = ds("OT_d", [16, 128, NQ], BF16)
    fext_d = ds("fext_d", [8, 2 * FEXT + 2], F32)
    hs_d = ds("hs_d", [NROW, D], BF16)
    ys_d = ds("ys_d", [NROW, D], F32)
    cnt_d = ds("cnt_d", [1, E], I32)

    P = Prog(nc, [nc.semaphore(f"s{i}").__enter__() for i in range(90)])
    SBG = lambda name, shape, dtype: nc.sbuf_tensor(name, shape, dtype).__enter__()
    PS = lambda name, shape, dtype=F32: nc.psum_tensor(name, shape, dtype).__enter__()
    es = [ExitStack()]
    SB = lambda name, shape, dtype: es[0].enter_context(nc.sbuf_tensor(name, shape, dtype))

    def end_phase():
        P.flush()
        es[0].close()
        es[0] = ExitStack()

    CM = SBG("CM", [128, 6, 128], BF16)
    bCM = Buf()
    dsem = P.sem()
    P.op("pool", lambda e: e.dma_start(out=CM[:], in_=cmat.rearrange("p (k n) -> p k n", k=6)), writes=[bCM], dma=dsem)
    IDENT, ONES, JM, SWP, TRI, BLK64 = (CM[:, i, :] for i in range(6))

    epsc = SBG("epsc", [128, 2], F32)
    beps = Buf()
    P.op("dve", lambda e: e.memset(epsc[:, 0:1], EPS), writes=[beps])
    P.op("dve", lambda e: e.memset(epsc[:, 1:2], 0.0), writes=[beps])
    zt = SBG("zt", [128, D], BF16); bzt = Buf()
    banks = [PS(f"bank{i}", [128, 512]) for i in range(8)]
    bbank = [Buf() for _ in range(8)]

    modrep = SB("modrep", [128, 6 * D], F32)
    bmod = [Buf() for _ in range(6)]
    cin = SB("cin", [128, DC], F32)
    cact = SB("cact", [128, DC], F32)
    crep = SB("crep", [128, DC, 128], BF16)
    bc = Buf(); bcr = Buf()
    s_c = P.sem()
    P.op("sp", lambda e: e.dma_start(out=cin[:], in_=c_fm), writes=[bc], dma=s_c)
    P.op("act", lambda e: e.activation(out=cact[:], in_=cin[:], func=AF.Silu), reads=[bc], writes=[bcr])
    P.op("dve", lambda e: e.tensor_copy(out=crep[:], in_=cact[:].unsqueeze(2).to_broadcast([128, DC, 128])),
         reads=[bcr], writes=[bcr])
    badar = [SB(f"badar{i}", [128, 512], F32) for i in range(2)]
    gnr = SB("gnr", [128, 2 * D], F32)
    bbada = [Buf(), Buf()]; bgn = Buf()
    s_b1 = [P.sem(), P.sem()]; s_b2 = P.sem()
    P.op("sp", lambda e: e.dma_start(out=gnr[:], in_=gn_rep), writes=[bgn], dma=s_b2)
    wa = [SB(f"wa{i}", [128, DC, 512], BF16) for i in range(2)]
    bwa = [Buf(), Buf()]
    s_wa = [P.sem(), P.sem()]
    wada_v = w_ada.rearrange("(c p) n -> p c n", p=128)
    NCB = 6 * D // 512
    for cb in range(NCB):
        sl = cb % 2
        P.op("pool", lambda e, sl=sl, cb=cb: e.dma_start(out=wa[sl][:], in_=wada_v[:, :, cb * 512:(cb + 1) * 512]),
             writes=[bwa[sl]], dma=s_wa[sl])
        bk = cb % 2
        P.op("sp", lambda e, sl=sl, cb=cb: e.dma_start(out=badar[sl][:], in_=bada_rep[:, cb * 512:(cb + 1) * 512]),
             writes=[bbada[sl]], dma=s_b1[sl])
        for j in range(DC):
            P.op("pe", lambda e, sl=sl, j=j, bk=bk: e.matmul(banks[bk][:], lhsT=crep[:, j, :], rhs=wa[sl][:, j, :],
                                                             start=(j == 0), stop=(j == DC - 1)),
                 reads=[bcr, bwa[sl]], writes=[bbank[bk]], sig=(j == DC - 1))
        seg = (cb * 512) // D
        P.op("dve", lambda e, bk=bk, cb=cb: e.tensor_tensor(out=modrep[:, cb * 512:(cb + 1) * 512], in0=banks[bk][:],
                                                            in1=badar[bk][:], op=ALU.add),
             reads=[bbank[bk], bbada[bk]], writes=[bmod[seg]])
    for seg, gi in ((1, 0), (4, 1)):
        P.op("dve", lambda e, seg=seg, gi=gi: e.scalar_tensor_tensor(
            out=modrep[:, seg * D:(seg + 1) * D], in0=modrep[:, seg * D:(seg + 1) * D], scalar=1.0,
            in1=gnr[:, gi * D:(gi + 1) * D], op0=ALU.add, op1=ALU.mult), reads=[bmod[seg], bgn], writes=[bmod[seg]])
    bmod_d = Buf()
    s_md = P.sem()
    P.op("sp", lambda e: e.dma_start(out=mod_d, in_=modrep[:]), reads=bmod, writes=[bmod_d], dma=s_md)

    xt = [SB(f"xt{i}", [128, D], F32) for i in range(2)]
    bxt = [Buf(), Buf()]
    s_xt = [P.sem(), P.sem()]
    junk = SB("junk", [128, D], BF16)
    bjunk = Buf()
    ssA = SB("ssA", [128, NTC], F32)
    rsA = SB("rsA", [128, NTC], F32)
    bss = [Buf() for _ in range(NTC)]
    tmpA = SB("tmpA", [128, D], F32)
    btmpA = Buf()
    hb = [SB(f"hb{i}", [128, 4, D], BF16) for i in range(2)]
    bhb = [Buf(), Buf()]
    hTb = [SB(f"hTb{i}", [128, DC, 512], BF16) for i in range(2)]
    bhTb = [Buf(), Buf()]
    s_hst = [P.sem(), P.sem()]
    bhT_d = Buf()
    hT_v = hT_d.rearrange("c p s -> p c s")
    for t in range(NTC):
        sl = t % 2
        g4 = (t // 4) % 2
        P.op("sp", lambda e, sl=sl, t=t: e.dma_start(out=xt[sl][:], in_=xkv[t * 128:(t + 1) * 128, :]),
             writes=[bxt[sl]], dma=s_xt[sl])
        P.op("act", lambda e, sl=sl, t=t: e.activation(out=junk[:], in_=xt[sl][:], func=AF.Square,
                                                       accum_out=ssA[:, t:t + 1]),
             reads=[bxt[sl]], writes=[bjunk, bss[t]])
        P.op("act", lambda e, t=t: e.activation(out=rsA[:, t:t + 1], in_=ssA[:, t:t + 1], func=AF.Sqrt,
                                                bias=epsc[:, 0:1], scale=1.0 / D), reads=[bss[t], beps], writes=[bss[t]])
        P.op("dve", lambda e, t=t: e.reciprocal(out=rsA[:, t:t + 1], in_=rsA[:, t:t + 1]), reads=[bss[t]], writes=[bss[t]])
        P.op("dve", lambda e, sl=sl, t=t: e.scalar_tensor_tensor(out=tmpA[:], in0=xt[sl][:], scalar=rsA[:, t:t + 1],
                                                                 in1=modrep[:, D:2 * D], op0=ALU.mult, op1=ALU.mult),
             reads=[bxt[sl], bss[t], bmod[1]], writes=[btmpA])
        P.op("dve", lambda e, g4=g4, t=t: e.tensor_tensor(out=hb[g4][:, t % 4, :], in0=tmpA[:], in1=modrep[:, 0:D],
                                                          op=ALU.add), reads=[btmpA, bmod[0]], writes=[bhb[g4]])
        if t % 4 == 3:
            tb = t // 4
            hs = tb % 2
            for j in range(DC):
                bk = 2 + (j % 2)
                pv = banks[bk][:].bitcast(BF16)
                for q in range(4):
                    P.op("pe", lambda e, g4=g4, j=j, q=q, pv=pv: e.transpose(
                        out=pv[:, q * 128:(q + 1) * 128], in_=hb[g4][:, q, j * 128:(j + 1) * 128], identity=IDENT),
                        reads=[bhb[g4], bCM], writes=[bbank[bk]], sig=(q == 3))
                P.op("act", lambda e, hs=hs, j=j, pv=pv: e.copy(out=hTb[hs][:, j, :], in_=pv[:, 0:512]),
                     reads=[bbank[bk]], writes=[bhTb[hs]])
            P.op("sp", lambda e, hs=hs, tb=tb: e.dma_start(out=hT_v[:, :, tb * 512:(tb + 1) * 512], in_=hTb[hs][:]),
                 reads=[bhTb[hs]], writes=[bhT_d], dma=s_hst[hs])
    end_phase()
    if stop_after == "A":
        return nc, P
    NW = 2 * FEXT + 2
    gsb = SB("gsb", [128, 8], F32); bg_ = Buf()
    lamr = SB("lamr", [128, 256], F32); blam = Buf()
    lamt = SB("lamt", [128, 128], F32)
    lams = SB("lams", [128, 4], F32)
    s_g = P.sem(); s_l = P.sem()
    P.op("sp", lambda e: e.dma_start(out=gsb[:], in_=gains), writes=[bg_], dma=s_g)
    P.op("sp", lambda e: e.dma_start(out=lamr[:], in_=lam_rep), writes=[blam], dma=s_l)
    P.op("dve", lambda e: e.tensor_tensor(out=lamt[:, 0:64], in0=lamr[:, 0:64], in1=lamr[:, 64:128], op=ALU.mult),
         reads=[blam], writes=[blam])
    P.op("dve", lambda e: e.tensor_tensor(out=lamt[:, 64:128], in0=lamr[:, 128:192], in1=lamr[:, 192:256], op=ALU.mult),
         reads=[blam], writes=[blam])
    P.op("dve", lambda e: e.reduce_sum(out=lams[:, 0:1], in_=lamt[:, 0:64], axis=AX.X), reads=[blam], writes=[blam])
    P.op("dve", lambda e: e.reduce_sum(out=lams[:, 1:2], in_=lamt[:, 64:128], axis=AX.X), reads=[blam], writes=[blam])
    P.op("act", lambda e: e.activation(out=lams[:, 0:2], in_=lams[:, 0:2], func=AF.Exp), reads=[blam], writes=[blam])
    P.op("dve", lambda e: e.tensor_tensor(out=lams[:, 3:4], in0=lams[:, 1:2], in1=lams[:, 0:1], op=ALU.subtract),
         reads=[blam], writes=[blam])
    P.op("dve", lambda e: e.tensor_scalar_add(out=lams[:, 2:3], in0=lams[:, 3:4], scalar1=-LAM_INIT), reads=[blam], writes=[blam])
    NEGLAM = lams[:, 2:3]
    P.op("dve", lambda e: e.tensor_scalar_mul(out=gsb[:, 5:6], in0=gsb[:, 4:5], scalar1=1.0 - LAM_INIT), reads=[bg_], writes=[bg_])
    SUBG = gsb[:, 5:6]

    tab32 = SB("tab32", [32, 16], F32); btab = Buf()
    tabh = SB("tabh", [32, 16], BF16)
    ohb = SB("ohb", [32, NW], BF16); boh = Buf()
    fx = SB("fx", [8, NW], F32); bfx = Buf()
    s_t = P.sem(); s_o = P.sem(); s_f = P.sem()
    P.op("sp", lambda e: e.dma_start(out=tab32[:], in_=relT), writes=[btab], dma=s_t)
    P.op("pool", lambda e: e.dma_start(out=ohb[:], in_=ohr), writes=[boh], dma=s_o)
    P.op("dve", lambda e: e.tensor_copy(out=tabh[:, 0:8], in_=tab32[:, 0:8]), reads=[btab], writes=[btab])
    P.op("dve", lambda e: e.tensor_tensor(out=tab32[:, 8:16], in0=tab32[:, 8:16], in1=tabh[:, 0:8], op=ALU.subtract),
         reads=[btab], writes=[btab])
    P.op("dve", lambda e: e.tensor_copy(out=tabh[:, 8:16], in_=tab32[:, 8:16]), reads=[btab], writes=[btab])
    for pc in range(0, NW, 512):
        n = min(512, NW - pc)
        P.op("pe", lambda e, pc=pc, n=n: e.matmul(banks[7][0:8, 0:n], lhsT=tabh[:, 0:8], rhs=ohb[:, pc:pc + n], start=True, stop=False),
             reads=[btab, boh], writes=[bbank[7]], sig=False)
        P.op("pe", lambda e, pc=pc, n=n: e.matmul(banks[7][0:8, 0:n], lhsT=tabh[:, 8:16], rhs=ohb[:, pc:pc + n], start=False, stop=True),
             reads=[btab, boh], writes=[bbank[7]])
        P.op("dve", lambda e, pc=pc, n=n: e.tensor_copy(out=fx[:, pc:pc + n], in_=banks[7][0:8, 0:n]), reads=[bbank[7]], writes=[bfx])
    bfext_d = Buf()
    P.op("sp", lambda e: e.dma_start(out=fext_d, in_=fx[:]), reads=[bfx], writes=[bfext_d], dma=s_f)
    cfar = SB("cfar", [128, 2, 8], F32); bcfar = Buf()
    s_cf = P.sem()
    for fs in range(2):
        cf_src = bass.AP(tensor=fext_d.tensor, offset=fs * 2 * FEXT, ap=[[0, 128], [NW, 8]])
        P.op("sp", lambda e, fs=fs, cf_src=cf_src: e.dma_start(out=cfar[:, fs, :], in_=cf_src, allow_slow_non_contiguous=True),
             reads=[bfext_d], writes=[bcfar], dma=s_cf)

    wg = SB("wg", [128, DC, 768], BF16); bwg = Buf(); s_wg = P.sem()
    hTb2 = [SB(f"hTc{i}", [128, DC, 512], BF16) for i in range(2)]
    bhTb2 = [Buf(), Buf()]; s_hl = [P.sem(), P.sem()]
    QT = [SB(f"QT{i}", [128, NQ], BF16) for i in range(4)]; bQT = [Buf() for _ in range(4)]
    KT = [SB(f"KT{i}", [128, S], BF16) for i in range(2)]; bKT = [Buf() for _ in range(2)]
    VV = SB("VV", [128, NTC, 256], BF16); bVV = Buf()
    sqb = [SB(f"sqb{i}", [128, 512], BF16) for i in range(2)]; bsqb = [Buf(), Buf()]
    rsb = [SB(f"rsb{i}", [128, 512], F32) for i in range(2)]; brsb = [Buf(), Buf()]
    qnb = [SB(f"qnb{i}", [128, 512], BF16) for i in range(2)]; bqnb = [Buf(), Buf()]
    t1b = [SB(f"t1b{i}", [128, 512], F32) for i in range(2)]; bt1b = [Buf(), Buf()]
    t2b = [SB(f"t2b{i}", [128, 512], F32) for i in range(2)]; bt2b = [Buf(), Buf()]
    cosb = [SB(f"cosb{i}", [128, 512], F32) for i in range(4)]; bcos = [Buf() for _ in range(4)]; s_cos = [P.sem() for _ in range(4)]
    sinb = [SB(f"sinb{i}", [128, 512], F32) for i in range(4)]; bsin = [Buf() for _ in range(4)]; s_sin = [P.sem() for _ in range(4)]
    PT = [SB(f"PT{i}", [128, 512], BF16) for i in range(8)]; bPT = [Buf() for _ in range(8)]
    ep = [SB(f"ep{i}", [128, 512], F32) for i in range(6)]; bep = [Buf() for _ in range(6)]
    epq = SB("epq", [128, 512], BF16); bepq = Buf()
    OTs = [SB(f"OTs{i}", [128, 512], BF16) for i in range(2)]; bOTs = [Buf(), Buf()]; s_ot = [P.sem(), P.sem()]
    HKf = SB("HKf", [128, 5, 128], F32); bHKf = Buf(); s_hk = P.sem()
    HKh = [SB(f"HKh{i}", [128, 5, 128], BF16) for i in range(2)]
    HKl = [SB(f"HKl{i}", [128, 5, 128], BF16) for i in range(2)]
    bHK = [Buf(), Buf()]
    hTd_v = hT_d.rearrange("c p s -> p c s")
    win_v = win_g.rearrange("(c p) n -> p c n", p=128)
    ctr = {"s": 0, "pt": 0, "ot": 0, "job": 0}
    deferred = []

    P.op("dve", lambda e: e.memset(zt[:], 0.0), writes=[bzt])
    s_z = P.sem(); s_zy = P.sem()
    fills = [("h", r0) for r0 in range(0, NROW, 128)] + [("y", r0) for r0 in range(0, NROW, 128)]
    glist = list(getattr(cfg, 'groups', range(6)))
    per_g = -(-len(fills) // max(1, len(glist)))
    for gi6, g in enumerate(glist):
        isA = g < 2
        nq = 4 if isA else 2
        nk = 1 if isA else 2
        vw = 128 if isA else 256
        koff = 512 if isA else 256
        voff = 640 if isA else 512
        P.op("pool", lambda e, g=g: e.dma_start(out=wg[:], in_=win_v[:, :, g * 768:(g + 1) * 768]), writes=[bwg], dma=s_wg)
        for kind, r0 in fills[gi6 * per_g:(gi6 + 1) * per_g]:
            if kind == "h":
                P.op("pool", lambda e, r0=r0: e.dma_start(out=hs_d[r0:r0 + 128, :], in_=zt[:]), reads=[bzt], dma=s_z)
            else:
                P.op("pool", lambda e, r0=r0: e.dma_start(out=ys_d[r0:r0 + 128, :], in_=zt[:]), reads=[bzt], dma=s_zy)
        if not isA:
            for hh in range(2):
                h = 2 * (g - 2) + hh
                src = bass.AP(tensor=fext_d.tensor, offset=h * NW + FEXT - 256 - 127, ap=[[1, 128], [128, 5], [1, 128]])
                P.op("sp", lambda e, src=src: e.dma_start(out=HKf[:], in_=src), reads=[bfext_d], writes=[bHKf], dma=s_hk)
                P.op("dve", lambda e: e.tensor_scalar_mul(out=HKf[:], in0=HKf[:], scalar1=8.0), reads=[bHKf], writes=[bHKf])
                P.op("dve", lambda e, hh=hh: e.tensor_copy(out=HKh[hh][:], in_=HKf[:]), reads=[bHKf], writes=[bHK[hh]])
                P.op("dve", lambda e, hh=hh: e.tensor_tensor(out=HKl[hh][:], in0=HKf[:], in1=HKh[hh][:], op=ALU.subtract),
                     reads=[bHKf, bHK[hh]], writes=[bHK[hh]])
        jobs = []
        for tb in range(NTB):
            for ki in range(nk):
                jobs.append((tb, "k", ki))
            if tb < NQB:
                for qi in range(nq):
                    jobs.append((tb, "q", qi))
        n = len(jobs)
        st = {}

        def stage1(i):
            tb, kind, idx = jobs[i]
            hs = tb % 2
            first = (i == 0) or (jobs[i - 1][0] != tb)
            if first:
                P.op("sp", lambda e, hs=hs, tb=tb: e.dma_start(out=hTb2[hs][:], in_=hTd_v[:, :, tb * 512:(tb + 1) * 512]),
                     writes=[bhTb2[hs]], dma=s_hl[hs])
                if isA:
                    h4 = tb % 4
                    P.op("act", lambda e, h4=h4, tb=tb: e.dma_start(out=cosb[h4][:], in_=ropec[:, tb * 512:(tb + 1) * 512]),
                         writes=[bcos[h4]], dma=s_cos[h4])
                    P.op("act", lambda e, h4=h4, tb=tb: e.dma_start(out=sinb[h4][:], in_=ropes[:, tb * 512:(tb + 1) * 512]),
                         writes=[bsin[h4]], dma=s_sin[h4])
            jn = ctr["job"]; ctr["job"] += 1
            pb = jn % 3
            c0 = (koff + idx * 128) if kind == "k" else idx * 128
            for j in range(DC):
                P.op("pe", lambda e, pb=pb, j=j, c0=c0, hs=hs: e.matmul(banks[pb][:], lhsT=wg[:, j, c0:c0 + 128], rhs=hTb2[hs][:, j, :],
                                                                      start=(j == 0), stop=(j == DC - 1)),
                     reads=[bwg, bhTb2[hs]], writes=[bbank[pb]], sig=(j == DC - 1))
            r2 = jn % 2
            P.op("act", lambda e, pb=pb, r2=r2: e.activation(out=sqb[r2][:], in_=banks[pb][:], func=AF.Square),
                 reads=[bbank[pb]], writes=[bsqb[r2]])
            st[i] = (jn, pb, r2)
            last = (i == n - 1) or (jobs[i + 1][0] != tb)
            if last:
                for tc in range(4):
                    for j in range(DC):
                        P.op("pe", lambda e, j=j, tc=tc, hs=hs: e.matmul(banks[7][:, 0:vw], lhsT=hTb2[hs][:, j, tc * 128:(tc + 1) * 128],
                                                                       rhs=wg[:, j, voff:voff + vw], start=(j == 0), stop=(j == DC - 1)),
                             reads=[bwg, bhTb2[hs]], writes=[bbank[7]], sig=(j == DC - 1))
                    P.op("act", lambda e, tb=tb, tc=tc: e.copy(out=VV[:, tb * 4 + tc, 0:vw], in_=banks[7][:, 0:vw]),
                         reads=[bbank[7]], writes=[bVV])

        def stage2(i):
            tb, kind, idx = jobs[i]
            jn, pb, r2 = st[i]
            sb_ = 3 + r2
            P.op("pe", lambda e, sb_=sb_, r2=r2: e.matmul(banks[sb_][:], lhsT=(ONES if isA else BLK64), rhs=sqb[r2][:], start=True, stop=True),
                 reads=[bCM, bsqb[r2]], writes=[bbank[sb_]])
            P.op("act", lambda e, sb_=sb_, r2=r2: e.activation(out=rsb[r2][:], in_=banks[sb_][:], func=AF.Sqrt, bias=epsc[:, 0:1],
                                                               scale=(1.0 / 128 if isA else 1.0 / 64)),
                 reads=[bbank[sb_], beps], writes=[brsb[r2]])
            P.op("dve", lambda e, r2=r2: e.reciprocal(out=rsb[r2][:], in_=rsb[r2][:]), reads=[brsb[r2]], writes=[brsb[r2]])
            gcol = (1 if kind == "k" else 0) + (0 if isA else 2)
            if isA:
                dst, dbuf = qnb[r2][:], bqnb[r2]
            elif kind == "k":
                dst, dbuf = KT[idx][:, tb * 512:(tb + 1) * 512], bKT[idx]
            else:
                dst, dbuf = QT[idx][:, tb * 512:(tb + 1) * 512], bQT[idx]
            P.op("dve", lambda e, pb=pb, r2=r2, gcol=gcol, dst=dst: e.scalar_tensor_tensor(
                out=dst, in0=banks[pb][:], scalar=gsb[:, gcol:gcol + 1], in1=rsb[r2][:], op0=ALU.mult, op1=ALU.mult),
                reads=[bbank[pb], bg_, brsb[r2]], writes=[dbuf])

        def stage3(i):
            if not isA:
                return
            tb, kind, idx = jobs[i]
            jn, pb, r2 = st[i]
            hs = tb % 4
            wb = 5 + r2
            P.op("pe", lambda e, wb=wb, r2=r2: e.matmul(banks[wb][:], lhsT=SWP, rhs=qnb[r2][:], start=True, stop=True),
                 reads=[bCM, bqnb[r2]], writes=[bbank[wb]])
            P.op("dve", lambda e, r2=r2, hs=hs: e.tensor_tensor(out=t1b[r2][:], in0=qnb[r2][:], in1=cosb[hs][:], op=ALU.mult),
                 reads=[bqnb[r2], bcos[hs]], writes=[bt1b[r2]])
            P.op("dve", lambda e, r2=r2, hs=hs, wb=wb: e.tensor_tensor(out=t2b[r2][:], in0=banks[wb][:], in1=sinb[hs][:], op=ALU.mult),
                 reads=[bbank[wb], bsin[hs]], writes=[bt2b[r2]])
            if kind == "k":
                dst, dbuf = KT[idx][:, tb * 512:(tb + 1) * 512], bKT[idx]
            else:
                dst, dbuf = QT[idx][:, tb * 512:(tb + 1) * 512], bQT[idx]
            P.op("dve", lambda e, r2=r2, dst=dst: e.tensor_tensor(out=dst, in0=t1b[r2][:], in1=t2b[r2][:], op=ALU.add),
                 reads=[bt1b[r2], bt2b[r2]], writes=[dbuf])

        for i in range(n + 2):
            if i < n:
                stage1(i)
            if 0 <= i - 1 < n:
                stage2(i - 1)
            if 0 <= i - 2 < n:
                stage3(i - 2)

        scale = (128.0 ** -0.5) if isA else 0.125
        nblk = 0
        for qi in range(nq):
            otile = (4 * g + qi) if isA else (8 + 2 * (g - 2) + qi)
            hglob = None if isA else 2 * (g - 2) + qi
            maps = [(0, 128)] if isA else [(0, 64), (64, 128)]
            ktile = 0 if isA else qi
            vc0 = 0 if isA else qi * 128
            for Qb in range(NQB):
                if isA:
                    accb = [3 + nblk % 2]; sumb = [5 + nblk % 2]
                else:
                    accb = [3, 4]; sumb = [5, 6]
                nblk += 1
                sinfo = {}

                def emitS(kc):
                    for mi, (p0, p1) in enumerate(maps):
                        sbk = ctr["s"] % 3; ctr["s"] += 1
                        ptk = ctr["pt"] % 8; ctr["pt"] += 1
                        jrel = kc - 4 * Qb
                        mixed = (not isA) and (-1 <= jrel <= 4) and not getattr(cfg, 'nobias', False)
                        near = [i for i in range(4) if abs(jrel - i) <= 1] if mixed else []
                        P.op("pe", lambda e, sbk=sbk, p0=p0, p1=p1, kc=kc: e.matmul(
                            banks[sbk][:], lhsT=KT[ktile][p0:p1, kc * 128:(kc + 1) * 128], rhs=QT[qi][p0:p1, Qb * 512:(Qb + 1) * 512],
                            start=True, stop=(len(near) == 0)), reads=[bKT[ktile], bQT[qi]], writes=[bbank[sbk]], sig=(len(near) == 0))
                        for ni, i in enumerate(near):
                            di = jrel - i + 2
                            lastn = ni == len(near) - 1
                            P.op("pe", lambda e, sbk=sbk, i=i, di=di: e.matmul(banks[sbk][:, i * 128:(i + 1) * 128], lhsT=HKh[qi][:, di, :], rhs=JM,
                                                                             start=False, stop=False),
                                 reads=[bHK[qi], bCM], writes=[bbank[sbk]], sig=False)
                            P.op("pe", lambda e, sbk=sbk, i=i, di=di, lastn=lastn: e.matmul(banks[sbk][:, i * 128:(i + 1) * 128], lhsT=HKl[qi][:, di, :], rhs=JM,
                                                                                         start=False, stop=lastn),
                                 reads=[bHK[qi], bCM], writes=[bbank[sbk]], sig=lastn)
                        if isA:
                            P.op("act", lambda e, sbk=sbk, ptk=ptk: e.activation(out=PT[ptk][:], in_=banks[sbk][:], func=AF.Exp, scale=scale),
                                 reads=[bbank[sbk]], writes=[bPT[ptk]])
                        elif getattr(cfg, 'nobias', False):
                            P.op("act", lambda e, sbk=sbk, ptk=ptk: e.activation(out=PT[ptk][:], in_=banks[sbk][:], func=AF.Exp, scale=scale),
                                 reads=[bbank[sbk]], writes=[bPT[ptk]])
                        elif not mixed:
                            fs = 0 if jrel < 0 else 1
                            P.op("act", lambda e, sbk=sbk, ptk=ptk, fs=fs: e.activation(out=PT[ptk][:], in_=banks[sbk][:], func=AF.Exp, scale=scale,
                                                                                    bias=cfar[:, fs, hglob:hglob + 1]),
                                 reads=[bbank[sbk], bcfar], writes=[bPT[ptk]])
                        else:
                            for i in range(4):
                                dd = jrel - i
                                bias_ap = epsc[:, 1:2] if abs(dd) <= 1 else cfar[:, (0 if dd < 0 else 1), hglob:hglob + 1]
                                P.op("act", lambda e, sbk=sbk, ptk=ptk, i=i, bias_ap=bias_ap: e.activation(
                                    out=PT[ptk][:, i * 128:(i + 1) * 128], in_=banks[sbk][:, i * 128:(i + 1) * 128], func=AF.Exp, scale=scale, bias=bias_ap),
                                    reads=[bbank[sbk], bcfar, beps], writes=[bPT[ptk]])
                        sinfo[(kc, mi)] = ptk

                def emitPV(kc):
                    for mi in range(len(maps)):
                        ptk = sinfo[(kc, mi)]
                        P.op("pe", lambda e, mi=mi, ptk=ptk, kc=kc: e.matmul(banks[accb[mi]][:], lhsT=VV[:, kc, vc0:vc0 + 128], rhs=PT[ptk][:],
                                                                           start=(kc == 0), stop=(kc == NTC - 1)),
                             reads=[bVV, bPT[ptk]], writes=[bbank[accb[mi]]], sig=False)
                        P.op("pe", lambda e, mi=mi, ptk=ptk, kc=kc: e.matmul(banks[sumb[mi]][:], lhsT=ONES, rhs=PT[ptk][:],
                                                                           start=(kc == 0), stop=(kc == NTC - 1)),
                             reads=[bCM, bPT[ptk]], writes=[bbank[sumb[mi]]], sig=True)

                for step in range(NTC + 2):
                    if step < NTC:
                        emitS(step)
                    if step == 3 and deferred:
                        for fdef in deferred:
                            fdef()
                        deferred.clear()
                    if step >= 2:
                        emitPV(step - 2)
                osl = ctr["ot"] % 2; ctr["ot"] += 1
                if isA:
                    P.op("dve", lambda e, sb_=sumb[0]: e.reciprocal(out=ep[0][:], in_=banks[sb_][:]), reads=[bbank[sumb[0]]], writes=[bep[0]])
                    P.op("dve", lambda e, ab=accb[0], osl=osl: e.tensor_tensor(out=OTs[osl][:], in0=banks[ab][:], in1=ep[0][:], op=ALU.mult),
                         reads=[bbank[accb[0]], bep[0]], writes=[bOTs[osl]])
                    P.op("sp", lambda e, osl=osl, otile=otile, Qb=Qb: e.dma_start(out=OT_d[otile, :, Qb * 512:(Qb + 1) * 512], in_=OTs[osl][:]),
                         reads=[bOTs[osl]], dma=s_ot[osl])
                else:
                    P.op("dve", lambda e: e.reciprocal(out=ep[0][:], in_=banks[5][:]), reads=[bbank[5]], writes=[bep[0]])
                    P.op("dve", lambda e: e.tensor_tensor(out=ep[1][:], in0=banks[3][:], in1=ep[0][:], op=ALU.mult),
                         reads=[bbank[3], bep[0]], writes=[bep[1]])
                    P.op("dve", lambda e: e.reciprocal(out=ep[2][:], in_=banks[6][:]), reads=[bbank[6]], writes=[bep[2]])
                    P.op("dve", lambda e: e.tensor_tensor(out=ep[3][:], in0=banks[4][:], in1=ep[2][:], op=ALU.mult),
                         reads=[bbank[4], bep[2]], writes=[bep[3]])
                    P.op("dve", lambda e: e.scalar_tensor_tensor(out=ep[4][:], in0=ep[3][:], scalar=NEGLAM, in1=ep[1][:], op0=ALU.mult, op1=ALU.add),
                         reads=[bep[3], bep[1], blam], writes=[bep[4]])
                    P.op("act", lambda e: e.activation(out=epq[:], in_=ep[4][:], func=AF.Square), reads=[bep[4]], writes=[bepq])

                    def tail(osl=osl, otile=otile, Qb=Qb):
                        P.op("pe", lambda e: e.matmul(banks[7][:], lhsT=ONES, rhs=epq[:], start=True, stop=True), reads=[bCM, bepq], writes=[bbank[7]])
                        P.op("act", lambda e: e.activation(out=ep[5][:], in_=banks[7][:], func=AF.Sqrt, bias=epsc[:, 0:1], scale=1.0 / 128),
                             reads=[bbank[7], beps], writes=[bep[5]])
                        P.op("dve", lambda e: e.reciprocal(out=ep[5][:], in_=ep[5][:]), reads=[bep[5]], writes=[bep[5]])
                        P.op("dve", lambda e, osl=osl: e.scalar_tensor_tensor(out=OTs[osl][:], in0=ep[4][:], scalar=SUBG, in1=ep[5][:], op0=ALU.mult, op1=ALU.mult),
                             reads=[bep[4], bep[5], bg_], writes=[bOTs[osl]])
                        P.op("sp", lambda e, osl=osl, otile=otile, Qb=Qb: e.dma_start(out=OT_d[otile, :, Qb * 512:(Qb + 1) * 512], in_=OTs[osl][:]),
                             reads=[bOTs[osl]], dma=s_ot[osl])
                    deferred.append(tail)
        for fdef in deferred:
            fdef()
        deferred.clear()
    if "dbgQ" in debug:
        dq = dt("dbgQ", [128, NQ], BF16, kind="ExternalOutput").ap()
        dk = dt("dbgK", [128, S], BF16, kind="ExternalOutput").ap()
        dv = dt("dbgV", [128, NTC * 256], BF16, kind="ExternalOutput").ap()
        sdbg = P.sem()
        de = dt("dbgE", [6, 128, 512], F32, kind="ExternalOutput").ap()
        for i6 in range(6):
            P.op("sp", lambda e, i6=i6: e.dma_start(out=de[i6], in_=ep[i6][:]), reads=[bep[i6]], dma=sdbg)
        dp = dt("dbgP", [4, 128, 512], BF16, kind="ExternalOutput").ap()
        for i4 in range(4):
            P.op("sp", lambda e, i4=i4: e.dma_start(out=dp[i4], in_=PT[i4][:]), reads=[bPT[i4]], dma=sdbg)
        P.op("sp", lambda e: e.dma_start(out=dq, in_=QT[1][:]), reads=[bQT[1]], dma=sdbg)
        P.op("sp", lambda e: e.dma_start(out=dk, in_=KT[1][:]), reads=[bKT[1]], dma=sdbg)
        P.op("sp", lambda e: e.dma_start(out=dv, in_=VV[:].rearrange("p a b -> p (a b)")), reads=[bVV], dma=sdbg)
    end_phase()
    if stop_after == "B":
        return nc, P
    IOA = bass.IndirectOffsetOnAxis
    dest_i = SBG("dest_i", [128, NQC, 4], I32); bdest = Buf()
    dsc_i = SBG("dsc_i", [128, NQC, 4], I32)
    gk = SBG("gk", [128, NQC, 4], F32); bgk = Buf()
    wo = SB("wo", [128, 16, D], BF16); bwo = Buf(); s_wo = P.sem()
    P.op("pool", lambda e: e.dma_start(out=wo[:], in_=w_out.rearrange("(k p) n -> p k n", p=128)), writes=[bwo], dma=s_wo)
    mrep = SB("mrep", [128, 3, D], F32); bmr = Buf(); s_mr = P.sem()
    for i3, seg in enumerate((2, 3, 4)):
        P.op("sp", lambda e, i3=i3, seg=seg: e.dma_start(out=mrep[:, i3, :], in_=mod_d[:, seg * D:(seg + 1) * D]), writes=[bmr], dma=s_mr)
    wr = SB("wr", [128, DC, E], BF16); bwr = Buf(); s_wr = P.sem()
    P.op("pool", lambda e: e.dma_start(out=wr[:], in_=w_router.rearrange("(c p) n -> p c n", p=128)), writes=[bwr], dma=s_wr)
    brr = SB("brr", [128, E], F32); iot = SB("iot", [128, E], F32); ecap = SB("ecap", [128, E], F32); bconst = Buf(); s_cn = P.sem()
    P.op("sp", lambda e: e.dma_start(out=brr[:], in_=brout_rep), writes=[bconst], dma=s_cn)
    P.op("sp", lambda e: e.dma_start(out=iot[:], in_=iota_e), writes=[bconst], dma=s_cn)
    prw = SB("prw", [128, E], F32)
    P.op("sp", lambda e: e.dma_start(out=prw[:], in_=padrow), writes=[bconst], dma=s_cn)
    P.op("dve", lambda e: e.tensor_scalar_mul(out=ecap[:], in0=iot[:], scalar1=float(CAP)), reads=[bconst], writes=[bconst])
    base = SB("base", [128, E], F32); bbase = Buf()
    P.op("dve", lambda e: e.memset(base[:], 0.0), writes=[bbase])
    OTc = SB("OTc", [128, 16, 128], BF16); bOTc = Buf(); s_oc = P.sem()
    xq = SB("xq", [128, D], F32); bxq = Buf(); s_xq = P.sem()
    x1 = SB("x1", [128, D], F32); bx1 = Buf(); s_x1 = P.sem()
    tmpc = SB("tmpc", [128, D], F32); btmpc = Buf()
    junkc = SB("junkc", [128, D], BF16); bjc = Buf()
    ssc = SB("ssc", [128, 2], F32); bssc = Buf()
    h2 = SB("h2", [128, D], BF16); bh2 = Buf(); s_sc = P.sem()
    h2T = SB("h2T", [128, DC, 128], BF16); bh2T = Buf()
    lg = SB("lg", [128, E], F32); blg = Buf()
    mx8 = SB("mx8", [128, 8], F32); negm = SB("negm", [128, 1], F32)
    msk = SB("msk", [128, E], F32); mskb = SB("mskb", [128, E], BF16)
    ex = SB("ex", [128, E], F32); gat = SB("gat", [128, E], F32); rsum = SB("rsum", [128, 2], F32)
    dst = SB("dst", [128, E], F32); oh = SB("oh", [128, E], F32); prod = SB("prod", [128, E], F32)
    dk = SB("dk", [128, 4], F32)
    dks = SB("dks", [128, 4], F32)
    dsts = SB("dsts", [128, E], F32)
    ovf = SB("ovf", [128, E], F32)
    brt = Buf()
    OTv = OT_d.rearrange("k p s -> p k s")
    for t in range(NQC):
        P.op("sp", lambda e, t=t: e.dma_start(out=OTc[:], in_=OTv[:, :, t * 128:(t + 1) * 128]), writes=[bOTc], dma=s_oc)
        P.op("sp", lambda e, t=t: e.dma_start(out=xq[:], in_=xkv[t * 128:(t + 1) * 128, :]), writes=[bxq], dma=s_xq)
        for cb in range(D // 512):
            bk = cb % 2
            for k in range(16):
                P.op("pe", lambda e, bk=bk, k=k, cb=cb: e.matmul(banks[bk][:], lhsT=OTc[:, k, :], rhs=wo[:, k, cb * 512:(cb + 1) * 512],
                                                               start=(k == 0), stop=(k == 15)),
                     reads=[bOTc, bwo], writes=[bbank[bk]], sig=(k == 15))
            P.op("dve", lambda e, bk=bk, cb=cb: e.tensor_tensor(out=tmpc[:, cb * 512:(cb + 1) * 512], in0=banks[bk][:],
                                                                in1=mrep[:, 0, cb * 512:(cb + 1) * 512], op=ALU.mult),
                 reads=[bbank[bk], bmr], writes=[btmpc])
            P.op("dve", lambda e, cb=cb: e.tensor_tensor(out=x1[:, cb * 512:(cb + 1) * 512], in0=tmpc[:, cb * 512:(cb + 1) * 512],
                                                         in1=xq[:, cb * 512:(cb + 1) * 512], op=ALU.add),
                 reads=[btmpc, bxq], writes=[bx1])
        P.op("sp", lambda e, t=t: e.dma_start(out=out[t * 128:(t + 1) * 128, :], in_=x1[:]), reads=[bx1], dma=s_x1)
        P.op("act", lambda e: e.activation(out=junkc[:], in_=x1[:], func=AF.Square, accum_out=ssc[:, 0:1]), reads=[bx1], writes=[bjc, bssc])
        P.op("act", lambda e: e.activation(out=ssc[:, 1:2], in_=ssc[:, 0:1], func=AF.Sqrt, bias=epsc[:, 0:1], scale=1.0 / D),
             reads=[bssc, beps], writes=[bssc])
        P.op("dve", lambda e: e.reciprocal(out=ssc[:, 1:2], in_=ssc[:, 1:2]), reads=[bssc], writes=[bssc])
        P.op("dve", lambda e: e.scalar_tensor_tensor(out=tmpc[:], in0=x1[:], scalar=ssc[:, 1:2], in1=mrep[:, 2, :], op0=ALU.mult, op1=ALU.mult),
             reads=[bx1, bssc, bmr], writes=[btmpc])
        P.op("dve", lambda e: e.tensor_tensor(out=h2[:], in0=tmpc[:], in1=mrep[:, 1, :], op=ALU.add), reads=[btmpc, bmr], writes=[bh2])
        for j in range(DC):
            bk = 2 + j % 2
            pv = banks[bk][:].bitcast(BF16)
            P.op("pe", lambda e, j=j, pv=pv: e.transpose(out=pv[:, 0:128], in_=h2[:, j * 128:(j + 1) * 128], identity=IDENT),
                 reads=[bh2, bCM], writes=[bbank[bk]])
            P.op("act", lambda e, j=j, pv=pv: e.copy(out=h2T[:, j, :], in_=pv[:, 0:128]), reads=[bbank[bk]], writes=[bh2T])
        for j in range(DC):
            P.op("pe", lambda e, j=j: e.matmul(banks[4][:, 0:E], lhsT=h2T[:, j, :], rhs=wr[:, j, :], start=(j == 0), stop=(j == DC - 1)),
                 reads=[bh2T, bwr], writes=[bbank[4]], sig=(j == DC - 1))
        R_ = dict(reads=[brt], writes=[brt])
        P.op("dve", lambda e: e.tensor_tensor(out=lg[:], in0=banks[4][:, 0:E], in1=brr[:], op=ALU.add), reads=[bbank[4], bconst, brt], writes=[brt])
        P.op("dve", lambda e: e.max(out=mx8[:], in_=lg[:]), **R_)
        P.op("dve", lambda e: e.tensor_scalar(out=msk[:], in0=lg[:], scalar1=mx8[:, 3:4], scalar2=None, op0=ALU.is_ge), **R_)
        P.op("dve", lambda e: e.tensor_copy(out=mskb[:], in_=msk[:]), **R_)
        P.op("dve", lambda e: e.tensor_scalar_mul(out=negm[:], in0=mx8[:, 0:1], scalar1=-1.0), **R_)
        P.op("act", lambda e: e.activation(out=ex[:], in_=lg[:], func=AF.Exp, bias=negm[:, 0:1], scale=1.0), **R_)
        P.op("dve", lambda e: e.tensor_tensor(out=ex[:], in0=ex[:], in1=msk[:], op=ALU.mult), **R_)
        P.op("dve", lambda e: e.reduce_sum(out=rsum[:, 0:1], in_=ex[:], axis=AX.X), **R_)
        P.op("dve", lambda e: e.reciprocal(out=rsum[:, 1:2], in_=rsum[:, 0:1]), **R_)
        P.op("dve", lambda e: e.tensor_scalar(out=gat[:], in0=ex[:], scalar1=rsum[:, 1:2], scalar2=None, op0=ALU.mult), **R_)
        P.op("pe", lambda e: e.matmul(banks[5][:, 0:E], lhsT=TRI, rhs=mskb[:], start=True, stop=True), reads=[bCM, brt], writes=[bbank[5]])
        P.op("pe", lambda e: e.matmul(banks[5][:, E:2 * E], lhsT=ONES, rhs=mskb[:], start=True, stop=True), reads=[bCM, brt], writes=[bbank[5]])
        P.op("dve", lambda e: e.tensor_tensor(out=dst[:], in0=banks[5][:, 0:E], in1=base[:], op=ALU.add), reads=[bbank[5], bbase, brt], writes=[brt])
        P.op("dve", lambda e: e.tensor_scalar(out=ovf[:], in0=dst[:], scalar1=float(CAP), scalar2=None, op0=ALU.is_ge), **R_)
        P.op("dve", lambda e: e.tensor_tensor(out=dst[:], in0=dst[:], in1=ecap[:], op=ALU.add), reads=[bconst, brt], writes=[brt])
        P.op("dve", lambda e: e.tensor_tensor(out=prod[:], in0=prw[:], in1=dst[:], op=ALU.subtract), reads=[bconst, brt], writes=[brt])
        P.op("dve", lambda e: e.tensor_tensor(out=prod[:], in0=prod[:], in1=ovf[:], op=ALU.mult), **R_)
        P.op("dve", lambda e: e.tensor_tensor(out=dst[:], in0=dst[:], in1=prod[:], op=ALU.add), **R_)
        P.op("dve", lambda e: e.scalar_tensor_tensor(out=dsts[:], in0=ovf[:], scalar=1.0e6, in1=dst[:], op0=ALU.mult, op1=ALU.add), **R_)
        P.op("dve", lambda e: e.tensor_tensor(out=base[:], in0=banks[5][:, E:2 * E], in1=base[:], op=ALU.add), reads=[bbank[5], bbase], writes=[bbase])
        for k in range(4):
            P.op("dve", lambda e, k=k: e.tensor_scalar(out=oh[:], in0=lg[:], scalar1=mx8[:, k:k + 1], scalar2=None, op0=ALU.is_equal), **R_)
            P.op("dve", lambda e: e.tensor_tensor(out=prod[:], in0=oh[:], in1=dst[:], op=ALU.mult), **R_)
            P.op("dve", lambda e, k=k: e.reduce_sum(out=dk[:, k:k + 1], in_=prod[:], axis=AX.X), **R_)
            P.op("dve", lambda e: e.tensor_tensor(out=prod[:], in0=oh[:], in1=dsts[:], op=ALU.mult), **R_)
            P.op("dve", lambda e, k=k: e.reduce_sum(out=dks[:, k:k + 1], in_=prod[:], axis=AX.X), **R_)
            P.op("dve", lambda e: e.tensor_tensor(out=prod[:], in0=oh[:], in1=gat[:], op=ALU.mult), **R_)
            P.op("dve", lambda e, k=k, t=t: e.reduce_sum(out=gk[:, t, k:k + 1], in_=prod[:], axis=AX.X), reads=[brt], writes=[brt, bgk])
        P.op("dve", lambda e, t=t: e.tensor_copy(out=dest_i[:, t, :], in_=dk[:]), reads=[brt], writes=[bdest])
        P.op("dve", lambda e, t=t: e.tensor_copy(out=dsc_i[:, t, :], in_=dks[:]), reads=[brt], writes=[bdest])
        for k in range(4):
            P.op("pool", lambda e, t=t, k=k: e.indirect_dma_start(out=hs_d[:, :], out_offset=IOA(ap=dsc_i[:, t, k:k + 1], axis=0),
                                                                  in_=h2[:, :], in_offset=None, bounds_check=NROW - 1, oob_is_err=False),
                 reads=[bh2, bdest], dma=s_sc)
    cnt_i = SB("cnt_i", [128, E], I32); bcnt = Buf(); s_cnt = P.sem()
    P.op("dve", lambda e: e.tensor_copy(out=cnt_i[:], in_=base[:]), reads=[bbase], writes=[bcnt])
    P.op("sp", lambda e: e.dma_start(out=cnt_d, in_=cnt_i[0:1, :]), reads=[bcnt], dma=s_cnt)
    end_phase()
    if stop_after == "C":
        return nc, P

    bgu = SB("bgu", [128, E * 2 * FC], F32); bbgu = Buf(); s_bg = P.sem()
    P.op("sp", lambda e: e.dma_start(out=bgu[:], in_=bgu_fm), writes=[bbgu], dma=s_bg)
    rows = [SB(f"rows{i}", [128, D], BF16) for i in range(2)]; brow = [Buf(), Buf()]; s_row = [P.sem(), P.sem()]
    XT = SB("XT", [128, DC, CAP], BF16); bXT = Buf()
    wgb = [SB(f"wgb{i}", [128, DC, 512], BF16) for i in range(2)]; bwgb = [Buf(), Buf()]; s_wgb = [P.sem(), P.sem()]
    wub = [SB(f"wub{i}", [128, DC, 512], BF16) for i in range(2)]; bwub = [Buf(), Buf()]; s_wub = [P.sem(), P.sem()]
    wdb = [SB(f"wdb{i}", [128, FC, 512], BF16) for i in range(2)]; bwdb = [Buf(), Buf()]; s_wdb = [P.sem(), P.sem()]
    actT = SB("actT", [128, FC, CAP], BF16); bact = Buf()
    gt = SB("gt", [128, 256], F32); sg = SB("sg", [128, 256], F32); u1 = SB("u1", [128, 256], F32); glu = SB("glu", [128, 256], F32)
    bgt = Buf(); bsg = Buf(); bu1 = Buf(); bglu = Buf()
    bdn = SB("bdn", [128, D], F32); bbdn = Buf(); s_bdn = P.sem()
    chunks = [(s0, min(128, CAP - s0)) for s0 in range(0, CAP, 128)]
    ybuf = [SB(f"ybuf{i}", [128, 512], F32) for i in range(4)]; bybuf = [Buf() for _ in range(4)]; s_y = [P.sem() for _ in range(4)]
    ri = 0; wi = 0; di = 0; yi = 0; gi_ = 0
    NFB = DFF // 512 if DFF >= 512 else 1
    FBW = min(512, DFF)
    GW_ = 256
    NG = CAP // GW_
    assert CAP % GW_ == 0
    dyn = getattr(cfg, "dynamic", True)

    def cond_begin(thr):
        if dyn:
            P.begin_cond(thr)

    def cond_end():
        if dyn:
            P.end_cond()

    for ex_ in range(E):
        if dyn:
            P.load_count(cnt_d[0:1, ex_:ex_ + 1])
        for g in range(NG):
            cond_begin(GW_ * g)
            for ci in range(2 * g, 2 * g + 2):
                s0, n = chunks[ci]
                rs = ri % 2; ri += 1
                P.op("sp", lambda e, rs=rs, s0=s0, n=n, ex_=ex_: e.dma_start(out=rows[rs][0:n, :], in_=hs_d[ex_ * CAP + s0:ex_ * CAP + s0 + n, :]),
                     writes=[brow[rs]], dma=s_row[rs])
                for j in range(DC):
                    bk = 6 + j % 2
                    pv = banks[bk][:].bitcast(BF16)
                    P.op("pe", lambda e, rs=rs, j=j, n=n, pv=pv: e.transpose(out=pv[:, 0:n], in_=rows[rs][0:n, j * 128:(j + 1) * 128], identity=IDENT[0:n, 0:n]),
                         reads=[brow[rs], bCM], writes=[bbank[bk]])
                    P.op("act", lambda e, j=j, s0=s0, n=n, pv=pv: e.copy(out=XT[:, j, s0:s0 + n], in_=pv[:, 0:n]), reads=[bbank[bk]], writes=[bXT])
            cond_end()
        P.op("sp", lambda e, ex_=ex_: e.dma_start(out=bdn[:], in_=bass.AP(tensor=b_down.tensor, offset=ex_ * D, ap=[[0, 128], [1, D]])),
             writes=[bbdn], dma=s_bdn)
        for fb in range(NFB):
            ws = wi % 2; wi += 1
            P.op("pool", lambda e, ws=ws, fb=fb, ex_=ex_: e.dma_start(out=wgb[ws][:, :, 0:FBW], in_=w_gu[ex_].rearrange("(c p) n -> p c n", p=128)[:, :, fb * FBW:(fb + 1) * FBW]),
                 writes=[bwgb[ws]], dma=s_wgb[ws])
            P.op("pool", lambda e, ws=ws, fb=fb, ex_=ex_: e.dma_start(out=wub[ws][:, :, 0:FBW], in_=w_gu[ex_].rearrange("(c p) n -> p c n", p=128)[:, :, DFF + fb * FBW:DFF + (fb + 1) * FBW]),
                 writes=[bwub[ws]], dma=s_wub[ws])
            for g in range(NG):
                h0 = g * GW_
                cond_begin(GW_ * g)
                for f4 in range(FBW // 128):
                    fc = fb * (FBW // 128) + f4
                    cg = ex_ * 2 * FC + fc
                    cu = ex_ * 2 * FC + FC + fc
                    gb = (gi_ % 2) * 2; gi_ += 1
                    for j in range(DC):
                        P.op("pe", lambda e, ws=ws, j=j, f4=f4, gb=gb, h0=h0: e.matmul(banks[gb][:, 0:GW_], lhsT=wgb[ws][:, j, f4 * 128:(f4 + 1) * 128],
                                                                                      rhs=XT[:, j, h0:h0 + GW_], start=(j == 0), stop=(j == DC - 1)),
                             reads=[bwgb[ws], bXT], writes=[bbank[gb]], sig=(j == DC - 1))
                    for j in range(DC):
                        P.op("pe", lambda e, ws=ws, j=j, f4=f4, gb=gb, h0=h0: e.matmul(banks[gb + 1][:, 0:GW_], lhsT=wub[ws][:, j, f4 * 128:(f4 + 1) * 128],
                                                                                      rhs=XT[:, j, h0:h0 + GW_], start=(j == 0), stop=(j == DC - 1)),
                             reads=[bwub[ws], bXT], writes=[bbank[gb + 1]], sig=(j == DC - 1))
                    P.op("dve", lambda e, cg=cg, gb=gb: e.tensor_scalar(out=gt[:, 0:GW_], in0=banks[gb][:, 0:GW_], scalar1=bgu[:, cg:cg + 1], scalar2=LIM, op0=ALU.add, op1=ALU.min),
                         reads=[bbank[gb], bbgu], writes=[bgt])
                    P.op("act", lambda e: e.activation(out=sg[:, 0:GW_], in_=gt[:, 0:GW_], func=AF.Sigmoid, scale=ALPHA), reads=[bgt], writes=[bsg])
                    P.op("dve", lambda e, cu=cu, gb=gb: e.tensor_scalar(out=u1[:, 0:GW_], in0=banks[gb + 1][:, 0:GW_], scalar1=bgu[:, cu:cu + 1], scalar2=LIM, op0=ALU.add, op1=ALU.min),
                         reads=[bbank[gb + 1], bbgu], writes=[bu1])
                    P.op("dve", lambda e: e.tensor_scalar(out=u1[:, 0:GW_], in0=u1[:, 0:GW_], scalar1=-LIM, scalar2=1.0, op0=ALU.max, op1=ALU.add), reads=[bu1], writes=[bu1])
                    P.op("dve", lambda e: e.tensor_tensor(out=glu[:, 0:GW_], in0=gt[:, 0:GW_], in1=sg[:, 0:GW_], op=ALU.mult), reads=[bgt, bsg], writes=[bglu])
                    P.op("dve", lambda e, fc=fc, h0=h0: e.tensor_tensor(out=actT[:, fc, h0:h0 + GW_], in0=u1[:, 0:GW_], in1=glu[:, 0:GW_], op=ALU.mult), reads=[bu1, bglu], writes=[bact])
                cond_end()
        for cb in range(D // 512):
            ds_ = di % 2; di += 1
            P.op("pool", lambda e, ds_=ds_, cb=cb, ex_=ex_: e.dma_start(out=wdb[ds_][:], in_=w_down[ex_].rearrange("(c p) n -> p c n", p=128)[:, :, cb * 512:(cb + 1) * 512]),
                 writes=[bwdb[ds_]], dma=s_wdb[ds_])
            for g in range(NG):
                cond_begin(GW_ * g)
                for ci in range(2 * g, 2 * g + 2):
                    s0, n = chunks[ci]
                    bk = 4 + (ci % 2)
                    ys = yi % 4; yi += 1
                    for fc in range(FC):
                        P.op("pe", lambda e, bk=bk, fc=fc, s0=s0, n=n, ds_=ds_: e.matmul(banks[bk][0:n, :], lhsT=actT[:, fc, s0:s0 + n], rhs=wdb[ds_][:, fc, :],
                                                                                      start=(fc == 0), stop=(fc == FC - 1)),
                             reads=[bact, bwdb[ds_]], writes=[bbank[bk]], sig=(fc == FC - 1))
                    P.op("dve", lambda e, bk=bk, ys=ys, n=n, cb=cb: e.tensor_tensor(out=ybuf[ys][0:n, :], in0=banks[bk][0:n, :],
                                                                                in1=bdn[0:n, cb * 512:(cb + 1) * 512], op=ALU.add),
                         reads=[bbank[bk], bbdn], writes=[bybuf[ys]])
                    P.op("sp", lambda e, ys=ys, s0=s0, n=n, ex_=ex_, cb=cb: e.dma_start(out=ys_d[ex_ * CAP + s0:ex_ * CAP + s0 + n, cb * 512:(cb + 1) * 512], in_=ybuf[ys][0:n, :]),
                         reads=[bybuf[ys]], dma=s_y[ys])
                cond_end()
    end_phase()
    if stop_after == "D":
        return nc, P

    gfr = SB("gfr", [128, D], F32); bgfr = Buf(); s_gf = P.sem()
    P.op("sp", lambda e: e.dma_start(out=gfr[:], in_=mod_d[:, 5 * D:6 * D]), writes=[bgfr], dma=s_gf)
    Yk = [SB(f"Yk{i}", [128, D], F32) for i in range(4)]; bYk = [Buf() for _ in range(4)]; s_yk = [P.sem() for _ in range(4)]
    x1e = SB("x1e", [128, D], F32); bx1e = Buf(); s_x1e = P.sem()
    acc = SB("acc", [128, D], F32); bacc = Buf()
    fin = SB("fin", [128, D], F32); bfin = Buf(); s_fin = P.sem()
    for t in range(NQC):
        P.op("sp", lambda e, t=t: e.dma_start(out=x1e[:], in_=out[t * 128:(t + 1) * 128, :]), writes=[bx1e], dma=s_x1e)
        for k in range(4):
            P.op("pool", lambda e, t=t, k=k: e.indirect_dma_start(out=Yk[k][:, :], out_offset=None, in_=ys_d[:, :],
                                                                  in_offset=IOA(ap=dest_i[:, t, k:k + 1], axis=0)),
                 reads=[bdest], writes=[bYk[k]], dma=s_yk[k])
        P.op("dve", lambda e, t=t: e.tensor_scalar(out=acc[:], in0=Yk[0][:], scalar1=gk[:, t, 0:1], scalar2=None, op0=ALU.mult),
             reads=[bYk[0], bgk], writes=[bacc])
        for k in range(1, 4):
            P.op("dve", lambda e, t=t, k=k: e.scalar_tensor_tensor(out=acc[:], in0=Yk[k][:], scalar=gk[:, t, k:k + 1], in1=acc[:], op0=ALU.mult, op1=ALU.add),
                 reads=[bYk[k], bgk, bacc], writes=[bacc])
        P.op("dve", lambda e: e.tensor_tensor(out=fin[:], in0=acc[:], in1=gfr[:], op=ALU.mult), reads=[bacc, bgfr], writes=[bfin])
        P.op("dve", lambda e: e.tensor_tensor(out=fin[:], in0=fin[:], in1=x1e[:], op=ALU.add), reads=[bfin, bx1e], writes=[bfin])
        P.op("sp", lambda e, t=t: e.dma_start(out=out[t * 128:(t + 1) * 128, :], in_=fin[:]), reads=[bfin, bx1e], dma=s_fin)
    end_phase()
    return nc, P


def _rel_bucket_np(rp):
    nb = 16
    max_exact = 8
    ret = np.where(rp > 0, nb, 0)
    n = np.abs(rp)
    nf = np.maximum(n, 1).astype(np.float32)
    large = max_exact + (np.log(nf / np.float32(max_exact)) / np.float32(math.log(128 / max_exact))
                         * np.float32(nb - max_exact)).astype(np.int32)
    large = np.minimum(large, nb - 1)
    return ret + np.where(n < max_exact, n, large)


def _static_tables(cfg, hf):
    S = cfg.S
    order = np.arange(S) if hf == 0 else np.arange(S)[::-1].copy()
    row = (order // 64).astype(np.float32)
    col = (order % 64).astype(np.float32)
    inv = (np.float32(10000.0) ** (-np.arange(0, 64, 2, dtype=np.float32) / np.float32(64))).astype(np.float32)
    ang = np.concatenate([row[:, None] * inv, col[:, None] * inv], axis=-1).astype(np.float32)
    c = np.cos(ang).astype(np.float32).T
    s = np.sin(ang).astype(np.float32).T
    ropec = np.ascontiguousarray(np.concatenate([c, c], 0))
    ropes = np.ascontiguousarray(np.concatenate([-s, s], 0))
    r = np.arange(-FEXT, FEXT + 2)
    sgn = 1 if hf == 0 else -1
    bk = _rel_bucket_np(sgn * r)
    ohr = (bk[None, :] == np.arange(32)[:, None]).astype(np.float32)
    cm = np.zeros((6, 128, 128), np.float32)
    cm[0] = np.eye(128)
    cm[1] = 1.0
    cm[2] = np.eye(128)[::-1]
    cm[3] = np.roll(np.eye(128), 64, axis=0)
    cm[4] = np.triu(np.ones((128, 128)), 1)
    cm[5, :64, :64] = 1.0
    cm[5, 64:, 64:] = 1.0
    cmat = np.ascontiguousarray(cm.transpose(1, 0, 2).reshape(128, 6 * 128))
    return order, ropec, ropes, ohr, cmat


def _win_cols():
    perm = np.concatenate([np.arange(0, 128, 2), np.arange(1, 128, 2)])
    cols = []
    for a in range(2):
        for h in range(4 * a, 4 * a + 4):
            cols.append(h * 128 + perm)
        cols.append(1024 + a * 128 + perm)
        cols.append(1280 + a * 128 + np.arange(128))
    for bg in range(4):
        for base in (1536, 2560, 3584):
            for h in (2 * bg, 2 * bg + 1):
                cols.append(base + h * 128 + np.arange(128))
    return np.concatenate(cols), perm


def prep_inputs(cfg, inp):
    D, S, E, DFF, DC, FC, NQ = cfg.D, cfg.S, cfg.E, cfg.DFF, cfg.DC, cfg.FC, cfg.NQ
    f = lambda a: np.ascontiguousarray(np.asarray(a, dtype=np.float32))
    rep = lambda v: np.ascontiguousarray(np.broadcast_to(np.asarray(v, np.float32).reshape(1, -1), (128, np.asarray(v).size)))
    cols, perm = _win_cols()
    shared = {
        "w_ada": f(inp["w_ada"][0]),
        "bada_rep": rep(inp["b_ada"][0]),
        "gn_rep": rep(np.concatenate([inp["norm_attn"][0], inp["norm_ffn"][0]])),
        "win_g": f(np.asarray(inp["w_in"][0])[:, cols]),
        "w_out": f(inp["w_out"][0]),
        "gains": f(np.stack([np.asarray(inp["a_q_norm"][0])[perm], np.asarray(inp["a_k_norm"][0])[perm],
                             np.tile(np.asarray(inp["b_q_norm"][0]), 2), np.tile(np.asarray(inp["b_k_norm"][0]), 2),
                             np.asarray(inp["b_subln"][0])] + [np.zeros(128, np.float32)] * 3, axis=1)),
        "lam_rep": rep(np.concatenate([inp["lambda_q1"][0], inp["lambda_k1"][0], inp["lambda_q2"][0], inp["lambda_k2"][0]])),
        "relT": f(np.concatenate([inp["rel_bias_table"], inp["rel_bias_table"]], axis=1)),
        "w_router": f(inp["w_router"][0]),
        "brout_rep": rep(inp["b_router"][0]),
        "w_gu": f(inp["w_gu"][0]),
        "bgu_fm": f(np.asarray(inp["b_gu"][0]).reshape(E, 2 * FC, 128).transpose(2, 0, 1).reshape(128, E * 2 * FC)),
        "w_down": f(inp["w_down"][0]),
        "b_down": f(inp["b_down"][0]),
        "iota_e": rep(np.arange(E)),
        "padrow": np.ascontiguousarray(np.broadcast_to((E * cfg.CAP + np.arange(128, dtype=np.float32))[:, None], (128, E))),
    }
    maps, orders = [], []
    tabs = [_static_tables(cfg, hf) for hf in range(2)]
    x = np.asarray(inp["x"])
    c = np.asarray(inp["c"])
    for core in range(8):
        b, hf = core // 2, core % 2
        order, ropec, ropes, ohr, cmat = tabs[hf]
        m = dict(shared)
        m["xkv"] = f(x[b][order])
        m["c_fm"] = f(c[b].reshape(DC, 128).T)
        m["ropec"], m["ropes"], m["ohr"], m["cmat"] = ropec, ropes, ohr, cmat
        maps.append(m)
        orders.append(order)
    return maps, orders


_CACHE = {}


def kernel(**inputs):
    cfg = Cfg()
    if "nc" not in _CACHE:
        _CACHE["nc"] = build(cfg)[0]
    nc = _CACHE["nc"]
    maps, orders = prep_inputs(cfg, inputs)
    res = run_bass_kernel_spmd(nc, maps, core_ids=list(range(8)))
    out = np.zeros((4, cfg.S, cfg.D), np.float32)
    for core in range(8):
        b = core // 2
        out[b, orders[core][:cfg.NQ]] = res.results[core]["out"]
    return out
```
